# Optimizing a Trainium2 kernel written in Bass

```python
import jax, jax.numpy as jnp
from jax import lax
import numpy as np

D_MODEL = 2048
BATCH = 4
SEQ = 2048
DEPTH = 4

HEAD_DIM = 128
N_HEADS = D_MODEL // HEAD_DIM
BRANCH_WIDTH = N_HEADS * HEAD_DIM
DSWA_PATTERNS = ((128, 1), (512, 4), (2048, 16))
N_GROUPS = 3
DSWA_QBLOCK = 128
MOBA_BLOCK = 256
MOBA_TOPK = 3
MOBA_QCHUNK = 16
N_MIXERS = 2
N_A = (DEPTH + 1) // 2
N_B = DEPTH // 2
A_IN_WIDTH = (3 * N_GROUPS + 1) * BRANCH_WIDTH
B_IN_WIDTH = 4 * BRANCH_WIDTH
EPS = 1e-6
NEG = -1e30

kernel_name = "hybrid_dilated_moba_adaln_trunk"


def rms_norm(x, w):
    xf = x.astype(jnp.float32)
    y = xf * lax.rsqrt(jnp.mean(xf * xf, axis=-1, keepdims=True) + EPS) * w.astype(jnp.float32)
    return y.astype(x.dtype)


def alibi_slopes(n):
    return jnp.exp2(-8.0 * jnp.arange(1, n + 1, dtype=jnp.float32) / n)


def dilated_window_attention(q, k, v, window, dilation, slopes):
    B, S, H, Dh = q.shape
    steps = window // dilation
    QB = DSWA_QBLOCK
    L = S // dilation
    nq = -(-L // QB)
    Lp = nq * QB

    def to_residue(a):
        a = a.reshape(B, L, dilation, H, Dh).transpose(0, 2, 1, 3, 4)
        return jnp.pad(a, ((0, 0), (0, 0), (0, Lp - L), (0, 0), (0, 0)))

    def band(a):
        cur = a.reshape(B, dilation, nq, QB, H, Dh)
        prev = jnp.pad(a, ((0, 0), (0, 0), (QB, 0), (0, 0), (0, 0)))[:, :, :Lp]
        prev = prev.reshape(B, dilation, nq, QB, H, Dh)
        return jnp.concatenate([prev, cur], axis=3)

    qr, kr, vr = to_residue(q), to_residue(k), to_residue(v)
    qb = qr.reshape(B, dilation, nq, QB, H, Dh)
    kw, vw = band(kr), band(vr)
    s = jnp.einsum('brnqhd,brnkhd->brnhqk', qb, kw).astype(jnp.float32) * (Dh ** -0.5)
    qi = jnp.arange(QB)[:, None]
    ki = jnp.arange(2 * QB)[None, :]
    step = qi + QB - ki
    key_l = jnp.arange(nq)[:, None, None] * QB + ki[None] - QB
    ok = ((step >= 0) & (step <= steps))[None] & (key_l >= 0)
    bias = -slopes[:, None, None] * (step * dilation).astype(jnp.float32)
    s = jnp.where(ok[:, None], s + bias, NEG)
    lse = jax.nn.logsumexp(s, axis=-1)
    p = jnp.exp(s - lse[..., None]).astype(v.dtype)
    o = jnp.einsum('brnhqk,brnkhd->brnqhd', p, vw)

    def from_residue(a):
        tail = a.shape[4:]
        a = a.reshape((B, dilation, Lp) + tail)[:, :, :L]
        a = jnp.moveaxis(a, 1, 2)
        return a.reshape((B, S) + tail)

    return from_residue(o), from_residue(jnp.swapaxes(lse, 3, 4))


def dilated_mixer(h, w_in, slopes):
    B, S, _ = h.shape
    parts = jnp.split(h @ w_in, 3 * N_GROUPS + 1, axis=-1)
    heads = lambda a: a.reshape(B, S, N_HEADS, HEAD_DIM)
    outs, lses = [], []
    for g, (window, dilation) in enumerate(DSWA_PATTERNS):
        o, lse = dilated_window_attention(heads(parts[3 * g]), heads(parts[3 * g + 1]),
                                          heads(parts[3 * g + 2]), window, dilation, slopes)
        outs.append(o)
        lses.append(lse)
    alpha = jax.nn.softmax(jnp.stack(lses), axis=0)
    o = jnp.einsum('gbsh,gbshd->bshd', alpha, jnp.stack(outs).astype(jnp.float32))
    return o.reshape(B, S, BRANCH_WIDTH).astype(h.dtype) * jax.nn.silu(parts[-1])


def moba_attention(q, k, v, slopes):
    B, S, H, Dh = q.shape
    BLK = MOBA_BLOCK
    nblk = -(-S // BLK)
    Sp = nblk * BLK
    q, k, v = [jnp.pad(a, ((0, 0), (0, Sp - S), (0, 0), (0, 0))).transpose(0, 2, 1, 3)
               for a in (q, k, v)]
    scale = Dh ** -0.5
    qb = q.reshape(B, H, nblk, BLK, Dh)
    kb = k.reshape(B, H, nblk, BLK, Dh)
    vb = v.reshape(B, H, nblk, BLK, Dh)

    s = jnp.einsum('bhnqd,bhnkd->bhnqk', qb, kb).astype(jnp.float32) * scale
    dist = jnp.arange(BLK)[:, None] - jnp.arange(BLK)[None, :]
    s = jnp.where(dist >= 0, s - slopes[:, None, None, None] * dist.astype(jnp.float32), NEG)
    lse_own = jax.nn.logsumexp(s, axis=-1)
    o_own = jnp.einsum('bhnqk,bhnkd->bhnqd', jnp.exp(s - lse_own[..., None]).astype(v.dtype), vb)
    lse_own = lse_own.reshape(B, H, Sp)
    o_own = o_own.reshape(B, H, Sp, Dh).astype(jnp.float32)

    n_sel = min(MOBA_TOPK, nblk - 1)
    if n_sel == 0:
        o = o_own
    else:
        pos = jnp.arange(Sp)
        qblk = pos // BLK
        kmean = kb.astype(jnp.float32).mean(axis=3).astype(kb.dtype)
        gate = jnp.einsum('bhtd,bhnd->bhtn', q, kmean).astype(jnp.float32)
        past = jnp.arange(nblk)[None, :] < qblk[:, None]
        gate = jnp.where(past, gate, NEG)
        _, idx = lax.top_k(gate, n_sel)
        valid = jnp.arange(n_sel)[None, :] < qblk[:, None]

        QC = MOBA_QCHUNK
        nC = Sp // QC
        q_c = jnp.moveaxis(q.reshape(B, H, nC, QC, Dh), 2, 0)
        i_c = jnp.moveaxis(idx.reshape(B, H, nC, QC, n_sel), 2, 0)
        t_c = pos.reshape(nC, QC)
        v_c = valid.reshape(nC, QC, n_sel)
        bi = jnp.arange(B)[:, None, None, None]
        hi = jnp.arange(H)[None, :, None, None]

        def attend(args):
            qc, ic, tc, vc = args
            kg = kb[bi, hi, ic]
            vg = vb[bi, hi, ic]
            sc = jnp.einsum('bhqd,bhqnkd->bhqnk', qc, kg).astype(jnp.float32) * scale
            kpos = ic[..., None] * BLK + jnp.arange(BLK)
            dd = (tc[:, None, None] - kpos).astype(jnp.float32)
            sc = jnp.where(vc[:, :, None], sc - slopes[:, None, None, None] * dd, NEG)
            sc = sc.reshape(B, H, QC, n_sel * BLK)
            lse = jax.nn.logsumexp(sc, axis=-1)
            p = jnp.exp(sc - lse[..., None]).astype(v.dtype).reshape(B, H, QC, n_sel, BLK)
            return jnp.einsum('bhqnk,bhqnkd->bhqd', p, vg), lse

        o_sel, lse_sel = lax.map(attend, (q_c, i_c, t_c, v_c))
        o_sel = jnp.moveaxis(o_sel, 0, 2).reshape(B, H, Sp, Dh).astype(jnp.float32)
        lse_sel = jnp.moveaxis(lse_sel, 0, 2).reshape(B, H, Sp)
        m = jnp.logaddexp(lse_own, lse_sel)
        o = (jnp.exp(lse_own - m)[..., None] * o_own
             + jnp.exp(lse_sel - m)[..., None] * o_sel)
    return o.transpose(0, 2, 1, 3)[:, :S]


def moba_mixer(h, w_in, slopes):
    B, S, _ = h.shape
    q, k, v, z = jnp.split(h @ w_in, 4, axis=-1)
    heads = lambda a: a.reshape(B, S, N_HEADS, HEAD_DIM)
    o = moba_attention(heads(q), heads(k), heads(v), slopes)
    return o.reshape(B, S, BRANCH_WIDTH).astype(h.dtype) * jax.nn.silu(z)


def setup_inputs(seed: int = 0) -> dict:
    key = jax.random.key(seed)
    ks = jax.random.split(key, 10)
    f32 = jnp.float32
    x = jax.random.normal(ks[0], (BATCH, SEQ, D_MODEL), f32)
    c = jax.random.normal(ks[1], (BATCH, D_MODEL), f32)
    norm_w = 1.0 + 0.02 * jax.random.normal(ks[2], (DEPTH, D_MODEL), f32)
    mod_w = jax.random.normal(ks[3], (DEPTH, D_MODEL, 3 * D_MODEL), f32) * (0.5 * D_MODEL ** -0.5)
    mod_b = 0.02 * jax.random.normal(ks[4], (DEPTH, 3 * D_MODEL), f32)
    a_w_in = jax.random.normal(ks[5], (N_A, D_MODEL, A_IN_WIDTH), f32) * D_MODEL ** -0.5
    a_w_out = jax.random.normal(ks[6], (N_A, BRANCH_WIDTH, D_MODEL), f32) * BRANCH_WIDTH ** -0.5
    b_w_in = jax.random.normal(ks[7], (N_B, D_MODEL, B_IN_WIDTH), f32) * D_MODEL ** -0.5
    b_w_out = jax.random.normal(ks[8], (N_B, BRANCH_WIDTH, D_MODEL), f32) * BRANCH_WIDTH ** -0.5
    final_norm_w = 1.0 + 0.02 * jax.random.normal(ks[9], (D_MODEL,), f32)
    return {"x": x, "c": c, "norm_w": norm_w, "mod_w": mod_w, "mod_b": mod_b,
            "a_w_in": a_w_in, "a_w_out": a_w_out, "b_w_in": b_w_in, "b_w_out": b_w_out,
            "final_norm_w": final_norm_w}


def reference(x, c, norm_w, mod_w, mod_b, a_w_in, a_w_out, b_w_in, b_w_out, final_norm_w):
    slopes = alibi_slopes(N_HEADS)
    cond = jax.nn.silu(c)
    for i in range(DEPTH):
        mod = cond @ mod_w[i] + mod_b[i]
        shift, scale, gate = jnp.split(mod[:, None, :], 3, axis=-1)
        h = rms_norm(x, norm_w[i]) * (1.0 + scale) + shift
        j = i // N_MIXERS
        if i % N_MIXERS == 0:
            y = dilated_mixer(h, a_w_in[j], slopes) @ a_w_out[j]
        else:
            y = moba_mixer(h, b_w_in[j], slopes) @ b_w_out[j]
        x = x + gate * y
    return rms_norm(x, final_norm_w)
```

```python
import contextlib
import numpy as np
import ml_dtypes
import concourse.bass as bass
import concourse.mybir as mybir
from concourse.bass_utils import run_bass_kernel_spmd

F32 = mybir.dt.float32
BF16 = mybir.dt.bfloat16
AF = mybir.ActivationFunctionType
ALU = mybir.AluOpType
AX = mybir.AxisListType
NPBF = ml_dtypes.bfloat16

P = 128
D = 2048
S = 2048
NH = 16
DEPTH = 4
NCH = D // P
NT = S // P
EPS = 1e-6
SCALE = 128 ** -0.5
NEG = -30000.0
ENGS = ["pe", "act", "dve", "pool", "sp"]
N_CORES = 8
NHL = NH // 2
GROUPS = [[0, 1], [2, 3], [4, 5], [6, 7]]


class Op:
    __slots__ = ("eng", "fn", "waits", "signal", "count", "dsem", "dval", "inc", "phase")


class Buf:
    def __init__(self, name):
        self.name = name
        self.w = None
        self.r = {}


class Builder:
    def __init__(self, nc):
        self.nc = nc
        self.ops = {e: [] for e in ENGS}
        self.dcount = {}
        self.gstack = contextlib.ExitStack()
        self.esem = {e: self.gstack.enter_context(nc.semaphore("es_" + e)) for e in ENGS}
        self.dsem = {}
        self.ecount = {e: 0 for e in ENGS}
        self.seen = {e: {} for e in ENGS}
        self.phase = 0
        self.lastd = {}

    def sb(self, stack, name, shape, dt):
        self.uid = getattr(self, "uid", 0) + 1
        return stack.enter_context(self.nc.sbuf_tensor("s%d_%s" % (self.uid, name), list(shape), dt))

    def _op(self, eng, fn, waits):
        o = Op()
        o.eng = eng
        o.fn = fn
        o.waits = [w for w in waits if w is not None and w.phase == self.phase]
        for w in o.waits:
            if w.dsem is None:
                w.signal = True
        o.signal = False
        o.dsem = None
        o.count = 0
        o.dval = 0
        o.inc = 16
        o.phase = self.phase
        self.ops[eng].append(o)
        return o

    def op(self, eng, fn, reads=(), writes=(), extra=(), dsem=None):
        waits = list(extra)
        for bf in reads:
            if bf.w is not None:
                waits.append(bf.w)
        for bf in writes:
            if bf.w is not None:
                waits.append(bf.w)
            waits.extend(bf.r.values())
        o = self._op(eng, fn, waits)
        if dsem is not None:
            o.dsem = dsem
            self.dcount[dsem] = self.dcount.get(dsem, 0) + 16
            o.dval = self.dcount[dsem]
            self.lastd[dsem] = o
        key = eng if dsem is None else ("d", dsem)
        for bf in reads:
            bf.r[key] = o
        for bf in writes:
            bf.w = o
            bf.r = {}
        return o

    def dma(self, eng, out, in_, dsem, reads=(), writes=(), extra=(), **kw):
        return self.op(eng, lambda e: e.dma_start(out=out, in_=in_, **kw), reads, writes, extra, dsem=dsem)

    def coll(self, fn, dsem, reads=(), writes=()):
        o = self.op("pool", fn, reads, writes, dsem=dsem)
        self.dcount[dsem] += 1 - 16
        o.dval = self.dcount[dsem]
        o.inc = 1
        return o

    def end_phase(self, final_waits=()):
        nc = self.nc
        lasts = {}
        for e in ENGS:
            for o in reversed(self.ops[e]):
                if o.dsem is None and o.fn is not None:
                    lasts[e] = o
                    break
        dl = [o for o in self.lastd.values() if o.phase == self.phase]
        for e in ENGS:
            self._op(e, None, [lasts[x] for x in lasts if x != e] + dl)
        for e in ENGS:
            c = self.ecount[e]
            for o in self.ops[e]:
                if o.dsem is None and o.signal:
                    c += 1
                    o.count = c
            self.ecount[e] = c
        for k in self.dcount:
            if k not in self.dsem:
                self.dsem[k] = self.gstack.enter_context(nc.semaphore("ds_%s" % (k,)))
        ops, esem, dsem = self.ops, self.esem, self.dsem

        def run(engname):
            def f(eng):
                seen = self.seen[engname]
                for o in ops[engname]:
                    for w in o.waits:
                        if w.dsem is not None:
                            sem, val, key = dsem[w.dsem], w.dval, ("d", w.dsem)
                        else:
                            if w.eng == engname and engname == "pe":
                                continue
                            sem, val, key = esem[w.eng], w.count, ("e", w.eng)
                        if seen.get(key, 0) >= val:
                            continue
                        eng.wait_ge(sem, val)
                        seen[key] = val
                    if o.fn is None:
                        continue
                    ins = o.fn(eng)
                    if o.dsem is not None:
                        ins.then_inc(dsem[o.dsem], o.inc)
                    elif o.signal:
                        ins.then_inc(esem[engname], 1)
            return f

        with nc.Block() as block:
            block.tensor(run("pe"))
            block.scalar(run("act"))
            block.vector(run("dve"))
            block.gpsimd(run("pool"))
            block.sync(run("sp"))
        self.ops = {e: [] for e in ENGS}
        self.phase += 1

    def close(self):
        self.gstack.close()


def build_program(layers, do_final, debug=False):
    nc = bass.Bass("TRN2", target_bir_lowering=False)
    dt_in = lambda n, s, d=F32: nc.dram_tensor(n, list(s), d, kind="ExternalInput").ap()
    x_in = dt_in("x_in", [S, D])
    cT_d = dt_in("cT", [P, NCH])
    nwT_d = dt_in("nwT", [P, DEPTH, NCH])
    fnw_d = dt_in("fnw", [1, D])
    mod_w = {l: dt_in("mod_w%d" % l, [D, 3 * D]) for l in layers}
    mod_b = dt_in("mod_b", [DEPTH, 3 * D])
    w_in_d = {l: dt_in("w_in%d" % l, [D, (10 if l % 2 == 0 else 4) * (D // 2)]) for l in layers}
    w_out_d = {l: dt_in("w_out%d" % l, [D, D]) for l in layers}
    ident_d = dt_in("ident", [P, P])
    tabA_d = dt_in("tabA", [NHL, P, 12 * P])
    tabB_d = dt_in("tabB", [NHL, P, 16 * P])
    pastb_d = dt_in("pastb", [P, NT * 8])
    x_out = nc.dram_tensor("x_out", [S, D], F32, kind="ExternalOutput").ap()
    xs = nc.dram_tensor("xs", [S, D], F32).ap()
    dk = dict(kind="ExternalOutput") if debug else {}
    oT_d = nc.dram_tensor("oT_s", [NHL, P, S], BF16, **dk).ap()
    og_t = [nc.dram_tensor("og%d" % i, [2 * P, S], BF16) for i in range(NHL)]
    modv = nc.dram_tensor("modv", [DEPTH, 3 * D], F32, **dk).ap()
    if debug:
        hT_dbg = nc.dram_tensor("hT_dbg", [P, NCH, S], BF16, kind="ExternalOutput").ap()
        g_dbg = nc.dram_tensor("g_dbg", [P, NT * 8], F32, kind="ExternalOutput").ap()
        e_dbg = nc.dram_tensor("e_dbg", [P, NT * 8], F32, kind="ExternalOutput").ap()
        m_dbg = nc.dram_tensor("m_dbg", [P, NT], F32, kind="ExternalOutput").ap()

    b = Builder(nc)
    g = contextlib.ExitStack()
    psb = [g.enter_context(nc.psum_tensor("ps%d" % i, [P, 512], F32)) for i in range(8)]
    PS = [Buf("ps%d" % i) for i in range(8)]
    ident_f = b.sb(g, "ident_f", [P, P], F32)
    ident_b = b.sb(g, "ident_b", [P, P], BF16)
    ones_b = b.sb(g, "ones_b", [P, P], BF16)
    hT = b.sb(g, "hT", [P, NCH, S], BF16)
    B_hT = Buf("hT")
    condT = b.sb(g, "condT", [P, NCH], BF16)
    nwT = b.sb(g, "nwT", [P, DEPTH, NCH], F32)
    B_const = Buf("const")
    B_id, B_nw, B_cond, B_ones = Buf("id"), Buf("nw"), Buf("cond"), Buf("ones")

    with contextlib.ExitStack() as st:
        cT_f = b.sb(st, "cT_f", [P, NCH], F32)
        modrow = b.sb(st, "modrow", [1, 3 * D], F32)
        mbrow = b.sb(st, "mbrow", [1, 3 * D], F32)
        wmod = [b.sb(st, "wmod%d" % i, [P, NCH, 512], BF16) for i in range(2)]
        B_wmod = [Buf("wmod%d" % i) for i in range(2)]
        B_cT, B_row, B_mb = Buf("cT"), Buf("modrow"), Buf("mbrow")
        b.dma("sp", ident_f[:], ident_d, "c0", writes=[B_id])
        b.dma("sp", nwT[:], nwT_d, "c1", writes=[B_nw])
        b.dma("sp", cT_f[:], cT_d, "c2", writes=[B_cT])
        b.op("dve", lambda e: e.tensor_copy(out=ident_b[:], in_=ident_f[:]), reads=[B_id], writes=[B_const])
        b.op("dve", lambda e: e.memset(ones_b[:], 1.0), writes=[B_ones])
        b.op("act", lambda e: e.activation(out=condT[:], in_=cT_f[:], func=AF.Silu), reads=[B_cT], writes=[B_cond])
        k = 0
        for l in layers:
            b.dma("sp", mbrow[:], mod_b[l:l + 1, :], "mb", writes=[B_mb])
            for cb in range(12):
                wt, bw = wmod[k % 2], B_wmod[k % 2]
                b.dma("pool", wt[:], mod_w[l][:, cb * 512:(cb + 1) * 512].rearrange("(c p) n -> p c n", p=P),
                      "wmod%d" % (k % 2), writes=[bw])
                pi = k % 2
                for c in range(NCH):
                    b.op("pe", lambda e, wt=wt, c=c, pi=pi: e.matmul(psb[pi][0:1, :], lhsT=condT[:, c:c + 1], rhs=wt[:, c, :],
                                                                     start=(c == 0), stop=(c == NCH - 1)),
                         reads=[bw, B_cond], writes=[PS[pi]] if c == 0 else [])
                PS[pi].w = b.ops["pe"][-1]
                b.op("dve", lambda e, cb=cb, pi=pi: e.tensor_tensor(out=modrow[:, cb * 512:(cb + 1) * 512], in0=psb[pi][0:1, :],
                                                                    in1=mbrow[:, cb * 512:(cb + 1) * 512], op=ALU.add),
                     reads=[PS[pi], B_mb], writes=[B_row])
                k += 1
            b.dma("sp", modv[l:l + 1, :], modrow[:], "mvst", reads=[B_row])
        b.end_phase()

    def norm_phase(l, src, final):
        with contextlib.ExitStack() as st:
            xt = [b.sb(st, "xt%d" % i, [P, D], F32) for i in range(2)]
            B_xt = [Buf("xt%d" % i) for i in range(2)]
            xn = [b.sb(st, "xn%d" % i, [P, D], F32) for i in range(2)]
            B_xn = [Buf("xn%d" % i) for i in range(2)]
            junk = b.sb(st, "junk", [P, D], BF16)
            ss = b.sb(st, "ss", [P, 2], F32)
            B_ss = Buf("ss")
            B_vec, B_sh, B_sc = Buf("vec"), Buf("sh"), Buf("sc")
            if final:
                fw = b.sb(st, "fw", [P, D], F32)
                b.dma("sp", fw[:], fnw_d.partition_broadcast(P), "fw", writes=[B_vec])
            else:
                sc = b.sb(st, "sc", [P, NCH], F32)
                sh = b.sb(st, "sh", [P, NCH], F32)
                weff = b.sb(st, "weff", [P, NCH], F32)
                b.dma("sp", sh[:], modv[l, 0:D].rearrange("(c p) -> p c", p=P), "sh", writes=[B_sh], allow_slow_non_contiguous=True)
                b.dma("sp", sc[:], modv[l, D:2 * D].rearrange("(c p) -> p c", p=P), "sc", writes=[B_sc], allow_slow_non_contiguous=True)
                b.op("dve", lambda e: e.scalar_tensor_tensor(out=weff[:], in0=sc[:], scalar=1.0, in1=nwT[:, l, :],
                                                            op0=ALU.add, op1=ALU.mult), reads=[B_sc, B_nw], writes=[B_vec])
            for t in range(NT):
                i = t % 2
                b.dma("sp", xt[i][:], src[t * P:(t + 1) * P, :], "xt%d" % i, writes=[B_xt[i]])
                b.op("dve", lambda e, i=i: e.memset(ss[:, i:i + 1], 0.0), writes=[B_ss])
                b.op("act", lambda e, i=i: e.activation(out=junk[:], in_=xt[i][:], func=AF.Square, accum_out=ss[:, i:i + 1]),
                     reads=[B_xt[i]], writes=[B_ss])
                b.op("dve", lambda e, i=i: e.tensor_scalar(out=ss[:, i:i + 1], in0=ss[:, i:i + 1], scalar1=1.0 / D, scalar2=EPS,
                                                           op0=ALU.mult, op1=ALU.add), reads=[B_ss], writes=[B_ss])
                b.op("act", lambda e, i=i: e.sqrt(out=ss[:, i:i + 1], in_=ss[:, i:i + 1]), reads=[B_ss], writes=[B_ss])
                b.op("dve", lambda e, i=i: e.reciprocal(out=ss[:, i:i + 1], in_=ss[:, i:i + 1]), reads=[B_ss], writes=[B_ss])
                if final:
                    b.op("dve", lambda e, i=i: e.scalar_tensor_tensor(out=xn[i][:], in0=xt[i][:], scalar=ss[:, i:i + 1], in1=fw[:],
                                                                      op0=ALU.mult, op1=ALU.mult),
                         reads=[B_xt[i], B_ss, B_vec], writes=[B_xn[i]])
                    b.dma("sp", x_out[t * P:(t + 1) * P, :], xn[i][:], "xo%d" % i, reads=[B_xn[i]])
                else:
                    b.op("dve", lambda e, i=i: e.tensor_scalar(out=xn[i][:], in0=xt[i][:], scalar1=ss[:, i:i + 1], scalar2=None,
                                                               op0=ALU.mult), reads=[B_xt[i], B_ss], writes=[B_xn[i]])
                    for q4 in range(4):
                        pi = (t * 4 + q4) % 4
                        for j in range(4):
                            c = q4 * 4 + j
                            b.op("pe", lambda e, i=i, c=c, j=j, pi=pi: e.transpose(out=psb[pi][:, j * P:(j + 1) * P],
                                                                                   in_=xn[i][:, c * P:(c + 1) * P], identity=ident_f[:]),
                                 reads=[B_xn[i], B_id], writes=[PS[pi]] if j == 0 else [])
                        PS[pi].w = b.ops["pe"][-1]
                        for j in range(4):
                            c = q4 * 4 + j
                            b.op("act", lambda e, c=c, j=j, pi=pi, t=t: e.activation(
                                out=hT[:, c, t * P:(t + 1) * P], in_=psb[pi][:, j * P:(j + 1) * P], func=AF.Identity,
                                bias=sh[:, c:c + 1], scale=weff[:, c:c + 1]), reads=[PS[pi], B_vec, B_sh], writes=[B_hT])
            if debug and not final and l == layers[0]:
                b.dma("sp", hT_dbg, hT[:], "hdbg", reads=[B_hT])
            b.end_phase()

    def out_phase(l, w_out, src):
        with contextlib.ExitStack() as st:
            wo = [b.sb(st, "wo%d" % i, [P, NCH, 512], BF16) for i in range(2)]
            B_wo = [Buf("wo%d" % i) for i in range(2)]
            ot = [b.sb(st, "ot%d" % i, [P, NH, 512], BF16) for i in range(2)]
            B_ot = [Buf("ot%d" % i) for i in range(2)]
            xp = [b.sb(st, "xp%d" % i, [P, 512], F32) for i in range(3)]
            B_xp = [Buf("xp%d" % i) for i in range(3)]
            gate = b.sb(st, "gate", [P, D], F32)
            B_gate = Buf("gate")
            b.dma("sp", gate[:], modv[l:l + 1, 2 * D:3 * D].partition_broadcast(P), "gate", writes=[B_gate])
            k = 0
            n = 0
            for cb in range(4):
                b.dma("pool", wo[cb % 2][:], w_out[:, cb * 512:(cb + 1) * 512].rearrange("(c p) n -> p c n", p=P),
                      "wo%d" % (cb % 2), writes=[B_wo[cb % 2]])
                for tt in range(4):
                    oi = k % 2
                    for hl in range(NHL):
                        b.dma("sp", ot[oi][:].rearrange("p (r h) t -> p r h t", r=2)[:, :, hl, :],
                              og_t[hl].ap()[:, tt * 512:(tt + 1) * 512].rearrange("(r p) t -> p r t", p=P), "ot%d" % oi,
                              writes=[B_ot[oi]] if hl == 0 else [])
                    B_ot[oi].w = b.ops["sp"][-1]
                    k += 1
                    for s4 in range(4):
                        t = tt * 4 + s4
                        pi = n % 4
                        xi = n % 3
                        n += 1
                        b.dma("sp", xp[xi][:], src[t * P:(t + 1) * P, cb * 512:(cb + 1) * 512], "xp%d" % xi, writes=[B_xp[xi]])
                        for c in range(NH):
                            b.op("pe", lambda e, oi=oi, c=c, s4=s4, cb=cb, pi=pi: e.matmul(
                                psb[pi][:, :], lhsT=ot[oi][:, c, s4 * P:(s4 + 1) * P], rhs=wo[cb % 2][:, c, :],
                                start=(c == 0), stop=(c == NH - 1)),
                                reads=[B_ot[oi], B_wo[cb % 2]], writes=[PS[pi]] if c == 0 else [])
                        PS[pi].w = b.ops["pe"][-1]
                        tmp = xp[xi]
                        b.op("dve", lambda e, pi=pi, cb=cb, xi=xi: e.tensor_tensor(
                            out=psb[pi][:, :], in0=psb[pi][:, :], in1=gate[:, cb * 512:(cb + 1) * 512], op=ALU.mult),
                            reads=[B_gate], writes=[PS[pi]])
                        b.op("dve", lambda e, pi=pi, xi=xi: e.tensor_tensor(out=xp[xi][:], in0=psb[pi][:, :], in1=xp[xi][:], op=ALU.add),
                             reads=[PS[pi]], writes=[B_xp[xi]])
                        b.dma("sp", xs[t * P:(t + 1) * P, cb * 512:(cb + 1) * 512], xp[xi][:], "xps%d" % xi, reads=[B_xp[xi]])
            b.end_phase()

    def mixer_phase(l, kind, w_in):
        ngrp = 3 if kind == "A" else 1
        dil = [1, 4, 16] if kind == "A" else [1]
        with contextlib.ExitStack() as st:
            NW = 6
            wr = [b.sb(st, "wr%d" % i, [P, NCH, P], BF16) for i in range(NW)]
            B_wr = [Buf("wr%d" % i) for i in range(NW)]
            QT = [b.sb(st, "QT%d" % i, [P, S], BF16) for i in range(2)]
            KT = [b.sb(st, "KT%d" % i, [P, S], BF16) for i in range(2)]
            V = [b.sb(st, "V%d" % i, [P, NT, P], BF16) for i in range(2)]
            B_Q = [Buf("Q%d" % i) for i in range(2)]
            B_K = [Buf("K%d" % i) for i in range(2)]
            B_V = [Buf("V%d" % i) for i in range(2)]
            SZ = [b.sb(st, "SZ%d" % i, [P, S], BF16) for i in range(2)]
            B_SZ = [Buf("SZ%d" % i) for i in range(2)]
            acc_o = b.sb(st, "acc_o", [P, S], F32)
            acc_s = b.sb(st, "acc_s", [P, S], F32)
            B_acc = Buf("acc")
            osb = [b.sb(st, "osb%d" % i, [P, S], BF16) for i in range(2)]
            B_osb = [Buf("osb%d" % i) for i in range(2)]
            tmp = [b.sb(st, "tmp%d" % i, [P, 512], F32) for i in range(2)]
            B_tmp = [Buf("tmp%d" % i) for i in range(2)]
            PT = [b.sb(st, "PT%d" % i, [P, 512], BF16) for i in range(3)]
            B_PT = [Buf("PT%d" % i) for i in range(3)]
            tw = 12 * P if kind == "A" else 16 * P
            tab = [b.sb(st, "tab%d" % i, [P, tw], F32) for i in range(2)]
            B_tab = [Buf("tab%d" % i) for i in range(2)]
            tab_d = tabA_d if kind == "A" else tabB_d
            if kind == "B":
                kmf = b.sb(st, "kmf", [P, 8], F32)
                kmb = b.sb(st, "kmb", [P, 8], BF16)
                pastb = b.sb(st, "pastb", [P, NT, 8], F32)
                gsb = b.sb(st, "gsb", [P, NT, 8], F32)
                g2 = b.sb(st, "g2", [P, NT, 8], F32)
                eq = b.sb(st, "eq", [P, NT, 8], F32)
                mx = b.sb(st, "mx", [P, NT], F32)
                nsel = [b.sb(st, "nsel%d" % i, [P, NT, 8], BF16) for i in range(2)]
                B_nsel = [Buf("nsel%d" % i) for i in range(2)]
                B_km, B_g = Buf("km"), Buf("g")
                B_km2, B_kj = Buf("km2"), Buf("kj")
                kjunk = b.sb(st, "kjunk", [P, 256], BF16)
                B_pb = Buf("pastb")
                b.dma("sp", pastb[:].rearrange("p a b -> p (a b)"), pastb_d, "pastb", writes=[B_pb])
            wk = [0]
            ps_in = [0]
            un = [0]

            def load_w(col0):
                i = wk[0] % NW
                wk[0] += 1
                b.dma("pool", wr[i][:], w_in[:, col0:col0 + P].rearrange("(c p) n -> p c n", p=P), "wr%d" % i, writes=[B_wr[i]])
                return i

            def proj_T(col0, dst, B_dst, d, func=None):
                wi = load_w(col0)
                for tt in range(4):
                    pi = ps_in[0] % 2
                    ps_in[0] += 1
                    for c in range(NCH):
                        b.op("pe", lambda e, wi=wi, c=c, tt=tt, pi=pi: e.matmul(
                            psb[pi][:, :], lhsT=wr[wi][:, c, :], rhs=hT[:, c, tt * 512:(tt + 1) * 512],
                            start=(c == 0), stop=(c == NCH - 1)),
                            reads=[B_wr[wi], B_hT], writes=[PS[pi]] if c == 0 else [])
                    PS[pi].w = b.ops["pe"][-1]
                    if d == 1:
                        o_ap = dst[:, tt * 512:(tt + 1) * 512]
                        i_ap = psb[pi][:, :]
                    else:
                        nl = 512 // d
                        o_ap = dst[:, :].rearrange("p (r l) -> p r l", r=d)[:, :, tt * nl:(tt + 1) * nl]
                        i_ap = psb[pi][:, :].rearrange("p (l r) -> p r l", r=d)
                    if func is None:
                        b.op("dve", lambda e, o_ap=o_ap, i_ap=i_ap: e.tensor_copy(out=o_ap, in_=i_ap), reads=[PS[pi]], writes=[B_dst])
                    else:
                        b.op("act", lambda e, o_ap=o_ap, i_ap=i_ap: e.activation(out=o_ap, in_=i_ap, func=func),
                             reads=[PS[pi]], writes=[B_dst])

            def proj_V(col0, dst, B_dst, d):
                wi = load_w(col0)
                L = S // d
                for j4 in range(4):
                    pi = ps_in[0] % 2
                    ps_in[0] += 1
                    for jj in range(4):
                        j = j4 * 4 + jj
                        r, l0 = (j * P) // L, (j * P) % L
                        for c in range(NCH):
                            if d == 1:
                                lh = hT[:, c, j * P:(j + 1) * P]
                            else:
                                lh = hT[:, c, :].rearrange("p (l r) -> p r l", r=d)[:, r, l0:l0 + P]
                            b.op("pe", lambda e, wi=wi, c=c, lh=lh, jj=jj, pi=pi: e.matmul(
                                psb[pi][:, jj * P:(jj + 1) * P], lhsT=lh, rhs=wr[wi][:, c, :],
                                start=(c == 0), stop=(c == NCH - 1)),
                                reads=[B_wr[wi], B_hT], writes=[PS[pi]] if (c == 0 and jj == 0) else [])
                    PS[pi].w = b.ops["pe"][-1]
                    b.op("act", lambda e, j4=j4, pi=pi: e.copy(out=dst[:, j4 * 4:(j4 + 1) * 4, :].rearrange("p a b -> p (a b)"),
                                                              in_=psb[pi][:, :]), reads=[PS[pi]], writes=[B_dst])

            def attention(h, gi, si, ti, units, first_group):
                qt, kt, v = QT[si], KT[si], V[si]
                flat = []
                for u in units:
                    firsts, lasts = {}, {}
                    n = 0
                    for stp in u["steps"]:
                        for (pos, ktile, sel) in stp["blocks"]:
                            firsts.setdefault(pos, n)
                            lasts[pos] = n
                            n += 1
                    uo = un[0]
                    un[0] += 1
                    n = 0
                    for sidx, stp in enumerate(u["steps"]):
                        flat.append((u, uo, sidx, stp, firsts, lasts, n))
                        n += len(stp["blocks"])

                def qk(item):
                    u, uo, sidx, stp, firsts, lasts, n0 = item
                    n = sn[0]
                    sn[0] += 1
                    sb_i, k2, k3 = 2 + n % 2, n % 2, n % 3
                    blocks = stp["blocks"]
                    nb = len(blocks)
                    for i, (pos, ktile, sel) in enumerate(blocks):
                        b.op("pe", lambda e, i=i, ktile=ktile, sb_i=sb_i, sel=sel, qc=u["qcols"][pos]: e.matmul(
                            psb[sb_i][:, i * P:(i + 1) * P], lhsT=kt[:, ktile * P:(ktile + 1) * P], rhs=qt[:, qc:qc + P],
                            start=True, stop=(sel is None)),
                            reads=[B_K[si], B_Q[si]], writes=[PS[sb_i]] if i == 0 else [])
                        if sel is not None:
                            b.op("pe", lambda e, i=i, sb_i=sb_i, sel=sel: e.matmul(
                                psb[sb_i][:, i * P:(i + 1) * P], lhsT=sel, rhs=ident_b[:], start=False, stop=True),
                                reads=[stp["selbuf"], B_const])
                    PS[sb_i].w = b.ops["pe"][-1]
                    tb = stp["tab"]
                    b.op("dve", lambda e, sb_i=sb_i, k2=k2, nb=nb, tb=tb: e.scalar_tensor_tensor(
                        out=tmp[k2][:, 0:nb * P].rearrange("p (a b) -> p a b", b=P),
                        in0=psb[sb_i][:, 0:nb * P].rearrange("p (a b) -> p a b", b=P), scalar=SCALE, in1=tb,
                        op0=ALU.mult, op1=ALU.add), reads=[PS[sb_i], B_tab[ti]], writes=[B_tmp[k2]])
                    b.op("act", lambda e, k2=k2, k3=k3, nb=nb: e.activation(
                        out=PT[k3][:, 0:nb * P], in_=tmp[k2][:, 0:nb * P], func=AF.Exp),
                        reads=[B_tmp[k2]], writes=[B_PT[k3]])
                    return k3

                def pv(item, k3):
                    u, uo, sidx, stp, firsts, lasts, n0 = item
                    po, pso = 4 + uo % 2, 6 + uo % 2
                    npos = len(u["qcols"])
                    for bank, lhs_fn, rd in ((po, lambda ktile: v[:, ktile, :], [B_V[si], B_PT[k3]]),
                                             (pso, lambda ktile: ones_b[:], [B_ones, B_PT[k3]])):
                        for i, (pos, ktile, sel) in enumerate(stp["blocks"]):
                            n = n0 + i
                            b.op("pe", lambda e, i=i, pos=pos, lh=lhs_fn(ktile), k3=k3, bank=bank, f=(firsts[pos] == n),
                                 la=(lasts[pos] == n): e.matmul(
                                psb[bank][:, pos * P:(pos + 1) * P], lhsT=lh, rhs=PT[k3][:, i * P:(i + 1) * P],
                                start=f, stop=la), reads=rd, writes=[PS[bank]] if (sidx == 0 and i == 0) else [])
                    if sidx != len(u["steps"]) - 1:
                        return
                    PS[po].w = b.ops["pe"][-1]
                    PS[pso].w = b.ops["pe"][-1]
                    ao, as_ = u["acc"](acc_o), u["acc"](acc_s)
                    shp = u["pshape"]
                    pin_o = psb[po][:, 0:npos * P]
                    pin_s = psb[pso][:, 0:npos * P]
                    if shp is not None:
                        pin_o = pin_o.rearrange("p (a b) -> p a b", b=shp)
                        pin_s = pin_s.rearrange("p (a b) -> p a b", b=shp)
                    if first_group:
                        b.op("dve", lambda e, ao=ao, pin_o=pin_o: e.tensor_copy(out=ao, in_=pin_o), reads=[PS[po]], writes=[B_acc])
                        b.op("dve", lambda e, as_=as_, pin_s=pin_s: e.tensor_copy(out=as_, in_=pin_s), reads=[PS[pso]], writes=[B_acc])
                    else:
                        b.op("dve", lambda e, ao=ao, pin_o=pin_o: e.tensor_tensor(out=ao, in0=pin_o, in1=ao, op=ALU.add),
                             reads=[PS[po]], writes=[B_acc])
                        b.op("dve", lambda e, as_=as_, pin_s=pin_s: e.tensor_tensor(out=as_, in0=pin_s, in1=as_, op=ALU.add),
                             reads=[PS[pso]], writes=[B_acc])

                prev = None
                for item in flat:
                    k3 = qk(item)
                    if prev is not None:
                        pv(*prev)
                    prev = (item, k3)
                pv(*prev)

            sn = [0]
            pv_cnt = [0]
            sidx_g = 0
            DH = D // 2
            pend_cc = None
            for h in range(NHL):
                ti = h % 2
                b.dma("sp", tab[ti][:], tab_d[h], "tab%d" % ti, writes=[B_tab[ti]])
                zi = h % 2
                zcol = (9 * DH if kind == "A" else 3 * DH) + h * P
                for gi in range(ngrp):
                    d = dil[gi]
                    L = S // d
                    si = sidx_g % 2
                    sidx_g += 1
                    base = gi * 3 * DH if kind == "A" else 0
                    proj_T(base + h * P, QT[si], B_Q[si], d)
                    proj_T(base + DH + h * P, KT[si], B_K[si], d)
                    proj_V(base + 2 * DH + h * P, V[si], B_V[si], d)
                    if pend_cc is not None:
                        pend_cc()
                        pend_cc = None
                    if gi == 0:
                        proj_T(zcol, SZ[zi], B_SZ[zi], 1, func=AF.Silu)
                    units = []
                    if kind == "A":
                        tb0 = gi * 4 * P
                        for u4 in range(4):
                            js = [u4 * 4 + i for i in range(4)]
                            ns = [(j * P % L) // P for j in js]
                            steps = []
                            if gi == 2:
                                steps.append(dict(blocks=[(i, js[i], None) for i in range(4)],
                                                  tab=tab[ti][:, tb0:tb0 + 4 * P].rearrange("p (a b) -> p a b", b=P)))
                            else:
                                for half in range(2):
                                    blocks = []
                                    for i in (2 * half, 2 * half + 1):
                                        if ns[i] >= 1:
                                            blocks.append((i, js[i] - 1, None))
                                        blocks.append((i, js[i], None))
                                    off = 4 - len(blocks)
                                    steps.append(dict(blocks=blocks, tab=tab[ti][:, tb0 + off * P:tb0 + 4 * P].rearrange(
                                        "p (a b) -> p a b", b=P)))
                            if d == 1:
                                accf = lambda a, u4=u4: a[:, u4 * 512:(u4 + 1) * 512]
                                pshape = None
                            elif d == 4:
                                accf = lambda a, u4=u4: a[:, :].rearrange("p (l r) -> p r l", r=4)[:, u4, :]
                                pshape = None
                            else:
                                accf = lambda a, u4=u4: a[:, :].rearrange("p (l r) -> p r l", r=16)[:, u4 * 4:(u4 + 1) * 4, :]
                                pshape = P
                            units.append(dict(qcols=[j * P for j in js], steps=steps, acc=accf, pshape=pshape))
                    else:
                        ni = h % 2
                        b.op("dve", lambda e: e.memset(kmf[:], 0.0), writes=[B_km])
                        for n in range(8):
                            b.op("act", lambda e, si=si, n=n: e.activation(out=kjunk[:], in_=KT[si][:, n * 256:(n + 1) * 256], func=AF.Identity,
                                                                            scale=1.0 / 256, accum_out=kmf[:, n:n + 1]),
                                 reads=[B_K[si], B_km], writes=[B_kj] if n < 7 else [B_kj, B_km2])
                        b.op("dve", lambda e: e.tensor_copy(out=kmb[:], in_=kmf[:]), reads=[B_km2], writes=[B_km])
                        pi = ps_in[0] % 2
                        ps_in[0] += 1
                        for t in range(NT):
                            b.op("pe", lambda e, t=t, si=si, pi=pi: e.matmul(psb[pi][:, t * 8:(t + 1) * 8], lhsT=QT[si][:, t * P:(t + 1) * P],
                                                                            rhs=kmb[:], start=True, stop=True),
                                 reads=[B_Q[si], B_km], writes=[PS[pi]] if t == 0 else [])
                        PS[pi].w = b.ops["pe"][-1]
                        g3v = lambda a: a[:]
                        mxb = lambda: mx[:].unsqueeze(2).to_broadcast([P, NT, 8])
                        b.op("dve", lambda e, pi=pi: e.tensor_tensor(out=gsb[:], in0=psb[pi][:, 0:NT * 8].rearrange("p (a b) -> p a b", b=8),
                                                                     in1=pastb[:], op=ALU.add), reads=[PS[pi], B_pb], writes=[B_g])
                        b.op("dve", lambda e: e.tensor_copy(out=g2[:], in_=gsb[:]), reads=[B_g], writes=[B_g])
                        for rep in range(2):
                            b.op("dve", lambda e: e.tensor_reduce(out=mx[:], in_=g2[:], axis=AX.X, op=ALU.max), reads=[B_g], writes=[B_g])
                            b.op("dve", lambda e: e.tensor_tensor(out=eq[:], in0=g2[:], in1=mxb(), op=ALU.is_ge), reads=[B_g], writes=[B_g])
                            b.op("dve", lambda e: e.scalar_tensor_tensor(out=g2[:], in0=eq[:], scalar=-1e30, in1=g2[:],
                                                                        op0=ALU.mult, op1=ALU.add), reads=[B_g], writes=[B_g])
                        b.op("dve", lambda e: e.tensor_reduce(out=mx[:], in_=g2[:], axis=AX.X, op=ALU.max), reads=[B_g], writes=[B_g])
                        b.op("dve", lambda e: e.tensor_scalar(out=mx[:], in0=mx[:], scalar1=-1e29, scalar2=None, op0=ALU.max),
                             reads=[B_g], writes=[B_g])
                        b.op("dve", lambda e: e.tensor_tensor(out=eq[:], in0=gsb[:], in1=mxb(), op=ALU.is_ge), reads=[B_g], writes=[B_g])
                        if debug and h == 1 and l == layers[0]:
                            b.dma("sp", g_dbg, gsb[:].rearrange("p a b -> p (a b)"), "gdbg", reads=[B_g])
                            b.dma("sp", e_dbg, eq[:].rearrange("p a b -> p (a b)"), "edbg", reads=[B_g])
                            b.dma("sp", m_dbg, mx[:], "mdbg", reads=[B_g])
                        b.op("dve", lambda e, ni=ni: e.tensor_scalar(out=nsel[ni][:], in0=eq[:], scalar1=-1.0, scalar2=-NEG,
                                                                     op0=ALU.add, op1=ALU.mult), reads=[B_g], writes=[B_nsel[ni]])
                        for q4 in range(4):
                            qts = [q4 * 4 + i for i in range(4)]
                            steps = []
                            for pos in range(4):
                                qtile = qts[pos]
                                for k0_ in range(0, qtile + 1, 4):
                                    kts = list(range(k0_, min(k0_ + 4, qtile + 1)))
                                    blocks = []
                                    for ktile in kts:
                                        sel = None
                                        if qtile // 2 > ktile // 2:
                                            sel = nsel[ni][:, qtile, (ktile // 2):(ktile // 2) + 1].to_broadcast([P, P])
                                        blocks.append((pos, ktile, sel))
                                    r0 = 15 - qtile + kts[0]
                                    steps.append(dict(blocks=blocks, selbuf=B_nsel[ni],
                                                      tab=tab[ti][:, r0 * P:(r0 + len(kts)) * P].rearrange("p (a b) -> p a b", b=P)))
                            units.append(dict(qcols=[q * P for q in qts], steps=steps,
                                              acc=lambda a, q4=q4: a[:, q4 * 512:(q4 + 1) * 512], pshape=None))
                    attention(h, gi, si, ti, units, gi == 0)
                oi = h % 2
                b.op("dve", lambda e: e.reciprocal(out=acc_s[:], in_=acc_s[:]), reads=[B_acc], writes=[B_acc])
                b.op("dve", lambda e: e.tensor_tensor(out=acc_o[:], in0=acc_o[:], in1=acc_s[:], op=ALU.mult), reads=[B_acc], writes=[B_acc])
                b.op("dve", lambda e, oi=oi, zi=zi: e.tensor_tensor(out=osb[oi][:], in0=acc_o[:], in1=SZ[zi][:], op=ALU.mult),
                     reads=[B_acc, B_SZ[zi]], writes=[B_osb[oi]])
                st_op = b.dma("sp", oT_d[h], osb[oi][:], "ost%d" % oi, reads=[B_osb[oi]])

                def issue_cc(h=h, st_op=st_op):
                    o = b.coll(lambda e: e.collective_compute("AllGather", ALU.bypass, replica_groups=GROUPS,
                                                              ins=[oT_d[h].opt()], outs=[og_t[h].ap().opt()]), "cc")
                    o.waits.append(st_op)
                pend_cc = issue_cc
            pend_cc()
            b.end_phase()

    src = x_in
    for l in layers:
        norm_phase(l, src, False)
        mixer_phase(l, "A" if l % 2 == 0 else "B", w_in_d[l])
        out_phase(l, w_out_d[l], src)
        src = xs
    if do_final:
        norm_phase(0, src, True)
    else:
        with contextlib.ExitStack() as st:
            cp = [b.sb(st, "cp%d" % i, [P, D], F32) for i in range(2)]
            B_cp = [Buf("cp%d" % i) for i in range(2)]
            for t in range(NT):
                i = t % 2
                b.dma("sp", cp[i][:], src[t * P:(t + 1) * P, :], "cpl%d" % i, writes=[B_cp[i]])
                b.dma("sp", x_out[t * P:(t + 1) * P, :], cp[i][:], "cps%d" % i, reads=[B_cp[i]])
            b.end_phase()
    g.close()
    b.close()
    return nc


def _consts():
    slopes = np.exp2(-8.0 * np.arange(1, NH + 1, dtype=np.float64) / NH)
    k = np.arange(P)[:, None].astype(np.float64)
    q = np.arange(P)[None, :].astype(np.float64)
    tabA = np.zeros((NH, P, 12, P), np.float32)
    for h in range(NH):
        for gi, d in enumerate([1, 4, 16]):
            for blk in range(4):
                prev = (blk % 2 == 0) and gi < 2
                if not prev:
                    step = q - k
                    ok = step >= 0
                else:
                    step = q - k + P
                    ok = step <= P
                tabA[h, :, gi * 4 + blk, :] = np.where(ok, np.maximum(-slopes[h] * d * step, NEG), NEG)
    tabB = np.zeros((NH, P, 16, P), np.float32)
    for h in range(NH):
        for dl in range(16):
            dist = q - k + P * dl
            tabB[h, :, 15 - dl, :] = np.where(dist >= 0, np.maximum(-slopes[h] * dist, NEG), NEG)
    pastb = np.zeros((P, NT, 8), np.float32)
    for t in range(NT):
        for n in range(8):
            if n >= t // 2:
                pastb[:, t, n] = -1e30
    return (np.eye(P, dtype=np.float32), tabA.reshape(NH, P, 12 * P), tabB.reshape(NH, P, 16 * P), pastb.reshape(P, NT * 8))


_PROG_CACHE = {}


def _run(layers, do_final, xin, inputs, debug=False):
    key = (tuple(layers), do_final, debug)
    if key not in _PROG_CACHE:
        _PROG_CACHE[key] = build_program(list(layers), do_final, debug)
    nc = _PROG_CACHE[key]
    ident, tabA, tabB, pastb = _consts()
    c = np.asarray(inputs["c"], np.float32)
    nw = np.asarray(inputs["norm_w"], np.float32)
    nwT = np.ascontiguousarray(nw.reshape(DEPTH, NCH, P).transpose(2, 0, 1))
    fnw = np.asarray(inputs["final_norm_w"], np.float32).reshape(1, D)
    mod_b = np.ascontiguousarray(np.asarray(inputs["mod_b"], np.float32))
    per_half = [dict(), dict()]
    shared = {"mod_b": mod_b}
    for l in layers:
        shared["mod_w%d" % l] = np.ascontiguousarray(np.asarray(inputs["mod_w"][l], np.float32))
        pre = "a" if l % 2 == 0 else "b"
        w = np.asarray(inputs[pre + "_w_in"][l // 2], np.float32)
        nparts = w.shape[1] // D
        w4 = w.reshape(D, nparts, 2, D // 2)
        for hh in range(2):
            per_half[hh]["w_in%d" % l] = np.ascontiguousarray(w4[:, :, hh, :].reshape(D, nparts * (D // 2)))
        shared["w_out%d" % l] = np.ascontiguousarray(np.asarray(inputs[pre + "_w_out"][l // 2], np.float32))
    in_maps = []
    for core in range(N_CORES):
        bb, hh = core // 2, core % 2
        m = dict(shared)
        m.update(per_half[hh])
        m["x_in"] = np.ascontiguousarray(xin[bb])
        m["cT"] = np.ascontiguousarray(c[bb].reshape(NCH, P).T)
        m["nwT"] = nwT
        m["fnw"] = fnw
        m["ident"] = ident
        m["tabA"] = np.ascontiguousarray(tabA[hh * NHL:(hh + 1) * NHL])
        m["tabB"] = np.ascontiguousarray(tabB[hh * NHL:(hh + 1) * NHL])
        m["pastb"] = pastb
        in_maps.append(m)
    res = run_bass_kernel_spmd(nc, in_maps, core_ids=list(range(N_CORES)))
    if debug:
        _run.dbg = res.results
    return np.stack([np.asarray(res.results[2 * i]["x_out"], np.float32) for i in range(N_CORES // 2)], 0)


LAUNCH_PLAN = [([0, 1, 2, 3], True)]


def kernel(x, c, norm_w, mod_w, mod_b, a_w_in, a_w_out, b_w_in, b_w_out, final_norm_w):
    inputs = dict(c=c, norm_w=norm_w, mod_w=mod_w, mod_b=mod_b, a_w_in=a_w_in, a_w_out=a_w_out,
                  b_w_in=b_w_in, b_w_out=b_w_out, final_norm_w=final_norm_w)
    cur = np.asarray(x, np.float32)
    for layers, fin in LAUNCH_PLAN:
        cur = _run(layers, fin, cur, inputs)
    return cur
```

```python
import contextlib
import numpy as np
import ml_dtypes
import concourse.bass as bass
import concourse.mybir as mybir
from concourse.bass_utils import run_bass_kernel_spmd

F32 = mybir.dt.float32
BF16 = mybir.dt.bfloat16
AF = mybir.ActivationFunctionType
ALU = mybir.AluOpType
AX = mybir.AxisListType
NPBF = ml_dtypes.bfloat16

P = 128
D = 2048
S = 2048
NH = 16
DEPTH = 4
NCH = D // P
NT = S // P
EPS = 1e-6
SCALE = 128 ** -0.5
NEG = -30000.0
ENGS = ["pe", "act", "dve", "pool", "sp"]
N_CORES = 8
NHL = NH // 2
GROUPS = [[0, 1], [2, 3], [4, 5], [6, 7]]


class Op:
    __slots__ = ("eng", "fn", "waits", "signal", "count", "dsem", "dval", "inc", "phase")


class Buf:
    def __init__(self, name):
        self.name = name
        self.w = None
        self.r = {}


class Builder:
    def __init__(self, nc):
        self.nc = nc
        self.ops = {e: [] for e in ENGS}
        self.dcount = {}
        self.gstack = contextlib.ExitStack()
        self.esem = {e: self.gstack.enter_context(nc.semaphore("es_" + e)) for e in ENGS}
        self.dsem = {}
        self.ecount = {e: 0 for e in ENGS}
        self.seen = {e: {} for e in ENGS}
        self.phase = 0
        self.lastd = {}

    def sb(self, stack, name, shape, dt):
        self.uid = getattr(self, "uid", 0) + 1
        return stack.enter_context(self.nc.sbuf_tensor("s%d_%s" % (self.uid, name), list(shape), dt))

    def _op(self, eng, fn, waits):
        o = Op()
        o.eng = eng
        o.fn = fn
        o.waits = [w for w in waits if w is not None and w.phase == self.phase]
        for w in o.waits:
            if w.dsem is None:
                w.signal = True
        o.signal = False
        o.dsem = None
        o.count = 0
        o.dval = 0
        o.inc = 16
        o.phase = self.phase
        self.ops[eng].append(o)
        return o

    def op(self, eng, fn, reads=(), writes=(), extra=(), dsem=None):
        waits = list(extra)
        for bf in reads:
            if bf.w is not None:
                waits.append(bf.w)
        for bf in writes:
            if bf.w is not None:
                waits.append(bf.w)
            waits.extend(bf.r.values())
        o = self._op(eng, fn, waits)
        if dsem is not None:
            o.dsem = dsem
            self.dcount[dsem] = self.dcount.get(dsem, 0) + 16
            o.dval = self.dcount[dsem]
            self.lastd[dsem] = o
        key = eng if dsem is None else ("d", dsem)
        for bf in reads:
            bf.r[key] = o
        for bf in writes:
            bf.w = o
            bf.r = {}
        return o

    def dma(self, eng, out, in_, dsem, reads=(), writes=(), extra=(), **kw):
        return self.op(eng, lambda e: e.dma_start(out=out, in_=in_, **kw), reads, writes, extra, dsem=dsem)

    def coll(self, fn, dsem, reads=(), writes=()):
        o = self.op("pool", fn, reads, writes, dsem=dsem)
        self.dcount[dsem] += 1 - 16
        o.dval = self.dcount[dsem]
        o.inc = 1
        return o

    def end_phase(self, final_waits=()):
        nc = self.nc
        lasts = {}
        for e in ENGS:
            for o in reversed(self.ops[e]):
                if o.dsem is None and o.fn is not None:
                    lasts[e] = o
                    break
        dl = [o for o in self.lastd.values() if o.phase == self.phase]
        for e in ENGS:
            self._op(e, None, [lasts[x] for x in lasts if x != e] + dl)
        for e in ENGS:
            c = self.ecount[e]
            for o in self.ops[e]:
                if o.dsem is None and o.signal:
                    c += 1
                    o.count = c
            self.ecount[e] = c
        for k in self.dcount:
            if k not in self.dsem:
                self.dsem[k] = self.gstack.enter_context(nc.semaphore("ds_%s" % (k,)))
        ops, esem, dsem = self.ops, self.esem, self.dsem

        def run(engname):
            def f(eng):
                seen = self.seen[engname]
                for o in ops[engname]:
                    for w in o.waits:
                        if w.dsem is not None:
                            sem, val, key = dsem[w.dsem], w.dval, ("d", w.dsem)
                        else:
                            if w.eng == engname and engname == "pe":
                                continue
                            sem, val, key = esem[w.eng], w.count, ("e", w.eng)
                        if seen.get(key, 0) >= val:
                            continue
                        eng.wait_ge(sem, val)
                        seen[key] = val
                    if o.fn is None:
                        continue
                    ins = o.fn(eng)
                    if o.dsem is not None:
                        ins.then_inc(dsem[o.dsem], o.inc)
                    elif o.signal:
                        ins.then_inc(esem[engname], 1)
            return f

        with nc.Block() as block:
            block.tensor(run("pe"))
            block.scalar(run("act"))
            block.vector(run("dve"))
            block.gpsimd(run("pool"))
            block.sync(run("sp"))
        self.ops = {e: [] for e in ENGS}
        self.phase += 1

    def close(self):
        self.gstack.close()


def build_program(layers, do_final, debug=False):
    nc = bass.Bass("TRN2", target_bir_lowering=False)
    dt_in = lambda n, s, d=F32: nc.dram_tensor(n, list(s), d, kind="ExternalInput").ap()
    x_in = dt_in("x_in", [S, D])
    cT_d = dt_in("cT", [P, NCH])
    nwT_d = dt_in("nwT", [P, DEPTH, NCH])
    fnw_d = dt_in("fnw", [1, D])
    mod_w = {l: dt_in("mod_w%d" % l, [D, 3 * D]) for l in layers}
    mod_b = dt_in("mod_b", [DEPTH, 3 * D])
    w_in_d = {l: dt_in("w_in%d" % l, [D, (10 if l % 2 == 0 else 4) * (D // 2)]) for l in layers}
    w_out_d = {l: dt_in("w_out%d" % l, [D, D]) for l in layers}
    ident_d = dt_in("ident", [P, P])
    tabA_d = dt_in("tabA", [NHL, P, 12 * P])
    tabB_d = dt_in("tabB", [NHL, P, 16 * P])
    pastb_d = dt_in("pastb", [P, NT * 8])
    x_out = nc.dram_tensor("x_out", [S, D], F32, kind="ExternalOutput").ap()
    xs = nc.dram_tensor("xs", [S, D], F32).ap()
    dk = dict(kind="ExternalOutput") if debug else {}
    oT_d = nc.dram_tensor("oT_s", [NHL, P, S], BF16, **dk).ap()
    og_t = [nc.dram_tensor("og%d" % i, [2 * P, S], BF16) for i in range(NHL)]
    modv = nc.dram_tensor("modv", [DEPTH, 3 * D], F32, **dk).ap()
    if debug:
        hT_dbg = nc.dram_tensor("hT_dbg", [P, NCH, S], BF16, kind="ExternalOutput").ap()
        g_dbg = nc.dram_tensor("g_dbg", [P, NT * 8], F32, kind="ExternalOutput").ap()
        e_dbg = nc.dram_tensor("e_dbg", [P, NT * 8], F32, kind="ExternalOutput").ap()
        m_dbg = nc.dram_tensor("m_dbg", [P, NT], F32, kind="ExternalOutput").ap()

    b = Builder(nc)
    g = contextlib.ExitStack()
    psb = [g.enter_context(nc.psum_tensor("ps%d" % i, [P, 512], F32)) for i in range(8)]
    PS = [Buf("ps%d" % i) for i in range(8)]
    ident_f = b.sb(g, "ident_f", [P, P], F32)
    ident_b = b.sb(g, "ident_b", [P, P], BF16)
    ones_b = b.sb(g, "ones_b", [P, P], BF16)
    hT = b.sb(g, "hT", [P, NCH, S], BF16)
    B_hT = Buf("hT")
    condT = b.sb(g, "condT", [P, NCH], BF16)
    nwT = b.sb(g, "nwT", [P, DEPTH, NCH], F32)
    B_const = Buf("const")
    B_id, B_nw, B_cond, B_ones = Buf("id"), Buf("nw"), Buf("cond"), Buf("ones")

    MW = 256
    NMU = 3 * D // MW
    ps_in = [0]

    def mod_bufs(st):
        return dict(w=[b.sb(st, "wmod%d" % i, [P, NCH, MW], BF16) for i in range(2)],
                    Bw=[Buf("wmod%d" % i) for i in range(2)],
                    mb=[b.sb(st, "mbp%d" % i, [1, MW], F32) for i in range(2)],
                    Bmb=[Buf("mbp%d" % i) for i in range(2)],
                    row=[b.sb(st, "mrow%d" % i, [1, MW], F32) for i in range(2)],
                    Brow=[Buf("mrow%d" % i) for i in range(2)], k=[0])

    def mod_unit(m, cb, mbuf):
        k = mbuf["k"][0] % 2
        mbuf["k"][0] += 1
        wt, bw = mbuf["w"][k], mbuf["Bw"][k]
        cs = slice(cb * MW, (cb + 1) * MW)
        b.dma("pool", wt[:], mod_w[m][:, cs].rearrange("(c p) n -> p c n", p=P), "wmod%d" % k, writes=[bw])
        b.dma("sp", mbuf["mb"][k][:], mod_b[m:m + 1, cs], "mbp%d" % k, writes=[mbuf["Bmb"][k]])
        pi = ps_in[0] % 2
        ps_in[0] += 1
        for c in range(NCH):
            b.op("pe", lambda e, wt=wt, c=c, pi=pi: e.matmul(psb[pi][0:1, 0:MW], lhsT=condT[:, c:c + 1], rhs=wt[:, c, :],
                                                             start=(c == 0), stop=(c == NCH - 1)),
                 reads=[bw, B_cond], writes=[PS[pi]] if c == 0 else [])
        PS[pi].w = b.ops["pe"][-1]
        b.op("dve", lambda e, k=k, pi=pi: e.tensor_tensor(out=mbuf["row"][k][:], in0=psb[pi][0:1, 0:MW], in1=mbuf["mb"][k][:], op=ALU.add),
             reads=[PS[pi], mbuf["Bmb"][k]], writes=[mbuf["Brow"][k]])
        b.dma("sp", modv[m:m + 1, cs], mbuf["row"][k][:], "mvst%d" % k, reads=[mbuf["Brow"][k]])

    with contextlib.ExitStack() as st:
        cT_f = b.sb(st, "cT_f", [P, NCH], F32)
        B_cT = Buf("cT")
        b.dma("sp", ident_f[:], ident_d, "c0", writes=[B_id])
        b.dma("sp", nwT[:], nwT_d, "c1", writes=[B_nw])
        b.dma("sp", cT_f[:], cT_d, "c2", writes=[B_cT])
        b.op("dve", lambda e: e.tensor_copy(out=ident_b[:], in_=ident_f[:]), reads=[B_id], writes=[B_const])
        b.op("dve", lambda e: e.memset(ones_b[:], 1.0), writes=[B_ones])
        b.op("act", lambda e: e.activation(out=condT[:], in_=cT_f[:], func=AF.Silu), reads=[B_cT], writes=[B_cond])
        mb0 = mod_bufs(st)
        for cb in range(NMU):
            mod_unit(layers[0], cb, mb0)
        b.end_phase()

    def norm_phase(l, src, final):
        with contextlib.ExitStack() as st:
            xt = [b.sb(st, "xt%d" % i, [P, D], F32) for i in range(2)]
            B_xt = [Buf("xt%d" % i) for i in range(2)]
            xn = [b.sb(st, "xn%d" % i, [P, D], F32) for i in range(2)]
            B_xn = [Buf("xn%d" % i) for i in range(2)]
            junk = b.sb(st, "junk", [P, D], BF16)
            ss = b.sb(st, "ss", [P, 2], F32)
            B_ss = Buf("ss")
            B_vec, B_sh, B_sc = Buf("vec"), Buf("sh"), Buf("sc")
            if final:
                fw = b.sb(st, "fw", [P, D], F32)
                b.dma("sp", fw[:], fnw_d.partition_broadcast(P), "fw", writes=[B_vec])
            else:
                sc = b.sb(st, "sc", [P, NCH], F32)
                sh = b.sb(st, "sh", [P, NCH], F32)
                weff = b.sb(st, "weff", [P, NCH], F32)
                b.dma("sp", sh[:], modv[l, 0:D].rearrange("(c p) -> p c", p=P), "sh", writes=[B_sh], allow_slow_non_contiguous=True)
                b.dma("sp", sc[:], modv[l, D:2 * D].rearrange("(c p) -> p c", p=P), "sc", writes=[B_sc], allow_slow_non_contiguous=True)
                b.op("dve", lambda e: e.scalar_tensor_tensor(out=weff[:], in0=sc[:], scalar=1.0, in1=nwT[:, l, :],
                                                            op0=ALU.add, op1=ALU.mult), reads=[B_sc, B_nw], writes=[B_vec])
            for t in range(NT):
                i = t % 2
                b.dma("sp", xt[i][:], src[t * P:(t + 1) * P, :], "xt%d" % i, writes=[B_xt[i]])
                b.op("dve", lambda e, i=i: e.memset(ss[:, i:i + 1], 0.0), writes=[B_ss])
                b.op("act", lambda e, i=i: e.activation(out=junk[:], in_=xt[i][:], func=AF.Square, accum_out=ss[:, i:i + 1]),
                     reads=[B_xt[i]], writes=[B_ss])
                b.op("dve", lambda e, i=i: e.tensor_scalar(out=ss[:, i:i + 1], in0=ss[:, i:i + 1], scalar1=1.0 / D, scalar2=EPS,
                                                           op0=ALU.mult, op1=ALU.add), reads=[B_ss], writes=[B_ss])
                b.op("act", lambda e, i=i: e.sqrt(out=ss[:, i:i + 1], in_=ss[:, i:i + 1]), reads=[B_ss], writes=[B_ss])
                b.op("dve", lambda e, i=i: e.reciprocal(out=ss[:, i:i + 1], in_=ss[:, i:i + 1]), reads=[B_ss], writes=[B_ss])
                if final:
                    b.op("dve", lambda e, i=i: e.scalar_tensor_tensor(out=xn[i][:], in0=xt[i][:], scalar=ss[:, i:i + 1], in1=fw[:],
                                                                      op0=ALU.mult, op1=ALU.mult),
                         reads=[B_xt[i], B_ss, B_vec], writes=[B_xn[i]])
                    b.dma("pool", x_out[t * P:(t + 1) * P, :], xn[i][:], "xo%d" % i, reads=[B_xn[i]])
                else:
                    b.op("dve", lambda e, i=i: e.tensor_scalar(out=xn[i][:], in0=xt[i][:], scalar1=ss[:, i:i + 1], scalar2=None,
                                                               op0=ALU.mult), reads=[B_xt[i], B_ss], writes=[B_xn[i]])
                    for q4 in range(4):
                        pi = (t * 4 + q4) % 4
                        for j in range(4):
                            c = q4 * 4 + j
                            b.op("pe", lambda e, i=i, c=c, j=j, pi=pi: e.transpose(out=psb[pi][:, j * P:(j + 1) * P],
                                                                                   in_=xn[i][:, c * P:(c + 1) * P], identity=ident_f[:]),
                                 reads=[B_xn[i], B_id], writes=[PS[pi]] if j == 0 else [])
                        PS[pi].w = b.ops["pe"][-1]
                        for j in range(4):
                            c = q4 * 4 + j
                            b.op("act", lambda e, c=c, j=j, pi=pi, t=t: e.activation(
                                out=hT[:, c, t * P:(t + 1) * P], in_=psb[pi][:, j * P:(j + 1) * P], func=AF.Identity,
                                bias=sh[:, c:c + 1], scale=weff[:, c:c + 1]), reads=[PS[pi], B_vec, B_sh], writes=[B_hT])
            if debug and not final and l == layers[0]:
                b.dma("sp", hT_dbg, hT[:], "hdbg", reads=[B_hT])
            b.end_phase()

    def out_phase(l, w_out, src):
        with contextlib.ExitStack() as st:
            wo = [b.sb(st, "wo%d" % i, [P, NCH, 512], BF16) for i in range(2)]
            B_wo = [Buf("wo%d" % i) for i in range(2)]
            ot = [b.sb(st, "ot%d" % i, [P, NH, 512], BF16) for i in range(2)]
            B_ot = [Buf("ot%d" % i) for i in range(2)]
            xp = [b.sb(st, "xp%d" % i, [P, 512], F32) for i in range(3)]
            B_xp = [Buf("xp%d" % i) for i in range(3)]
            gate = b.sb(st, "gate", [P, D], F32)
            B_gate = Buf("gate")
            b.dma("sp", gate[:], modv[l:l + 1, 2 * D:3 * D].partition_broadcast(P), "gate", writes=[B_gate])
            k = 0
            n = 0
            for cb in range(4):
                b.dma("pool", wo[cb % 2][:], w_out[:, cb * 512:(cb + 1) * 512].rearrange("(c p) n -> p c n", p=P),
                      "wo%d" % (cb % 2), writes=[B_wo[cb % 2]])
                for tt in range(4):
                    oi = k % 2
                    for hl in range(NHL):
                        b.dma("sp", ot[oi][:].rearrange("p (r h) t -> p r h t", r=2)[:, :, hl, :],
                              og_t[hl].ap()[:, tt * 512:(tt + 1) * 512].rearrange("(r p) t -> p r t", p=P), "ot%d" % oi,
                              writes=[B_ot[oi]] if hl == 0 else [])
                    B_ot[oi].w = b.ops["sp"][-1]
                    k += 1
                    for s4 in range(4):
                        t = tt * 4 + s4
                        pi = n % 4
                        xi = n % 3
                        n += 1
                        b.dma("sp", xp[xi][:], src[t * P:(t + 1) * P, cb * 512:(cb + 1) * 512], "xp%d" % xi, writes=[B_xp[xi]])
                        for c in range(NH):
                            b.op("pe", lambda e, oi=oi, c=c, s4=s4, cb=cb, pi=pi: e.matmul(
                                psb[pi][:, :], lhsT=ot[oi][:, c, s4 * P:(s4 + 1) * P], rhs=wo[cb % 2][:, c, :],
                                start=(c == 0), stop=(c == NH - 1)),
                                reads=[B_ot[oi], B_wo[cb % 2]], writes=[PS[pi]] if c == 0 else [])
                        PS[pi].w = b.ops["pe"][-1]
                        tmp = xp[xi]
                        b.op("dve", lambda e, pi=pi, cb=cb, xi=xi: e.tensor_tensor(
                            out=psb[pi][:, :], in0=psb[pi][:, :], in1=gate[:, cb * 512:(cb + 1) * 512], op=ALU.mult),
                            reads=[B_gate], writes=[PS[pi]])
                        b.op("dve", lambda e, pi=pi, xi=xi: e.tensor_tensor(out=xp[xi][:], in0=psb[pi][:, :], in1=xp[xi][:], op=ALU.add),
                             reads=[PS[pi]], writes=[B_xp[xi]])
                        b.dma("act", xs[t * P:(t + 1) * P, cb * 512:(cb + 1) * 512], xp[xi][:], "xps%d" % xi, reads=[B_xp[xi]])
            b.end_phase()

    def mixer_phase(l, kind, w_in):
        ngrp = 3 if kind == "A" else 1
        dil = [1, 4, 16] if kind == "A" else [1]
        with contextlib.ExitStack() as st:
            NW = 6
            wr = [b.sb(st, "wr%d" % i, [P, NCH, P], BF16) for i in range(NW)]
            B_wr = [Buf("wr%d" % i) for i in range(NW)]
            QT = [b.sb(st, "QT%d" % i, [P, S], BF16) for i in range(2)]
            KT = [b.sb(st, "KT%d" % i, [P, S], BF16) for i in range(2)]
            V = [b.sb(st, "V%d" % i, [P, NT, P], BF16) for i in range(2)]
            B_Q = [Buf("Q%d" % i) for i in range(2)]
            B_K = [Buf("K%d" % i) for i in range(2)]
            B_V = [Buf("V%d" % i) for i in range(2)]
            SZ = [b.sb(st, "SZ%d" % i, [P, S], BF16) for i in range(2)]
            B_SZ = [Buf("SZ%d" % i) for i in range(2)]
            acc_o = b.sb(st, "acc_o", [P, S], F32)
            acc_s = b.sb(st, "acc_s", [P, S], F32)
            B_acc = Buf("acc")
            osb = [b.sb(st, "osb%d" % i, [P, S], BF16) for i in range(2)]
            B_osb = [Buf("osb%d" % i) for i in range(2)]
            tmp = [b.sb(st, "tmp%d" % i, [P, 512], F32) for i in range(2)]
            B_tmp = [Buf("tmp%d" % i) for i in range(2)]
            PT = [b.sb(st, "PT%d" % i, [P, 512], BF16) for i in range(3)]
            B_PT = [Buf("PT%d" % i) for i in range(3)]
            tw = 12 * P if kind == "A" else 16 * P
            tab = [b.sb(st, "tab%d" % i, [P, tw], F32) for i in range(2)]
            B_tab = [Buf("tab%d" % i) for i in range(2)]
            tab_d = tabA_d if kind == "A" else tabB_d
            if kind == "B":
                kmf = b.sb(st, "kmf", [P, 8], F32)
                kmb = b.sb(st, "kmb", [P, 8], BF16)
                pastb = b.sb(st, "pastb", [P, NT, 8], F32)
                gsb = b.sb(st, "gsb", [P, NT, 8], F32)
                g2 = b.sb(st, "g2", [P, NT, 8], F32)
                eq = b.sb(st, "eq", [P, NT, 8], F32)
                mx = b.sb(st, "mx", [P, NT], F32)
                nsel = [b.sb(st, "nsel%d" % i, [P, NT, 8], BF16) for i in range(2)]
                B_nsel = [Buf("nsel%d" % i) for i in range(2)]
                B_km, B_g = Buf("km"), Buf("g")
                B_km2, B_kj = Buf("km2"), Buf("kj")
                kjunk = b.sb(st, "kjunk", [P, 256], BF16)
                B_pb = Buf("pastb")
                b.dma("sp", pastb[:].rearrange("p a b -> p (a b)"), pastb_d, "pastb", writes=[B_pb])
            wk = [0]
            un = [0]
            nxt = layers[layers.index(l) + 1] if layers.index(l) + 1 < len(layers) else None
            mbm = mod_bufs(st) if nxt is not None else None
            mod_todo = list(range(NMU)) if nxt is not None else []

            def load_w(col0):
                i = wk[0] % NW
                wk[0] += 1
                b.dma("pool", wr[i][:], w_in[:, col0:col0 + P].rearrange("(c p) n -> p c n", p=P), "wr%d" % i, writes=[B_wr[i]])
                return i

            def proj_T(col0, dst, B_dst, d, func=None):
                wi = load_w(col0)
                for tt in range(4):
                    pi = ps_in[0] % 2
                    ps_in[0] += 1
                    for c in range(NCH):
                        b.op("pe", lambda e, wi=wi, c=c, tt=tt, pi=pi: e.matmul(
                            psb[pi][:, :], lhsT=wr[wi][:, c, :], rhs=hT[:, c, tt * 512:(tt + 1) * 512],
                            start=(c == 0), stop=(c == NCH - 1)),
                            reads=[B_wr[wi], B_hT], writes=[PS[pi]] if c == 0 else [])
                    PS[pi].w = b.ops["pe"][-1]
                    if d == 1:
                        o_ap = dst[:, tt * 512:(tt + 1) * 512]
                        i_ap = psb[pi][:, :]
                    else:
                        nl = 512 // d
                        o_ap = dst[:, :].rearrange("p (r l) -> p r l", r=d)[:, :, tt * nl:(tt + 1) * nl]
                        i_ap = psb[pi][:, :].rearrange("p (l r) -> p r l", r=d)
                    if func is None:
                        b.op("dve", lambda e, o_ap=o_ap, i_ap=i_ap: e.tensor_copy(out=o_ap, in_=i_ap), reads=[PS[pi]], writes=[B_dst])
                    else:
                        b.op("act", lambda e, o_ap=o_ap, i_ap=i_ap: e.activation(out=o_ap, in_=i_ap, func=func),
                             reads=[PS[pi]], writes=[B_dst])

            def proj_V(col0, dst, B_dst, d):
                wi = load_w(col0)
                L = S // d
                for j4 in range(4):
                    pi = ps_in[0] % 2
                    ps_in[0] += 1
                    for jj in range(4):
                        j = j4 * 4 + jj
                        r, l0 = (j * P) // L, (j * P) % L
                        for c in range(NCH):
                            if d == 1:
                                lh = hT[:, c, j * P:(j + 1) * P]
                            else:
                                lh = hT[:, c, :].rearrange("p (l r) -> p r l", r=d)[:, r, l0:l0 + P]
                            b.op("pe", lambda e, wi=wi, c=c, lh=lh, jj=jj, pi=pi: e.matmul(
                                psb[pi][:, jj * P:(jj + 1) * P], lhsT=lh, rhs=wr[wi][:, c, :],
                                start=(c == 0), stop=(c == NCH - 1)),
                                reads=[B_wr[wi], B_hT], writes=[PS[pi]] if (c == 0 and jj == 0) else [])
                    PS[pi].w = b.ops["pe"][-1]
                    b.op("act", lambda e, j4=j4, pi=pi: e.copy(out=dst[:, j4 * 4:(j4 + 1) * 4, :].rearrange("p a b -> p (a b)"),
                                                              in_=psb[pi][:, :]), reads=[PS[pi]], writes=[B_dst])

            def attention(h, gi, si, ti, units, first_group):
                qt, kt, v = QT[si], KT[si], V[si]
                flat = []
                for u in units:
                    firsts, lasts = {}, {}
                    n = 0
                    for stp in u["steps"]:
                        for (pos, ktile, sel) in stp["blocks"]:
                            firsts.setdefault(pos, n)
                            lasts[pos] = n
                            n += 1
                    uo = un[0]
                    un[0] += 1
                    n = 0
                    for sidx, stp in enumerate(u["steps"]):
                        flat.append((u, uo, sidx, stp, firsts, lasts, n))
                        n += len(stp["blocks"])

                def qk(item):
                    u, uo, sidx, stp, firsts, lasts, n0 = item
                    n = sn[0]
                    sn[0] += 1
                    sb_i, k2, k3 = 2 + n % 2, n % 2, n % 3
                    blocks = stp["blocks"]
                    nb = len(blocks)
                    for i, (pos, ktile, sel) in enumerate(blocks):
                        b.op("pe", lambda e, i=i, ktile=ktile, sb_i=sb_i, sel=sel, qc=u["qcols"][pos]: e.matmul(
                            psb[sb_i][:, i * P:(i + 1) * P], lhsT=kt[:, ktile * P:(ktile + 1) * P], rhs=qt[:, qc:qc + P],
                            start=True, stop=(sel is None)),
                            reads=[B_K[si], B_Q[si]], writes=[PS[sb_i]] if i == 0 else [])
                        if sel is not None:
                            b.op("pe", lambda e, i=i, sb_i=sb_i, sel=sel: e.matmul(
                                psb[sb_i][:, i * P:(i + 1) * P], lhsT=sel, rhs=ident_b[:], start=False, stop=True),
                                reads=[stp["selbuf"], B_const])
                    PS[sb_i].w = b.ops["pe"][-1]
                    tb = stp["tab"]
                    b.op("dve", lambda e, sb_i=sb_i, k2=k2, nb=nb, tb=tb: e.scalar_tensor_tensor(
                        out=tmp[k2][:, 0:nb * P].rearrange("p (a b) -> p a b", b=P),
                        in0=psb[sb_i][:, 0:nb * P].rearrange("p (a b) -> p a b", b=P), scalar=SCALE, in1=tb,
                        op0=ALU.mult, op1=ALU.add), reads=[PS[sb_i], B_tab[ti]], writes=[B_tmp[k2]])
                    b.op("act", lambda e, k2=k2, k3=k3, nb=nb: e.activation(
                        out=PT[k3][:, 0:nb * P], in_=tmp[k2][:, 0:nb * P], func=AF.Exp),
                        reads=[B_tmp[k2]], writes=[B_PT[k3]])
                    return k3

                def pv(item, k3):
                    u, uo, sidx, stp, firsts, lasts, n0 = item
                    po, pso = 4 + uo % 2, 6 + uo % 2
                    npos = len(u["qcols"])
                    for bank, lhs_fn, rd in ((po, lambda ktile: v[:, ktile, :], [B_V[si], B_PT[k3]]),
                                             (pso, lambda ktile: ones_b[:], [B_ones, B_PT[k3]])):
                        for i, (pos, ktile, sel) in enumerate(stp["blocks"]):
                            n = n0 + i
                            b.op("pe", lambda e, i=i, pos=pos, lh=lhs_fn(ktile), k3=k3, bank=bank, f=(firsts[pos] == n),
                                 la=(lasts[pos] == n): e.matmul(
                                psb[bank][:, pos * P:(pos + 1) * P], lhsT=lh, rhs=PT[k3][:, i * P:(i + 1) * P],
                                start=f, stop=la), reads=rd, writes=[PS[bank]] if (sidx == 0 and i == 0) else [])
                    if sidx != len(u["steps"]) - 1:
                        return
                    PS[po].w = b.ops["pe"][-1]
                    PS[pso].w = b.ops["pe"][-1]
                    ao, as_ = u["acc"](acc_o), u["acc"](acc_s)
                    shp = u["pshape"]
                    pin_o = psb[po][:, 0:npos * P]
                    pin_s = psb[pso][:, 0:npos * P]
                    if shp is not None:
                        pin_o = pin_o.rearrange("p (a b) -> p a b", b=shp)
                        pin_s = pin_s.rearrange("p (a b) -> p a b", b=shp)
                    if first_group:
                        b.op("dve", lambda e, ao=ao, pin_o=pin_o: e.tensor_copy(out=ao, in_=pin_o), reads=[PS[po]], writes=[B_acc])
                        b.op("dve", lambda e, as_=as_, pin_s=pin_s: e.tensor_copy(out=as_, in_=pin_s), reads=[PS[pso]], writes=[B_acc])
                    else:
                        b.op("dve", lambda e, ao=ao, pin_o=pin_o: e.tensor_tensor(out=ao, in0=pin_o, in1=ao, op=ALU.add),
                             reads=[PS[po]], writes=[B_acc])
                        b.op("dve", lambda e, as_=as_, pin_s=pin_s: e.tensor_tensor(out=as_, in0=pin_s, in1=as_, op=ALU.add),
                             reads=[PS[pso]], writes=[B_acc])

                prev = None
                for item in flat:
                    k3 = qk(item)
                    if prev is not None:
                        pv(*prev)
                    prev = (item, k3)
                pv(*prev)

            sn = [0]
            pv_cnt = [0]
            sidx_g = 0
            DH = D // 2
            pend_cc = None
            for h in range(NHL):
                ti = h % 2
                b.dma("sp", tab[ti][:], tab_d[h], "tab%d" % ti, writes=[B_tab[ti]])
                zi = h % 2
                zcol = (9 * DH if kind == "A" else 3 * DH) + h * P
                for gi in range(ngrp):
                    d = dil[gi]
                    L = S // d
                    si = sidx_g % 2
                    sidx_g += 1
                    base = gi * 3 * DH if kind == "A" else 0
                    proj_T(base + h * P, QT[si], B_Q[si], d)
                    proj_T(base + DH + h * P, KT[si], B_K[si], d)
                    proj_V(base + 2 * DH + h * P, V[si], B_V[si], d)
                    if pend_cc is not None:
                        pend_cc()
                        pend_cc = None
                    if gi == 0:
                        for _ in range((NMU + NHL - 1) // NHL):
                            if mod_todo:
                                mod_unit(nxt, mod_todo.pop(0), mbm)
                    if gi == 0:
                        proj_T(zcol, SZ[zi], B_SZ[zi], 1, func=AF.Silu)
                    units = []
                    if kind == "A":
                        tb0 = gi * 4 * P
                        for u4 in range(4):
                            js = [u4 * 4 + i for i in range(4)]
                            ns = [(j * P % L) // P for j in js]
                            steps = []
                            if gi == 2:
                                steps.append(dict(blocks=[(i, js[i], None) for i in range(4)],
                                                  tab=tab[ti][:, tb0:tb0 + 4 * P].rearrange("p (a b) -> p a b", b=P)))
                            else:
                                for half in range(2):
                                    blocks = []
                                    for i in (2 * half, 2 * half + 1):
                                        if ns[i] >= 1:
                                            blocks.append((i, js[i] - 1, None))
                                        blocks.append((i, js[i], None))
                                    off = 4 - len(blocks)
                                    steps.append(dict(blocks=blocks, tab=tab[ti][:, tb0 + off * P:tb0 + 4 * P].rearrange(
                                        "p (a b) -> p a b", b=P)))
                            if d == 1:
                                accf = lambda a, u4=u4: a[:, u4 * 512:(u4 + 1) * 512]
                                pshape = None
                            elif d == 4:
                                accf = lambda a, u4=u4: a[:, :].rearrange("p (l r) -> p r l", r=4)[:, u4, :]
                                pshape = None
                            else:
                                accf = lambda a, u4=u4: a[:, :].rearrange("p (l r) -> p r l", r=16)[:, u4 * 4:(u4 + 1) * 4, :]
                                pshape = P
                            units.append(dict(qcols=[j * P for j in js], steps=steps, acc=accf, pshape=pshape))
                    else:
                        ni = h % 2
                        b.op("dve", lambda e: e.memset(kmf[:], 0.0), writes=[B_km])
                        for n in range(8):
                            b.op("act", lambda e, si=si, n=n: e.activation(out=kjunk[:], in_=KT[si][:, n * 256:(n + 1) * 256], func=AF.Identity,
                                                                            scale=1.0 / 256, accum_out=kmf[:, n:n + 1]),
                                 reads=[B_K[si], B_km], writes=[B_kj] if n < 7 else [B_kj, B_km2])
                        b.op("dve", lambda e: e.tensor_copy(out=kmb[:], in_=kmf[:]), reads=[B_km2], writes=[B_km])
                        pi = ps_in[0] % 2
                        ps_in[0] += 1
                        for t in range(NT):
                            b.op("pe", lambda e, t=t, si=si, pi=pi: e.matmul(psb[pi][:, t * 8:(t + 1) * 8], lhsT=QT[si][:, t * P:(t + 1) * P],
                                                                            rhs=kmb[:], start=True, stop=True),
                                 reads=[B_Q[si], B_km], writes=[PS[pi]] if t == 0 else [])
                        PS[pi].w = b.ops["pe"][-1]
                        g3v = lambda a: a[:]
                        mxb = lambda: mx[:].unsqueeze(2).to_broadcast([P, NT, 8])
                        b.op("dve", lambda e, pi=pi: e.tensor_tensor(out=gsb[:], in0=psb[pi][:, 0:NT * 8].rearrange("p (a b) -> p a b", b=8),
                                                                     in1=pastb[:], op=ALU.add), reads=[PS[pi], B_pb], writes=[B_g])
                        b.op("dve", lambda e: e.tensor_copy(out=g2[:], in_=gsb[:]), reads=[B_g], writes=[B_g])
                        for rep in range(2):
                            b.op("dve", lambda e: e.tensor_reduce(out=mx[:], in_=g2[:], axis=AX.X, op=ALU.max), reads=[B_g], writes=[B_g])
                            b.op("dve", lambda e: e.tensor_tensor(out=eq[:], in0=g2[:], in1=mxb(), op=ALU.is_ge), reads=[B_g], writes=[B_g])
                            b.op("dve", lambda e: e.scalar_tensor_tensor(out=g2[:], in0=eq[:], scalar=-1e30, in1=g2[:],
                                                                        op0=ALU.mult, op1=ALU.add), reads=[B_g], writes=[B_g])
                        b.op("dve", lambda e: e.tensor_reduce(out=mx[:], in_=g2[:], axis=AX.X, op=ALU.max), reads=[B_g], writes=[B_g])
                        b.op("dve", lambda e: e.tensor_scalar(out=mx[:], in0=mx[:], scalar1=-1e29, scalar2=None, op0=ALU.max),
                             reads=[B_g], writes=[B_g])
                        b.op("dve", lambda e: e.tensor_tensor(out=eq[:], in0=gsb[:], in1=mxb(), op=ALU.is_ge), reads=[B_g], writes=[B_g])
                        if debug and h == 1 and l == layers[0]:
                            b.dma("sp", g_dbg, gsb[:].rearrange("p a b -> p (a b)"), "gdbg", reads=[B_g])
                            b.dma("sp", e_dbg, eq[:].rearrange("p a b -> p (a b)"), "edbg", reads=[B_g])
                            b.dma("sp", m_dbg, mx[:], "mdbg", reads=[B_g])
                        b.op("dve", lambda e, ni=ni: e.tensor_scalar(out=nsel[ni][:], in0=eq[:], scalar1=-1.0, scalar2=-NEG,
                                                                     op0=ALU.add, op1=ALU.mult), reads=[B_g], writes=[B_nsel[ni]])
                        for q4 in range(4):
                            qts = [q4 * 4 + i for i in range(4)]
                            steps = []
                            for pos in range(4):
                                qtile = qts[pos]
                                for k0_ in range(0, qtile + 1, 4):
                                    kts = list(range(k0_, min(k0_ + 4, qtile + 1)))
                                    blocks = []
                                    for ktile in kts:
                                        sel = None
                                        if qtile // 2 > ktile // 2:
                                            sel = nsel[ni][:, qtile, (ktile // 2):(ktile // 2) + 1].to_broadcast([P, P])
                                        blocks.append((pos, ktile, sel))
                                    r0 = 15 - qtile + kts[0]
                                    steps.append(dict(blocks=blocks, selbuf=B_nsel[ni],
                                                      tab=tab[ti][:, r0 * P:(r0 + len(kts)) * P].rearrange("p (a b) -> p a b", b=P)))
                            units.append(dict(qcols=[q * P for q in qts], steps=steps,
                                              acc=lambda a, q4=q4: a[:, q4 * 512:(q4 + 1) * 512], pshape=None))
                    attention(h, gi, si, ti, units, gi == 0)
                oi = h % 2
                b.op("dve", lambda e: e.reciprocal(out=acc_s[:], in_=acc_s[:]), reads=[B_acc], writes=[B_acc])
                b.op("dve", lambda e: e.tensor_tensor(out=acc_o[:], in0=acc_o[:], in1=acc_s[:], op=ALU.mult), reads=[B_acc], writes=[B_acc])
                b.op("dve", lambda e, oi=oi, zi=zi: e.tensor_tensor(out=osb[oi][:], in0=acc_o[:], in1=SZ[zi][:], op=ALU.mult),
                     reads=[B_acc, B_SZ[zi]], writes=[B_osb[oi]])
                st_op = b.dma("sp", oT_d[h], osb[oi][:], "ost%d" % oi, reads=[B_osb[oi]])

                def issue_cc(h=h, st_op=st_op):
                    o = b.coll(lambda e: e.collective_compute("AllGather", ALU.bypass, replica_groups=GROUPS,
                                                              ins=[oT_d[h].opt()], outs=[og_t[h].ap().opt()]), "cc")
                    o.waits.append(st_op)
                pend_cc = issue_cc
            pend_cc()
            b.end_phase()

    src = x_in
    for l in layers:
        norm_phase(l, src, False)
        mixer_phase(l, "A" if l % 2 == 0 else "B", w_in_d[l])
        out_phase(l, w_out_d[l], src)
        src = xs
    if do_final:
        norm_phase(0, src, True)
    else:
        with contextlib.ExitStack() as st:
            cp = [b.sb(st, "cp%d" % i, [P, D], F32) for i in range(2)]
            B_cp = [Buf("cp%d" % i) for i in range(2)]
            for t in range(NT):
                i = t % 2
                b.dma("sp", cp[i][:], src[t * P:(t + 1) * P, :], "cpl%d" % i, writes=[B_cp[i]])
                b.dma("sp", x_out[t * P:(t + 1) * P, :], cp[i][:], "cps%d" % i, reads=[B_cp[i]])
            b.end_phase()
    g.close()
    b.close()
    return nc


def _consts():
    slopes = np.exp2(-8.0 * np.arange(1, NH + 1, dtype=np.float64) / NH)
    k = np.arange(P)[:, None].astype(np.float64)
    q = np.arange(P)[None, :].astype(np.float64)
    tabA = np.zeros((NH, P, 12, P), np.float32)
    for h in range(NH):
        for gi, d in enumerate([1, 4, 16]):
            for blk in range(4):
                prev = (blk % 2 == 0) and gi < 2
                if not prev:
                    step = q - k
                    ok = step >= 0
                else:
                    step = q - k + P
                    ok = step <= P
                tabA[h, :, gi * 4 + blk, :] = np.where(ok, np.maximum(-slopes[h] * d * step, NEG), NEG)
    tabB = np.zeros((NH, P, 16, P), np.float32)
    for h in range(NH):
        for dl in range(16):
            dist = q - k + P * dl
            tabB[h, :, 15 - dl, :] = np.where(dist >= 0, np.maximum(-slopes[h] * dist, NEG), NEG)
    pastb = np.zeros((P, NT, 8), np.float32)
    for t in range(NT):
        for n in range(8):
            if n >= t // 2:
                pastb[:, t, n] = -1e30
    return (np.eye(P, dtype=np.float32), tabA.reshape(NH, P, 12 * P), tabB.reshape(NH, P, 16 * P), pastb.reshape(P, NT * 8))


_PROG_CACHE = {}


def _run(layers, do_final, xin, inputs, debug=False):
    key = (tuple(layers), do_final, debug)
    if key not in _PROG_CACHE:
        _PROG_CACHE[key] = build_program(list(layers), do_final, debug)
    nc = _PROG_CACHE[key]
    ident, tabA, tabB, pastb = _consts()
    c = np.asarray(inputs["c"], np.float32)
    nw = np.asarray(inputs["norm_w"], np.float32)
    nwT = np.ascontiguousarray(nw.reshape(DEPTH, NCH, P).transpose(2, 0, 1))
    fnw = np.asarray(inputs["final_norm_w"], np.float32).reshape(1, D)
    mod_b = np.ascontiguousarray(np.asarray(inputs["mod_b"], np.float32))
    per_half = [dict(), dict()]
    shared = {"mod_b": mod_b}
    for l in layers:
        shared["mod_w%d" % l] = np.ascontiguousarray(np.asarray(inputs["mod_w"][l], np.float32))
        pre = "a" if l % 2 == 0 else "b"
        w = np.asarray(inputs[pre + "_w_in"][l // 2], np.float32)
        nparts = w.shape[1] // D
        w4 = w.reshape(D, nparts, 2, D // 2)
        for hh in range(2):
            per_half[hh]["w_in%d" % l] = np.ascontiguousarray(w4[:, :, hh, :].reshape(D, nparts * (D // 2)))
        shared["w_out%d" % l] = np.ascontiguousarray(np.asarray(inputs[pre + "_w_out"][l // 2], np.float32))
    in_maps = []
    for core in range(N_CORES):
        bb, hh = core // 2, core % 2
        m = dict(shared)
        m.update(per_half[hh])
        m["x_in"] = np.ascontiguousarray(xin[bb])
        m["cT"] = np.ascontiguousarray(c[bb].reshape(NCH, P).T)
        m["nwT"] = nwT
        m["fnw"] = fnw
        m["ident"] = ident
        m["tabA"] = np.ascontiguousarray(tabA[hh * NHL:(hh + 1) * NHL])
        m["tabB"] = np.ascontiguousarray(tabB[hh * NHL:(hh + 1) * NHL])
        m["pastb"] = pastb
        in_maps.append(m)
    res = run_bass_kernel_spmd(nc, in_maps, core_ids=list(range(N_CORES)))
    if debug:
        _run.dbg = res.results
    return np.stack([np.asarray(res.results[2 * i]["x_out"], np.float32) for i in range(N_CORES // 2)], 0)


LAUNCH_PLAN = [([0, 1, 2, 3], True)]


def kernel(x, c, norm_w, mod_w, mod_b, a_w_in, a_w_out, b_w_in, b_w_out, final_norm_w):
    inputs = dict(c=c, norm_w=norm_w, mod_w=mod_w, mod_b=mod_b, a_w_in=a_w_in, a_w_out=a_w_out,
                  b_w_in=b_w_in, b_w_out=b_w_out, final_norm_w=final_norm_w)
    cur = np.asarray(x, np.float32)
    for layers, fin in LAUNCH_PLAN:
        cur = _run(layers, fin, cur, inputs)
    return cur
```

```python
import contextlib
import numpy as np
import ml_dtypes
import concourse.bass as bass
import concourse.mybir as mybir
from concourse.bass_utils import run_bass_kernel_spmd

F32 = mybir.dt.float32
BF16 = mybir.dt.bfloat16
AF = mybir.ActivationFunctionType
ALU = mybir.AluOpType
AX = mybir.AxisListType
NPBF = ml_dtypes.bfloat16

P = 128
D = 2048
S = 2048
NH = 16
DEPTH = 4
NCH = D // P
NT = S // P
EPS = 1e-6
SCALE = 128 ** -0.5
NEG = -30000.0
ENGS = ["pe", "act", "dve", "pool", "sp"]
N_CORES = 8
NHL = NH // 2
GROUPS = [[0, 1], [2, 3], [4, 5], [6, 7]]


class Op:
    __slots__ = ("eng", "fn", "waits", "signal", "count", "dsem", "dval", "inc", "phase")


class Buf:
    def __init__(self, name):
        self.name = name
        self.w = None
        self.r = {}


class Builder:
    def __init__(self, nc):
        self.nc = nc
        self.ops = {e: [] for e in ENGS}
        self.dcount = {}
        self.gstack = contextlib.ExitStack()
        self.esem = {e: self.gstack.enter_context(nc.semaphore("es_" + e)) for e in ENGS}
        self.dsem = {}
        self.ecount = {e: 0 for e in ENGS}
        self.seen = {e: {} for e in ENGS}
        self.phase = 0
        self.lastd = {}

    def sb(self, stack, name, shape, dt):
        self.uid = getattr(self, "uid", 0) + 1
        return stack.enter_context(self.nc.sbuf_tensor("s%d_%s" % (self.uid, name), list(shape), dt))

    def _op(self, eng, fn, waits):
        o = Op()
        o.eng = eng
        o.fn = fn
        o.waits = [w for w in waits if w is not None and w.phase == self.phase]
        for w in o.waits:
            if w.dsem is None:
                w.signal = True
        o.signal = False
        o.dsem = None
        o.count = 0
        o.dval = 0
        o.inc = 16
        o.phase = self.phase
        self.ops[eng].append(o)
        return o

    def op(self, eng, fn, reads=(), writes=(), extra=(), dsem=None):
        waits = list(extra)
        for bf in reads:
            if bf.w is not None:
                waits.append(bf.w)
        for bf in writes:
            if bf.w is not None:
                waits.append(bf.w)
            waits.extend(bf.r.values())
        o = self._op(eng, fn, waits)
        if dsem is not None:
            o.dsem = dsem
            self.dcount[dsem] = self.dcount.get(dsem, 0) + 16
            o.dval = self.dcount[dsem]
            self.lastd[dsem] = o
        key = eng if dsem is None else ("d", dsem)
        for bf in reads:
            bf.r[key] = o
        for bf in writes:
            bf.w = o
            bf.r = {}
        return o

    def dma(self, eng, out, in_, dsem, reads=(), writes=(), extra=(), **kw):
        return self.op(eng, lambda e: e.dma_start(out=out, in_=in_, **kw), reads, writes, extra, dsem=dsem)

    def coll(self, fn, dsem, reads=(), writes=()):
        o = self.op("pool", fn, reads, writes, dsem=dsem)
        self.dcount[dsem] += 1 - 16
        o.dval = self.dcount[dsem]
        o.inc = 1
        return o

    def end_phase(self, final_waits=()):
        nc = self.nc
        lasts = {}
        for e in ENGS:
            for o in reversed(self.ops[e]):
                if o.dsem is None and o.fn is not None:
                    lasts[e] = o
                    break
        dl = [o for o in self.lastd.values() if o.phase == self.phase]
        for e in ENGS:
            self._op(e, None, [lasts[x] for x in lasts if x != e] + dl)
        for e in ENGS:
            c = self.ecount[e]
            for o in self.ops[e]:
                if o.dsem is None and o.signal:
                    c += 1
                    o.count = c
            self.ecount[e] = c
        for k in self.dcount:
            if k not in self.dsem:
                self.dsem[k] = self.gstack.enter_context(nc.semaphore("ds_%s" % (k,)))
        ops, esem, dsem = self.ops, self.esem, self.dsem

        def run(engname):
            def f(eng):
                seen = self.seen[engname]
                for o in ops[engname]:
                    for w in o.waits:
                        if w.dsem is not None:
                            sem, val, key = dsem[w.dsem], w.dval, ("d", w.dsem)
                        else:
                            if w.eng == engname and engname == "pe":
                                continue
                            sem, val, key = esem[w.eng], w.count, ("e", w.eng)
                        if seen.get(key, 0) >= val:
                            continue
                        eng.wait_ge(sem, val)
                        seen[key] = val
                    if o.fn is None:
                        continue
                    ins = o.fn(eng)
                    if o.dsem is not None:
                        ins.then_inc(dsem[o.dsem], o.inc)
                    elif o.signal:
                        ins.then_inc(esem[engname], 1)
            return f

        with nc.Block() as block:
            block.tensor(run("pe"))
            block.scalar(run("act"))
            block.vector(run("dve"))
            block.gpsimd(run("pool"))
            block.sync(run("sp"))
        self.ops = {e: [] for e in ENGS}
        self.phase += 1

    def close(self):
        self.gstack.close()


def build_program(layers, do_final, debug=False):
    nc = bass.Bass("TRN2", target_bir_lowering=False)
    dt_in = lambda n, s, d=F32: nc.dram_tensor(n, list(s), d, kind="ExternalInput").ap()
    x_in = dt_in("x_in", [S, D])
    cT_d = dt_in("cT", [P, NCH])
    nwT_d = dt_in("nwT", [P, DEPTH, NCH])
    fnw_d = dt_in("fnw", [1, D])
    mod_w = {l: dt_in("mod_w%d" % l, [D, 3 * D]) for l in layers}
    mod_b = dt_in("mod_b", [DEPTH, 3 * D])
    w_in_d = {l: dt_in("w_in%d" % l, [D, (10 if l % 2 == 0 else 4) * (D // 2)]) for l in layers}
    w_out_d = {l: dt_in("w_out%d" % l, [D, D]) for l in layers}
    ident_d = dt_in("ident", [P, P])
    tabA_d = dt_in("tabA", [NHL, P, 12 * P])
    tabB_d = dt_in("tabB", [NHL, P, 16 * P])
    pastb_d = dt_in("pastb", [P, NT * 8])
    x_out = nc.dram_tensor("x_out", [S, D], F32, kind="ExternalOutput").ap()
    xs = nc.dram_tensor("xs", [S, D], F32).ap()
    dk = dict(kind="ExternalOutput") if debug else {}
    oT_d = nc.dram_tensor("oT_s", [NHL, P, S], BF16, **dk).ap()
    og_t = [nc.dram_tensor("og%d" % i, [2 * P, S], BF16) for i in range(NHL)]
    modv = nc.dram_tensor("modv", [DEPTH, 3 * D], F32, **dk).ap()
    if debug:
        hT_dbg = nc.dram_tensor("hT_dbg", [P, NCH, S], BF16, kind="ExternalOutput").ap()
        g_dbg = nc.dram_tensor("g_dbg", [P, NT * 8], F32, kind="ExternalOutput").ap()
        e_dbg = nc.dram_tensor("e_dbg", [P, NT * 8], F32, kind="ExternalOutput").ap()
        m_dbg = nc.dram_tensor("m_dbg", [P, NT], F32, kind="ExternalOutput").ap()

    b = Builder(nc)
    g = contextlib.ExitStack()
    psb = [g.enter_context(nc.psum_tensor("ps%d" % i, [P, 512], F32)) for i in range(8)]
    PS = [Buf("ps%d" % i) for i in range(8)]
    ident_f = b.sb(g, "ident_f", [P, P], F32)
    ident_b = b.sb(g, "ident_b", [P, P], BF16)
    ones_b = b.sb(g, "ones_b", [P, P], BF16)
    hT = b.sb(g, "hT", [P, NCH, S], BF16)
    B_hT = Buf("hT")
    condT = b.sb(g, "condT", [P, NCH], BF16)
    nwT = b.sb(g, "nwT", [P, DEPTH, NCH], F32)
    B_const = Buf("const")
    B_id, B_nw, B_cond, B_ones = Buf("id"), Buf("nw"), Buf("cond"), Buf("ones")

    MW = 256
    NMU = 3 * D // MW
    ps_in = [0]

    def mod_bufs(st):
        return dict(w=[b.sb(st, "wmod%d" % i, [P, NCH, MW], BF16) for i in range(2)],
                    Bw=[Buf("wmod%d" % i) for i in range(2)],
                    mb=[b.sb(st, "mbp%d" % i, [1, MW], F32) for i in range(2)],
                    Bmb=[Buf("mbp%d" % i) for i in range(2)],
                    row=[b.sb(st, "mrow%d" % i, [1, MW], F32) for i in range(2)],
                    Brow=[Buf("mrow%d" % i) for i in range(2)], k=[0])

    def mod_unit(m, cb, mbuf):
        k = mbuf["k"][0] % 2
        mbuf["k"][0] += 1
        wt, bw = mbuf["w"][k], mbuf["Bw"][k]
        cs = slice(cb * MW, (cb + 1) * MW)
        b.dma("pool", wt[:], mod_w[m][:, cs].rearrange("(c p) n -> p c n", p=P), "wmod%d" % k, writes=[bw])
        b.dma("sp", mbuf["mb"][k][:], mod_b[m:m + 1, cs], "mbp%d" % k, writes=[mbuf["Bmb"][k]])
        pi = ps_in[0] % 2
        ps_in[0] += 1
        for c in range(NCH):
            b.op("pe", lambda e, wt=wt, c=c, pi=pi: e.matmul(psb[pi][0:1, 0:MW], lhsT=condT[:, c:c + 1], rhs=wt[:, c, :],
                                                             start=(c == 0), stop=(c == NCH - 1)),
                 reads=[bw, B_cond], writes=[PS[pi]] if c == 0 else [])
        PS[pi].w = b.ops["pe"][-1]
        b.op("dve", lambda e, k=k, pi=pi: e.tensor_tensor(out=mbuf["row"][k][:], in0=psb[pi][0:1, 0:MW], in1=mbuf["mb"][k][:], op=ALU.add),
             reads=[PS[pi], mbuf["Bmb"][k]], writes=[mbuf["Brow"][k]])
        b.dma("sp", modv[m:m + 1, cs], mbuf["row"][k][:], "mvst%d" % k, reads=[mbuf["Brow"][k]])

    with contextlib.ExitStack() as st:
        cT_f = b.sb(st, "cT_f", [P, NCH], F32)
        B_cT = Buf("cT")
        b.dma("sp", ident_f[:], ident_d, "c0", writes=[B_id])
        b.dma("sp", nwT[:], nwT_d, "c1", writes=[B_nw])
        b.dma("sp", cT_f[:], cT_d, "c2", writes=[B_cT])
        b.op("dve", lambda e: e.tensor_copy(out=ident_b[:], in_=ident_f[:]), reads=[B_id], writes=[B_const])
        b.op("dve", lambda e: e.memset(ones_b[:], 1.0), writes=[B_ones])
        b.op("act", lambda e: e.activation(out=condT[:], in_=cT_f[:], func=AF.Silu), reads=[B_cT], writes=[B_cond])
        mb0 = mod_bufs(st)
        for cb in range(NMU):
            mod_unit(layers[0], cb, mb0)
        b.end_phase()

    def norm_phase(l, src, final):
        with contextlib.ExitStack() as st:
            xt = [b.sb(st, "xt%d" % i, [P, D], F32) for i in range(2)]
            B_xt = [Buf("xt%d" % i) for i in range(2)]
            xn = [b.sb(st, "xn%d" % i, [P, D], F32) for i in range(2)]
            B_xn = [Buf("xn%d" % i) for i in range(2)]
            junk = b.sb(st, "junk", [P, D], BF16)
            ss = b.sb(st, "ss", [P, 2], F32)
            B_ss = Buf("ss")
            B_vec, B_sh, B_sc = Buf("vec"), Buf("sh"), Buf("sc")
            if final:
                fw = b.sb(st, "fw", [P, D], F32)
                b.dma("sp", fw[:], fnw_d.partition_broadcast(P), "fw", writes=[B_vec])
            else:
                sc = b.sb(st, "sc", [P, NCH], F32)
                sh = b.sb(st, "sh", [P, NCH], F32)
                weff = b.sb(st, "weff", [P, NCH], F32)
                b.dma("sp", sh[:], modv[l, 0:D].rearrange("(c p) -> p c", p=P), "sh", writes=[B_sh], allow_slow_non_contiguous=True)
                b.dma("sp", sc[:], modv[l, D:2 * D].rearrange("(c p) -> p c", p=P), "sc", writes=[B_sc], allow_slow_non_contiguous=True)
                b.op("dve", lambda e: e.scalar_tensor_tensor(out=weff[:], in0=sc[:], scalar=1.0, in1=nwT[:, l, :],
                                                            op0=ALU.add, op1=ALU.mult), reads=[B_sc, B_nw], writes=[B_vec])
            for t in range(NT):
                i = t % 2
                b.dma("sp", xt[i][:], src[t * P:(t + 1) * P, :], "xt%d" % i, writes=[B_xt[i]])
                b.op("dve", lambda e, i=i: e.memset(ss[:, i:i + 1], 0.0), writes=[B_ss])
                b.op("act", lambda e, i=i: e.activation(out=junk[:], in_=xt[i][:], func=AF.Square, accum_out=ss[:, i:i + 1]),
                     reads=[B_xt[i]], writes=[B_ss])
                b.op("dve", lambda e, i=i: e.tensor_scalar(out=ss[:, i:i + 1], in0=ss[:, i:i + 1], scalar1=1.0 / D, scalar2=EPS,
                                                           op0=ALU.mult, op1=ALU.add), reads=[B_ss], writes=[B_ss])
                b.op("act", lambda e, i=i: e.sqrt(out=ss[:, i:i + 1], in_=ss[:, i:i + 1]), reads=[B_ss], writes=[B_ss])
                b.op("dve", lambda e, i=i: e.reciprocal(out=ss[:, i:i + 1], in_=ss[:, i:i + 1]), reads=[B_ss], writes=[B_ss])
                if final:
                    b.op("dve", lambda e, i=i: e.scalar_tensor_tensor(out=xn[i][:], in0=xt[i][:], scalar=ss[:, i:i + 1], in1=fw[:],
                                                                      op0=ALU.mult, op1=ALU.mult),
                         reads=[B_xt[i], B_ss, B_vec], writes=[B_xn[i]])
                    b.dma("pool", x_out[t * P:(t + 1) * P, :], xn[i][:], "xo%d" % i, reads=[B_xn[i]])
                else:
                    b.op("dve", lambda e, i=i: e.tensor_scalar(out=xn[i][:], in0=xt[i][:], scalar1=ss[:, i:i + 1], scalar2=None,
                                                               op0=ALU.mult), reads=[B_xt[i], B_ss], writes=[B_xn[i]])
                    for q4 in range(4):
                        pi = (t * 4 + q4) % 4
                        for j in range(4):
                            c = q4 * 4 + j
                            b.op("pe", lambda e, i=i, c=c, j=j, pi=pi: e.transpose(out=psb[pi][:, j * P:(j + 1) * P],
                                                                                   in_=xn[i][:, c * P:(c + 1) * P], identity=ident_f[:]),
                                 reads=[B_xn[i], B_id], writes=[PS[pi]] if j == 0 else [])
                        PS[pi].w = b.ops["pe"][-1]
                        for j in range(4):
                            c = q4 * 4 + j
                            b.op("act", lambda e, c=c, j=j, pi=pi, t=t: e.activation(
                                out=hT[:, c, t * P:(t + 1) * P], in_=psb[pi][:, j * P:(j + 1) * P], func=AF.Identity,
                                bias=sh[:, c:c + 1], scale=weff[:, c:c + 1]), reads=[PS[pi], B_vec, B_sh], writes=[B_hT])
            if debug and not final and l == layers[0]:
                b.dma("sp", hT_dbg, hT[:], "hdbg", reads=[B_hT])
            b.end_phase()

    def out_phase(l, w_out, src):
        with contextlib.ExitStack() as st:
            wo = [b.sb(st, "wo%d" % i, [P, NCH, 512], BF16) for i in range(2)]
            B_wo = [Buf("wo%d" % i) for i in range(2)]
            ot = [b.sb(st, "ot%d" % i, [P, NH, 512], BF16) for i in range(2)]
            B_ot = [Buf("ot%d" % i) for i in range(2)]
            xp = [b.sb(st, "xp%d" % i, [P, 512], F32) for i in range(3)]
            B_xp = [Buf("xp%d" % i) for i in range(3)]
            gate = b.sb(st, "gate", [P, D], F32)
            B_gate = Buf("gate")
            b.dma("sp", gate[:], modv[l:l + 1, 2 * D:3 * D].partition_broadcast(P), "gate", writes=[B_gate])
            k = 0
            n = 0
            for cb in range(4):
                b.dma("pool", wo[cb % 2][:], w_out[:, cb * 512:(cb + 1) * 512].rearrange("(c p) n -> p c n", p=P),
                      "wo%d" % (cb % 2), writes=[B_wo[cb % 2]])
                for tt in range(4):
                    oi = k % 2
                    for hl in range(NHL):
                        b.dma("sp", ot[oi][:].rearrange("p (r h) t -> p r h t", r=2)[:, :, hl, :],
                              og_t[hl].ap()[:, tt * 512:(tt + 1) * 512].rearrange("(r p) t -> p r t", p=P), "ot%d" % oi,
                              writes=[B_ot[oi]] if hl == 0 else [])
                    B_ot[oi].w = b.ops["sp"][-1]
                    k += 1
                    for s4 in range(4):
                        t = tt * 4 + s4
                        pi = n % 4
                        xi = n % 3
                        n += 1
                        b.dma("sp", xp[xi][:], src[t * P:(t + 1) * P, cb * 512:(cb + 1) * 512], "xp%d" % xi, writes=[B_xp[xi]])
                        for c in range(NH):
                            b.op("pe", lambda e, oi=oi, c=c, s4=s4, cb=cb, pi=pi: e.matmul(
                                psb[pi][:, :], lhsT=ot[oi][:, c, s4 * P:(s4 + 1) * P], rhs=wo[cb % 2][:, c, :],
                                start=(c == 0), stop=(c == NH - 1)),
                                reads=[B_ot[oi], B_wo[cb % 2]], writes=[PS[pi]] if c == 0 else [])
                        PS[pi].w = b.ops["pe"][-1]
                        tmp = xp[xi]
                        b.op("dve", lambda e, pi=pi, cb=cb, xi=xi: e.tensor_tensor(
                            out=psb[pi][:, :], in0=psb[pi][:, :], in1=gate[:, cb * 512:(cb + 1) * 512], op=ALU.mult),
                            reads=[B_gate], writes=[PS[pi]])
                        b.op("dve", lambda e, pi=pi, xi=xi: e.tensor_tensor(out=xp[xi][:], in0=psb[pi][:, :], in1=xp[xi][:], op=ALU.add),
                             reads=[PS[pi]], writes=[B_xp[xi]])
                        b.dma("act", xs[t * P:(t + 1) * P, cb * 512:(cb + 1) * 512], xp[xi][:], "xps%d" % xi, reads=[B_xp[xi]])
            b.end_phase()

    def mixer_phase(l, kind, w_in):
        ngrp = 3 if kind == "A" else 1
        dil = [1, 4, 16] if kind == "A" else [1]
        with contextlib.ExitStack() as st:
            NW = 6
            wr = [b.sb(st, "wr%d" % i, [P, NCH, P], BF16) for i in range(NW)]
            B_wr = [Buf("wr%d" % i) for i in range(NW)]
            QT = [b.sb(st, "QT%d" % i, [P, S], BF16) for i in range(2)]
            KT = [b.sb(st, "KT%d" % i, [P, S], BF16) for i in range(2)]
            V = [b.sb(st, "V%d" % i, [P, NT, P], BF16) for i in range(2)]
            B_Q = [Buf("Q%d" % i) for i in range(2)]
            B_K = [Buf("K%d" % i) for i in range(2)]
            B_V = [Buf("V%d" % i) for i in range(2)]
            SZ = [b.sb(st, "SZ%d" % i, [P, S], BF16) for i in range(2)]
            B_SZ = [Buf("SZ%d" % i) for i in range(2)]
            acc_o = b.sb(st, "acc_o", [P, S], F32)
            acc_s = b.sb(st, "acc_s", [P, S], F32)
            B_acc = Buf("acc")
            osb = [b.sb(st, "osb%d" % i, [P, S], BF16) for i in range(2)]
            B_osb = [Buf("osb%d" % i) for i in range(2)]
            tmp = [b.sb(st, "tmp%d" % i, [P, 512], F32) for i in range(2)]
            B_tmp = [Buf("tmp%d" % i) for i in range(2)]
            PT = [b.sb(st, "PT%d" % i, [P, 512], BF16) for i in range(3)]
            B_PT = [Buf("PT%d" % i) for i in range(3)]
            tw = 12 * P if kind == "A" else 16 * P
            tab = [b.sb(st, "tab%d" % i, [P, tw], F32) for i in range(2)]
            B_tab = [Buf("tab%d" % i) for i in range(2)]
            tab_d = tabA_d if kind == "A" else tabB_d
            if kind == "B":
                kmf = b.sb(st, "kmf", [P, 8], F32)
                kmb = b.sb(st, "kmb", [P, 8], BF16)
                pastb = b.sb(st, "pastb", [P, NT, 8], F32)
                gsb = b.sb(st, "gsb", [P, NT, 8], F32)
                g2 = b.sb(st, "g2", [P, NT, 8], F32)
                eq = b.sb(st, "eq", [P, NT, 8], F32)
                mx = b.sb(st, "mx", [P, NT], F32)
                nsel = [b.sb(st, "nsel%d" % i, [P, NT, 8], BF16) for i in range(2)]
                B_nsel = [Buf("nsel%d" % i) for i in range(2)]
                B_km, B_g = Buf("km"), Buf("g")
                B_km2, B_kj = Buf("km2"), Buf("kj")
                kjunk = b.sb(st, "kjunk", [P, 256], BF16)
                B_pb = Buf("pastb")
                b.dma("sp", pastb[:].rearrange("p a b -> p (a b)"), pastb_d, "pastb", writes=[B_pb])
            wk = [0]
            un = [0]
            nxt = layers[layers.index(l) + 1] if layers.index(l) + 1 < len(layers) else None
            mbm = mod_bufs(st) if nxt is not None else None
            mod_todo = list(range(NMU)) if nxt is not None else []

            def load_w(col0):
                i = wk[0] % NW
                wk[0] += 1
                b.dma("pool", wr[i][:], w_in[:, col0:col0 + P].rearrange("(c p) n -> p c n", p=P), "wr%d" % i, writes=[B_wr[i]])
                return i

            def proj_T(col0, dst, B_dst, d, func=None):
                wi = load_w(col0)
                for tt in range(4):
                    pi = ps_in[0] % 2
                    ps_in[0] += 1
                    for c in range(NCH):
                        b.op("pe", lambda e, wi=wi, c=c, tt=tt, pi=pi: e.matmul(
                            psb[pi][:, :], lhsT=wr[wi][:, c, :], rhs=hT[:, c, tt * 512:(tt + 1) * 512],
                            start=(c == 0), stop=(c == NCH - 1)),
                            reads=[B_wr[wi], B_hT], writes=[PS[pi]] if c == 0 else [])
                    PS[pi].w = b.ops["pe"][-1]
                    if d == 1:
                        o_ap = dst[:, tt * 512:(tt + 1) * 512]
                        i_ap = psb[pi][:, :]
                    else:
                        nl = 512 // d
                        o_ap = dst[:, :].rearrange("p (r l) -> p r l", r=d)[:, :, tt * nl:(tt + 1) * nl]
                        i_ap = psb[pi][:, :].rearrange("p (l r) -> p r l", r=d)
                    if func is None:
                        b.op("dve", lambda e, o_ap=o_ap, i_ap=i_ap: e.tensor_copy(out=o_ap, in_=i_ap), reads=[PS[pi]], writes=[B_dst])
                    else:
                        b.op("act", lambda e, o_ap=o_ap, i_ap=i_ap: e.activation(out=o_ap, in_=i_ap, func=func),
                             reads=[PS[pi]], writes=[B_dst])
                    yield

            def proj_V(col0, dst, B_dst, d):
                wi = load_w(col0)
                L = S // d
                for j4 in range(4):
                    pi = ps_in[0] % 2
                    ps_in[0] += 1
                    for jj in range(4):
                        j = j4 * 4 + jj
                        r, l0 = (j * P) // L, (j * P) % L
                        for c in range(NCH):
                            if d == 1:
                                lh = hT[:, c, j * P:(j + 1) * P]
                            else:
                                lh = hT[:, c, :].rearrange("p (l r) -> p r l", r=d)[:, r, l0:l0 + P]
                            b.op("pe", lambda e, wi=wi, c=c, lh=lh, jj=jj, pi=pi: e.matmul(
                                psb[pi][:, jj * P:(jj + 1) * P], lhsT=lh, rhs=wr[wi][:, c, :],
                                start=(c == 0), stop=(c == NCH - 1)),
                                reads=[B_wr[wi], B_hT], writes=[PS[pi]] if (c == 0 and jj == 0) else [])
                    PS[pi].w = b.ops["pe"][-1]
                    b.op("act", lambda e, j4=j4, pi=pi: e.copy(out=dst[:, j4 * 4:(j4 + 1) * 4, :].rearrange("p a b -> p (a b)"),
                                                              in_=psb[pi][:, :]), reads=[PS[pi]], writes=[B_dst])
                    yield

            def attention(h, gi, si, ti, units, first_group):
                qt, kt, v = QT[si], KT[si], V[si]
                flat = []
                for u in units:
                    firsts, lasts = {}, {}
                    n = 0
                    for stp in u["steps"]:
                        for (pos, ktile, sel) in stp["blocks"]:
                            firsts.setdefault(pos, n)
                            lasts[pos] = n
                            n += 1
                    uo = un[0]
                    un[0] += 1
                    n = 0
                    for sidx, stp in enumerate(u["steps"]):
                        flat.append((u, uo, sidx, stp, firsts, lasts, n))
                        n += len(stp["blocks"])

                def qk(item):
                    u, uo, sidx, stp, firsts, lasts, n0 = item
                    n = sn[0]
                    sn[0] += 1
                    sb_i, k2, k3 = 2 + n % 2, n % 2, n % 3
                    blocks = stp["blocks"]
                    nb = len(blocks)
                    for i, (pos, ktile, sel) in enumerate(blocks):
                        b.op("pe", lambda e, i=i, ktile=ktile, sb_i=sb_i, sel=sel, qc=u["qcols"][pos]: e.matmul(
                            psb[sb_i][:, i * P:(i + 1) * P], lhsT=kt[:, ktile * P:(ktile + 1) * P], rhs=qt[:, qc:qc + P],
                            start=True, stop=(sel is None)),
                            reads=[B_K[si], B_Q[si]], writes=[PS[sb_i]] if i == 0 else [])
                        if sel is not None:
                            b.op("pe", lambda e, i=i, sb_i=sb_i, sel=sel: e.matmul(
                                psb[sb_i][:, i * P:(i + 1) * P], lhsT=sel, rhs=ident_b[:], start=False, stop=True),
                                reads=[stp["selbuf"], B_const])
                    PS[sb_i].w = b.ops["pe"][-1]
                    tb = stp["tab"]
                    b.op("dve", lambda e, sb_i=sb_i, k2=k2, nb=nb, tb=tb: e.scalar_tensor_tensor(
                        out=tmp[k2][:, 0:nb * P].rearrange("p (a b) -> p a b", b=P),
                        in0=psb[sb_i][:, 0:nb * P].rearrange("p (a b) -> p a b", b=P), scalar=SCALE, in1=tb,
                        op0=ALU.mult, op1=ALU.add), reads=[PS[sb_i], B_tab[ti]], writes=[B_tmp[k2]])
                    b.op("act", lambda e, k2=k2, k3=k3, nb=nb: e.activation(
                        out=PT[k3][:, 0:nb * P], in_=tmp[k2][:, 0:nb * P], func=AF.Exp),
                        reads=[B_tmp[k2]], writes=[B_PT[k3]])
                    return k3

                def pv(item, k3):
                    u, uo, sidx, stp, firsts, lasts, n0 = item
                    po, pso = 4 + uo % 2, 6 + uo % 2
                    npos = len(u["qcols"])
                    for bank, lhs_fn, rd in ((po, lambda ktile: v[:, ktile, :], [B_V[si], B_PT[k3]]),
                                             (pso, lambda ktile: ones_b[:], [B_ones, B_PT[k3]])):
                        for i, (pos, ktile, sel) in enumerate(stp["blocks"]):
                            n = n0 + i
                            b.op("pe", lambda e, i=i, pos=pos, lh=lhs_fn(ktile), k3=k3, bank=bank, f=(firsts[pos] == n),
                                 la=(lasts[pos] == n): e.matmul(
                                psb[bank][:, pos * P:(pos + 1) * P], lhsT=lh, rhs=PT[k3][:, i * P:(i + 1) * P],
                                start=f, stop=la), reads=rd, writes=[PS[bank]] if (sidx == 0 and i == 0) else [])
                    if sidx != len(u["steps"]) - 1:
                        return
                    PS[po].w = b.ops["pe"][-1]
                    PS[pso].w = b.ops["pe"][-1]
                    ao, as_ = u["acc"](acc_o), u["acc"](acc_s)
                    shp = u["pshape"]
                    pin_o = psb[po][:, 0:npos * P]
                    pin_s = psb[pso][:, 0:npos * P]
                    if shp is not None:
                        pin_o = pin_o.rearrange("p (a b) -> p a b", b=shp)
                        pin_s = pin_s.rearrange("p (a b) -> p a b", b=shp)
                    if first_group:
                        b.op("dve", lambda e, ao=ao, pin_o=pin_o: e.tensor_copy(out=ao, in_=pin_o), reads=[PS[po]], writes=[B_acc])
                        b.op("dve", lambda e, as_=as_, pin_s=pin_s: e.tensor_copy(out=as_, in_=pin_s), reads=[PS[pso]], writes=[B_acc])
                    else:
                        b.op("dve", lambda e, ao=ao, pin_o=pin_o: e.tensor_tensor(out=ao, in0=pin_o, in1=ao, op=ALU.add),
                             reads=[PS[po]], writes=[B_acc])
                        b.op("dve", lambda e, as_=as_, pin_s=pin_s: e.tensor_tensor(out=as_, in0=pin_s, in1=as_, op=ALU.add),
                             reads=[PS[pso]], writes=[B_acc])

                prev = None
                for item in flat:
                    k3 = qk(item)
                    if prev is not None:
                        pv(*prev)
                    prev = (item, k3)
                    yield
                pv(*prev)

            sn = [0]
            pv_cnt = [0]
            DH = D // 2
            pairs = [(h, gi) for h in range(NHL) for gi in range(ngrp)]
            pair_units = {}

            def P_gen(idx):
                h, gi = pairs[idx]
                ti, zi, si = h % 2, h % 2, idx % 2
                if True:
                    d = dil[gi]
                    L = S // d
                    base = gi * 3 * DH if kind == "A" else 0
                    zcol = (9 * DH if kind == "A" else 3 * DH) + h * P
                    if gi == 0:
                        b.dma("sp", tab[ti][:], tab_d[h], "tab%d" % ti, writes=[B_tab[ti]])
                    yield from proj_T(base + h * P, QT[si], B_Q[si], d)
                    yield from proj_T(base + DH + h * P, KT[si], B_K[si], d)
                    yield from proj_V(base + 2 * DH + h * P, V[si], B_V[si], d)
                    if gi == 0:
                        for _ in range((NMU + NHL - 1) // NHL):
                            if mod_todo:
                                mod_unit(nxt, mod_todo.pop(0), mbm)
                                yield
                        yield from proj_T(zcol, SZ[zi], B_SZ[zi], 1, func=AF.Silu)
                    units = []
                    if kind == "A":
                        tb0 = gi * 4 * P
                        for u4 in range(4):
                            js = [u4 * 4 + i for i in range(4)]
                            ns = [(j * P % L) // P for j in js]
                            steps = []
                            if gi == 2:
                                steps.append(dict(blocks=[(i, js[i], None) for i in range(4)],
                                                  tab=tab[ti][:, tb0:tb0 + 4 * P].rearrange("p (a b) -> p a b", b=P)))
                            else:
                                for half in range(2):
                                    blocks = []
                                    for i in (2 * half, 2 * half + 1):
                                        if ns[i] >= 1:
                                            blocks.append((i, js[i] - 1, None))
                                        blocks.append((i, js[i], None))
                                    off = 4 - len(blocks)
                                    steps.append(dict(blocks=blocks, tab=tab[ti][:, tb0 + off * P:tb0 + 4 * P].rearrange(
                                        "p (a b) -> p a b", b=P)))
                            if d == 1:
                                accf = lambda a, u4=u4: a[:, u4 * 512:(u4 + 1) * 512]
                                pshape = None
                            elif d == 4:
                                accf = lambda a, u4=u4: a[:, :].rearrange("p (l r) -> p r l", r=4)[:, u4, :]
                                pshape = None
                            else:
                                accf = lambda a, u4=u4: a[:, :].rearrange("p (l r) -> p r l", r=16)[:, u4 * 4:(u4 + 1) * 4, :]
                                pshape = P
                            units.append(dict(qcols=[j * P for j in js], steps=steps, acc=accf, pshape=pshape))
                    else:
                        ni = h % 2
                        b.op("dve", lambda e: e.memset(kmf[:], 0.0), writes=[B_km])
                        for n in range(8):
                            b.op("act", lambda e, si=si, n=n: e.activation(out=kjunk[:], in_=KT[si][:, n * 256:(n + 1) * 256], func=AF.Identity,
                                                                            scale=1.0 / 256, accum_out=kmf[:, n:n + 1]),
                                 reads=[B_K[si], B_km], writes=[B_kj] if n < 7 else [B_kj, B_km2])
                        b.op("dve", lambda e: e.tensor_copy(out=kmb[:], in_=kmf[:]), reads=[B_km2], writes=[B_km])
                        pi = ps_in[0] % 2
                        ps_in[0] += 1
                        for t in range(NT):
                            b.op("pe", lambda e, t=t, si=si, pi=pi: e.matmul(psb[pi][:, t * 8:(t + 1) * 8], lhsT=QT[si][:, t * P:(t + 1) * P],
                                                                            rhs=kmb[:], start=True, stop=True),
                                 reads=[B_Q[si], B_km], writes=[PS[pi]] if t == 0 else [])
                        PS[pi].w = b.ops["pe"][-1]
                        g3v = lambda a: a[:]
                        mxb = lambda: mx[:].unsqueeze(2).to_broadcast([P, NT, 8])
                        b.op("dve", lambda e, pi=pi: e.tensor_tensor(out=gsb[:], in0=psb[pi][:, 0:NT * 8].rearrange("p (a b) -> p a b", b=8),
                                                                     in1=pastb[:], op=ALU.add), reads=[PS[pi], B_pb], writes=[B_g])
                        b.op("dve", lambda e: e.tensor_copy(out=g2[:], in_=gsb[:]), reads=[B_g], writes=[B_g])
                        for rep in range(2):
                            b.op("dve", lambda e: e.tensor_reduce(out=mx[:], in_=g2[:], axis=AX.X, op=ALU.max), reads=[B_g], writes=[B_g])
                            b.op("dve", lambda e: e.tensor_tensor(out=eq[:], in0=g2[:], in1=mxb(), op=ALU.is_ge), reads=[B_g], writes=[B_g])
                            b.op("dve", lambda e: e.scalar_tensor_tensor(out=g2[:], in0=eq[:], scalar=-1e30, in1=g2[:],
                                                                        op0=ALU.mult, op1=ALU.add), reads=[B_g], writes=[B_g])
                        b.op("dve", lambda e: e.tensor_reduce(out=mx[:], in_=g2[:], axis=AX.X, op=ALU.max), reads=[B_g], writes=[B_g])
                        b.op("dve", lambda e: e.tensor_scalar(out=mx[:], in0=mx[:], scalar1=-1e29, scalar2=None, op0=ALU.max),
                             reads=[B_g], writes=[B_g])
                        b.op("dve", lambda e: e.tensor_tensor(out=eq[:], in0=gsb[:], in1=mxb(), op=ALU.is_ge), reads=[B_g], writes=[B_g])
                        if debug and h == 1 and l == layers[0]:
                            b.dma("sp", g_dbg, gsb[:].rearrange("p a b -> p (a b)"), "gdbg", reads=[B_g])
                            b.dma("sp", e_dbg, eq[:].rearrange("p a b -> p (a b)"), "edbg", reads=[B_g])
                            b.dma("sp", m_dbg, mx[:], "mdbg", reads=[B_g])
                        b.op("dve", lambda e, ni=ni: e.tensor_scalar(out=nsel[ni][:], in0=eq[:], scalar1=-1.0, scalar2=-NEG,
                                                                     op0=ALU.add, op1=ALU.mult), reads=[B_g], writes=[B_nsel[ni]])
                        for q4 in range(4):
                            qts = [q4 * 4 + i for i in range(4)]
                            steps = []
                            for pos in range(4):
                                qtile = qts[pos]
                                for k0_ in range(0, qtile + 1, 4):
                                    kts = list(range(k0_, min(k0_ + 4, qtile + 1)))
                                    blocks = []
                                    for ktile in kts:
                                        sel = None
                                        if qtile // 2 > ktile // 2:
                                            sel = nsel[ni][:, qtile, (ktile // 2):(ktile // 2) + 1].to_broadcast([P, P])
                                        blocks.append((pos, ktile, sel))
                                    r0 = 15 - qtile + kts[0]
                                    steps.append(dict(blocks=blocks, selbuf=B_nsel[ni],
                                                      tab=tab[ti][:, r0 * P:(r0 + len(kts)) * P].rearrange("p (a b) -> p a b", b=P)))
                            units.append(dict(qcols=[q * P for q in qts], steps=steps,
                                              acc=lambda a, q4=q4: a[:, q4 * 512:(q4 + 1) * 512], pshape=None))
                    pair_units[idx] = units
                    yield

            def A_gen(idx):
                h, gi = pairs[idx]
                ti, zi, si = h % 2, h % 2, idx % 2
                yield from attention(h, gi, si, ti, pair_units[idx], gi == 0)
                if gi != ngrp - 1:
                    return
                oi = h % 2
                b.op("dve", lambda e: e.reciprocal(out=acc_s[:], in_=acc_s[:]), reads=[B_acc], writes=[B_acc])
                b.op("dve", lambda e: e.tensor_tensor(out=acc_o[:], in0=acc_o[:], in1=acc_s[:], op=ALU.mult), reads=[B_acc], writes=[B_acc])
                b.op("dve", lambda e, oi=oi, zi=zi: e.tensor_tensor(out=osb[oi][:], in0=acc_o[:], in1=SZ[zi][:], op=ALU.mult),
                     reads=[B_acc, B_SZ[zi]], writes=[B_osb[oi]])
                st_op = b.dma("sp", oT_d[h], osb[oi][:], "ost%d" % oi, reads=[B_osb[oi]])
                o = b.coll(lambda e, h=h: e.collective_compute("AllGather", ALU.bypass, replica_groups=GROUPS,
                                                               ins=[oT_d[h].opt()], outs=[og_t[h].ap().opt()]), "cc")
                o.waits.append(st_op)

            for _ in P_gen(0):
                pass
            for idx in range(len(pairs)):
                pg = P_gen(idx + 1) if idx + 1 < len(pairs) else iter(())
                na = sum(len(u["steps"]) for u in pair_units[idx])
                npc = 24 if (idx + 1 < len(pairs) and pairs[idx + 1][1] == 0) else 14
                done = 0
                for ai, _ in enumerate(A_gen(idx)):
                    target = ((ai + 1) * npc + na - 1) // na
                    while done < target:
                        if next(pg, "end") == "end":
                            done = 10 ** 9
                            break
                        done += 1
                for _ in pg:
                    pass
            b.end_phase()

    src = x_in
    for l in layers:
        norm_phase(l, src, False)
        mixer_phase(l, "A" if l % 2 == 0 else "B", w_in_d[l])
        out_phase(l, w_out_d[l], src)
        src = xs
    if do_final:
        norm_phase(0, src, True)
    else:
        with contextlib.ExitStack() as st:
            cp = [b.sb(st, "cp%d" % i, [P, D], F32) for i in range(2)]
            B_cp = [Buf("cp%d" % i) for i in range(2)]
            for t in range(NT):
                i = t % 2
                b.dma("sp", cp[i][:], src[t * P:(t + 1) * P, :], "cpl%d" % i, writes=[B_cp[i]])
                b.dma("sp", x_out[t * P:(t + 1) * P, :], cp[i][:], "cps%d" % i, reads=[B_cp[i]])
            b.end_phase()
    g.close()
    b.close()
    return nc


def _consts():
    slopes = np.exp2(-8.0 * np.arange(1, NH + 1, dtype=np.float64) / NH)
    k = np.arange(P)[:, None].astype(np.float64)
    q = np.arange(P)[None, :].astype(np.float64)
    tabA = np.zeros((NH, P, 12, P), np.float32)
    for h in range(NH):
        for gi, d in enumerate([1, 4, 16]):
            for blk in range(4):
                prev = (blk % 2 == 0) and gi < 2
                if not prev:
                    step = q - k
                    ok = step >= 0
                else:
                    step = q - k + P
                    ok = step <= P
                tabA[h, :, gi * 4 + blk, :] = np.where(ok, np.maximum(-slopes[h] * d * step, NEG), NEG)
    tabB = np.zeros((NH, P, 16, P), np.float32)
    for h in range(NH):
        for dl in range(16):
            dist = q - k + P * dl
            tabB[h, :, 15 - dl, :] = np.where(dist >= 0, np.maximum(-slopes[h] * dist, NEG), NEG)
    pastb = np.zeros((P, NT, 8), np.float32)
    for t in range(NT):
        for n in range(8):
            if n >= t // 2:
                pastb[:, t, n] = -1e30
    return (np.eye(P, dtype=np.float32), tabA.reshape(NH, P, 12 * P), tabB.reshape(NH, P, 16 * P), pastb.reshape(P, NT * 8))


_PROG_CACHE = {}


def _run(layers, do_final, xin, inputs, debug=False):
    key = (tuple(layers), do_final, debug)
    if key not in _PROG_CACHE:
        _PROG_CACHE[key] = build_program(list(layers), do_final, debug)
    nc = _PROG_CACHE[key]
    ident, tabA, tabB, pastb = _consts()
    c = np.asarray(inputs["c"], np.float32)
    nw = np.asarray(inputs["norm_w"], np.float32)
    nwT = np.ascontiguousarray(nw.reshape(DEPTH, NCH, P).transpose(2, 0, 1))
    fnw = np.asarray(inputs["final_norm_w"], np.float32).reshape(1, D)
    mod_b = np.ascontiguousarray(np.asarray(inputs["mod_b"], np.float32))
    per_half = [dict(), dict()]
    shared = {"mod_b": mod_b}
    for l in layers:
        shared["mod_w%d" % l] = np.ascontiguousarray(np.asarray(inputs["mod_w"][l], np.float32))
        pre = "a" if l % 2 == 0 else "b"
        w = np.asarray(inputs[pre + "_w_in"][l // 2], np.float32)
        nparts = w.shape[1] // D
        w4 = w.reshape(D, nparts, 2, D // 2)
        for hh in range(2):
            per_half[hh]["w_in%d" % l] = np.ascontiguousarray(w4[:, :, hh, :].reshape(D, nparts * (D // 2)))
        shared["w_out%d" % l] = np.ascontiguousarray(np.asarray(inputs[pre + "_w_out"][l // 2], np.float32))
    in_maps = []
    for core in range(N_CORES):
        bb, hh = core // 2, core % 2
        m = dict(shared)
        m.update(per_half[hh])
        m["x_in"] = np.ascontiguousarray(xin[bb])
        m["cT"] = np.ascontiguousarray(c[bb].reshape(NCH, P).T)
        m["nwT"] = nwT
        m["fnw"] = fnw
        m["ident"] = ident
        m["tabA"] = np.ascontiguousarray(tabA[hh * NHL:(hh + 1) * NHL])
        m["tabB"] = np.ascontiguousarray(tabB[hh * NHL:(hh + 1) * NHL])
        m["pastb"] = pastb
        in_maps.append(m)
    res = run_bass_kernel_spmd(nc, in_maps, core_ids=list(range(N_CORES)))
    if debug:
        _run.dbg = res.results
    return np.stack([np.asarray(res.results[2 * i]["x_out"], np.float32) for i in range(N_CORES // 2)], 0)


LAUNCH_PLAN = [([0, 1, 2, 3], True)]


def kernel(x, c, norm_w, mod_w, mod_b, a_w_in, a_w_out, b_w_in, b_w_out, final_norm_w):
    inputs = dict(c=c, norm_w=norm_w, mod_w=mod_w, mod_b=mod_b, a_w_in=a_w_in, a_w_out=a_w_out,
                  b_w_in=b_w_in, b_w_out=b_w_out, final_norm_w=final_norm_w)
    cur = np.asarray(x, np.float32)
    for layers, fin in LAUNCH_PLAN:
        cur = _run(layers, fin, cur, inputs)
    return cur
```

```python
import contextlib
import numpy as np
import ml_dtypes
import concourse.bass as bass
import concourse.mybir as mybir
from concourse.bass_utils import run_bass_kernel_spmd

F32 = mybir.dt.float32
BF16 = mybir.dt.bfloat16
AF = mybir.ActivationFunctionType
ALU = mybir.AluOpType
AX = mybir.AxisListType
NPBF = ml_dtypes.bfloat16

P = 128
D = 2048
S = 2048
NH = 16
DEPTH = 4
NCH = D // P
NT = S // P
EPS = 1e-6
SCALE = 128 ** -0.5
NEG = -30000.0
ENGS = ["pe", "act", "dve", "pool", "sp"]
N_CORES = 8
NHL = NH // 2
GROUPS = [[0, 1], [2, 3], [4, 5], [6, 7]]


class Op:
    __slots__ = ("eng", "fn", "waits", "signal", "count", "dsem", "dval", "inc", "phase", "raw")


class Buf:
    def __init__(self, name):
        self.name = name
        self.w = None
        self.r = {}


class Builder:
    def __init__(self, nc):
        self.nc = nc
        self.ops = {e: [] for e in ENGS}
        self.dcount = {}
        self.gstack = contextlib.ExitStack()
        self.esem = {e: self.gstack.enter_context(nc.semaphore("es_" + e)) for e in ENGS}
        self.dsem = {}
        self.ecount = {e: 0 for e in ENGS}
        self.seen = {e: {} for e in ENGS}
        self.phase = 0
        self.lastd = {}

    def sb(self, stack, name, shape, dt):
        self.uid = getattr(self, "uid", 0) + 1
        return stack.enter_context(self.nc.sbuf_tensor("s%d_%s" % (self.uid, name), list(shape), dt))

    def _op(self, eng, fn, waits):
        o = Op()
        o.eng = eng
        o.fn = fn
        o.waits = [w for w in waits if w is not None and w.phase == self.phase]
        for w in o.waits:
            if w.dsem is None:
                w.signal = True
        o.signal = False
        o.dsem = None
        o.count = 0
        o.dval = 0
        o.inc = 16
        o.phase = self.phase
        o.raw = ()
        self.ops[eng].append(o)
        return o

    def op(self, eng, fn, reads=(), writes=(), extra=(), dsem=None):
        waits = list(extra)
        raw = []
        for bf in reads:
            if bf.w is not None:
                waits.append(bf.w)
                raw.append(bf.w)
        for bf in writes:
            if bf.w is not None:
                waits.append(bf.w)
            waits.extend(bf.r.values())
        o = self._op(eng, fn, waits)
        o.raw = raw
        if dsem is not None:
            o.dsem = dsem
            self.dcount[dsem] = self.dcount.get(dsem, 0) + 16
            o.dval = self.dcount[dsem]
            self.lastd[dsem] = o
        key = eng if dsem is None else ("d", dsem)
        for bf in reads:
            bf.r[key] = o
        for bf in writes:
            bf.w = o
            bf.r = {}
        return o

    def dma(self, eng, out, in_, dsem, reads=(), writes=(), extra=(), **kw):
        return self.op(eng, lambda e: e.dma_start(out=out, in_=in_, **kw), reads, writes, extra, dsem=dsem)

    def coll(self, fn, dsem, reads=(), writes=()):
        o = self.op("pool", fn, reads, writes, dsem=dsem)
        self.dcount[dsem] += 1 - 16
        o.dval = self.dcount[dsem]
        o.inc = 1
        return o

    def end_phase(self, final_waits=()):
        nc = self.nc
        lasts = {}
        for e in ENGS:
            for o in reversed(self.ops[e]):
                if o.dsem is None and o.fn is not None:
                    lasts[e] = o
                    break
        dl = [o for o in self.lastd.values() if o.phase == self.phase]
        for e in ENGS:
            self._op(e, None, [lasts[x] for x in lasts if x != e] + dl)
        for e in ENGS:
            c = self.ecount[e]
            for o in self.ops[e]:
                if o.dsem is None and o.signal:
                    c += 1
                    o.count = c
            self.ecount[e] = c
        for k in self.dcount:
            if k not in self.dsem:
                self.dsem[k] = self.gstack.enter_context(nc.semaphore("ds_%s" % (k,)))
        ops, esem, dsem = self.ops, self.esem, self.dsem

        def run(engname):
            def f(eng):
                seen = self.seen[engname]
                for o in ops[engname]:
                    for w in o.waits:
                        if w.dsem is not None:
                            sem, val, key = dsem[w.dsem], w.dval, ("d", w.dsem)
                        else:
                            if w.eng == engname and (engname == "pe" or not any(w is r for r in o.raw)):
                                continue
                            sem, val, key = esem[w.eng], w.count, ("e", w.eng)
                        if seen.get(key, 0) >= val:
                            continue
                        eng.wait_ge(sem, val)
                        seen[key] = val
                    if o.fn is None:
                        continue
                    ins = o.fn(eng)
                    if o.dsem is not None:
                        ins.then_inc(dsem[o.dsem], o.inc)
                    elif o.signal:
                        ins.then_inc(esem[engname], 1)
            return f

        with nc.Block() as block:
            block.tensor(run("pe"))
            block.scalar(run("act"))
            block.vector(run("dve"))
            block.gpsimd(run("pool"))
            block.sync(run("sp"))
        self.ops = {e: [] for e in ENGS}
        self.phase += 1

    def close(self):
        self.gstack.close()


def build_program(layers, do_final, debug=False):
    nc = bass.Bass("TRN2", target_bir_lowering=False)
    dt_in = lambda n, s, d=F32: nc.dram_tensor(n, list(s), d, kind="ExternalInput").ap()
    x_in = dt_in("x_in", [S, D])
    cT_d = dt_in("cT", [P, NCH])
    nwT_d = dt_in("nwT", [P, DEPTH, NCH])
    fnw_d = dt_in("fnw", [1, D])
    mod_w = {l: dt_in("mod_w%d" % l, [D, 3 * D]) for l in layers}
    mod_b = dt_in("mod_b", [DEPTH, 3 * D])
    w_in_d = {l: dt_in("w_in%d" % l, [D, (10 if l % 2 == 0 else 4) * (D // 2)]) for l in layers}
    w_out_d = {l: dt_in("w_out%d" % l, [D, D]) for l in layers}
    ident_d = dt_in("ident", [P, P])
    tabA_d = dt_in("tabA", [NHL, P, 12 * P])
    tabB_d = dt_in("tabB", [NHL, P, 16 * P])
    pastb_d = dt_in("pastb", [P, NT * 8])
    x_out = nc.dram_tensor("x_out", [S, D], F32, kind="ExternalOutput").ap()
    xs = nc.dram_tensor("xs", [S, D], F32).ap()
    dk = dict(kind="ExternalOutput") if debug else {}
    oT_d = nc.dram_tensor("oT_s", [NHL, P, S], BF16, **dk).ap()
    og_t = [nc.dram_tensor("og%d" % i, [2 * P, S], BF16) for i in range(NHL)]
    modv = nc.dram_tensor("modv", [DEPTH, 3 * D], F32, **dk).ap()
    if debug:
        hT_dbg = nc.dram_tensor("hT_dbg", [P, NCH, S], BF16, kind="ExternalOutput").ap()
        g_dbg = nc.dram_tensor("g_dbg", [P, NT * 8], F32, kind="ExternalOutput").ap()
        e_dbg = nc.dram_tensor("e_dbg", [P, NT * 8], F32, kind="ExternalOutput").ap()
        m_dbg = nc.dram_tensor("m_dbg", [P, NT], F32, kind="ExternalOutput").ap()

    b = Builder(nc)
    g = contextlib.ExitStack()
    psb = [g.enter_context(nc.psum_tensor("ps%d" % i, [P, 512], F32)) for i in range(8)]
    PS = [Buf("ps%d" % i) for i in range(8)]
    ident_f = b.sb(g, "ident_f", [P, P], F32)
    ident_b = b.sb(g, "ident_b", [P, P], BF16)
    ones_b = b.sb(g, "ones_b", [P, P], BF16)
    hT = b.sb(g, "hT", [P, NCH, S], BF16)
    B_hT = Buf("hT")
    condT = b.sb(g, "condT", [P, NCH], BF16)
    nwT = b.sb(g, "nwT", [P, DEPTH, NCH], F32)
    B_const = Buf("const")
    B_id, B_nw, B_cond, B_ones = Buf("id"), Buf("nw"), Buf("cond"), Buf("ones")

    MW = 256
    NMU = 3 * D // MW
    ps_in = [0]

    def mod_bufs(st):
        return dict(w=[b.sb(st, "wmod%d" % i, [P, NCH, MW], BF16) for i in range(2)],
                    Bw=[Buf("wmod%d" % i) for i in range(2)],
                    mb=[b.sb(st, "mbp%d" % i, [1, MW], F32) for i in range(2)],
                    Bmb=[Buf("mbp%d" % i) for i in range(2)],
                    row=[b.sb(st, "mrow%d" % i, [1, MW], F32) for i in range(2)],
                    Brow=[Buf("mrow%d" % i) for i in range(2)], k=[0])

    mv_last = {}

    def mod_unit(m, cb, mbuf):
        k = mbuf["k"][0] % 2
        mbuf["k"][0] += 1
        wt, bw = mbuf["w"][k], mbuf["Bw"][k]
        cs = slice(cb * MW, (cb + 1) * MW)
        b.dma("pool", wt[:], mod_w[m][:, cs].rearrange("(c p) n -> p c n", p=P), "wmod%d" % k, writes=[bw])
        b.dma("sp", mbuf["mb"][k][:], mod_b[m:m + 1, cs], "mbp%d" % k, writes=[mbuf["Bmb"][k]])
        pi = ps_in[0] % 2
        ps_in[0] += 1
        for c in range(NCH):
            b.op("pe", lambda e, wt=wt, c=c, pi=pi: e.matmul(psb[pi][0:1, 0:MW], lhsT=condT[:, c:c + 1], rhs=wt[:, c, :],
                                                             start=(c == 0), stop=(c == NCH - 1)),
                 reads=[bw, B_cond], writes=[PS[pi]] if c == 0 else [])
        PS[pi].w = b.ops["pe"][-1]
        b.op("dve", lambda e, k=k, pi=pi: e.tensor_tensor(out=mbuf["row"][k][:], in0=psb[pi][0:1, 0:MW], in1=mbuf["mb"][k][:], op=ALU.add),
             reads=[PS[pi], mbuf["Bmb"][k]], writes=[mbuf["Brow"][k]])
        mv_last[k] = b.dma("sp", modv[m:m + 1, cs], mbuf["row"][k][:], "mvst%d" % k, reads=[mbuf["Brow"][k]])

    cT_f = b.sb(g, "cT_f", [P, NCH], F32)
    B_cT = Buf("cT")
    mb_glob = mod_bufs(g)
    b.dma("sp", ident_f[:], ident_d, "c0", writes=[B_id])
    b.dma("sp", nwT[:], nwT_d, "c1", writes=[B_nw])
    b.dma("sp", cT_f[:], cT_d, "c2", writes=[B_cT])
    b.op("dve", lambda e: e.tensor_copy(out=ident_b[:], in_=ident_f[:]), reads=[B_id], writes=[B_const])
    b.op("dve", lambda e: e.memset(ones_b[:], 1.0), writes=[B_ones])
    b.op("act", lambda e: e.activation(out=condT[:], in_=cT_f[:], func=AF.Silu), reads=[B_cT], writes=[B_cond])
    for cb in range(NMU):
        mod_unit(layers[0], cb, mb_glob)

    def norm_phase(l, src, final):
        with contextlib.ExitStack() as st:
            xt = [b.sb(st, "xt%d" % i, [P, D], F32) for i in range(2)]
            B_xt = [Buf("xt%d" % i) for i in range(2)]
            xn = [b.sb(st, "xn%d" % i, [P, D], F32) for i in range(2)]
            B_xn = [Buf("xn%d" % i) for i in range(2)]
            junk = b.sb(st, "junk", [P, D], BF16)
            ss = b.sb(st, "ss", [P, 2], F32)
            B_ss = Buf("ss")
            B_vec, B_sh, B_sc = Buf("vec"), Buf("sh"), Buf("sc")
            if final:
                fw = b.sb(st, "fw", [P, D], F32)
                b.dma("sp", fw[:], fnw_d.partition_broadcast(P), "fw", writes=[B_vec])
            else:
                sc = b.sb(st, "sc", [P, NCH], F32)
                sh = b.sb(st, "sh", [P, NCH], F32)
                weff = b.sb(st, "weff", [P, NCH], F32)
                b.dma("sp", sh[:], modv[l, 0:D].rearrange("(c p) -> p c", p=P), "sh", writes=[B_sh], extra=list(mv_last.values()), allow_slow_non_contiguous=True)
                b.dma("sp", sc[:], modv[l, D:2 * D].rearrange("(c p) -> p c", p=P), "sc", writes=[B_sc], extra=list(mv_last.values()), allow_slow_non_contiguous=True)
                b.op("dve", lambda e: e.scalar_tensor_tensor(out=weff[:], in0=sc[:], scalar=1.0, in1=nwT[:, l, :],
                                                            op0=ALU.add, op1=ALU.mult), reads=[B_sc, B_nw], writes=[B_vec])
            for t in range(NT):
                i = t % 2
                b.dma("sp", xt[i][:], src[t * P:(t + 1) * P, :], "xt%d" % i, writes=[B_xt[i]])
                b.op("dve", lambda e, i=i: e.memset(ss[:, i:i + 1], 0.0), writes=[B_ss])
                b.op("act", lambda e, i=i: e.activation(out=junk[:], in_=xt[i][:], func=AF.Square, accum_out=ss[:, i:i + 1]),
                     reads=[B_xt[i]], writes=[B_ss])
                b.op("dve", lambda e, i=i: e.tensor_scalar(out=ss[:, i:i + 1], in0=ss[:, i:i + 1], scalar1=1.0 / D, scalar2=EPS,
                                                           op0=ALU.mult, op1=ALU.add), reads=[B_ss], writes=[B_ss])
                b.op("act", lambda e, i=i: e.sqrt(out=ss[:, i:i + 1], in_=ss[:, i:i + 1]), reads=[B_ss], writes=[B_ss])
                b.op("dve", lambda e, i=i: e.reciprocal(out=ss[:, i:i + 1], in_=ss[:, i:i + 1]), reads=[B_ss], writes=[B_ss])
                if final:
                    b.op("dve", lambda e, i=i: e.scalar_tensor_tensor(out=xn[i][:], in0=xt[i][:], scalar=ss[:, i:i + 1], in1=fw[:],
                                                                      op0=ALU.mult, op1=ALU.mult),
                         reads=[B_xt[i], B_ss, B_vec], writes=[B_xn[i]])
                    b.dma("pool", x_out[t * P:(t + 1) * P, :], xn[i][:], "xo%d" % i, reads=[B_xn[i]])
                else:
                    b.op("dve", lambda e, i=i: e.tensor_scalar(out=xn[i][:], in0=xt[i][:], scalar1=ss[:, i:i + 1], scalar2=None,
                                                               op0=ALU.mult), reads=[B_xt[i], B_ss], writes=[B_xn[i]])
                    for q4 in range(4):
                        pi = (t * 4 + q4) % 4
                        for j in range(4):
                            c = q4 * 4 + j
                            b.op("pe", lambda e, i=i, c=c, j=j, pi=pi: e.transpose(out=psb[pi][:, j * P:(j + 1) * P],
                                                                                   in_=xn[i][:, c * P:(c + 1) * P], identity=ident_f[:]),
                                 reads=[B_xn[i], B_id], writes=[PS[pi]] if j == 0 else [])
                        PS[pi].w = b.ops["pe"][-1]
                        for j in range(4):
                            c = q4 * 4 + j
                            b.op("act", lambda e, c=c, j=j, pi=pi, t=t: e.activation(
                                out=hT[:, c, t * P:(t + 1) * P], in_=psb[pi][:, j * P:(j + 1) * P], func=AF.Identity,
                                bias=sh[:, c:c + 1], scale=weff[:, c:c + 1]), reads=[PS[pi], B_vec, B_sh], writes=[B_hT])
            if debug and not final and l == layers[0]:
                b.dma("sp", hT_dbg, hT[:], "hdbg", reads=[B_hT])
            b.end_phase()

    def out_phase(l, w_out, src):
        with contextlib.ExitStack() as st:
            wo = b.sb(st, "wo", [P, NCH, D], BF16)
            B_wo = [Buf("wo%d" % i) for i in range(4)]
            ot = [b.sb(st, "ot%d" % i, [P, NH, 512], BF16) for i in range(2)]
            B_ot = [Buf("ot%d" % i) for i in range(2)]
            xt = [b.sb(st, "xq%d" % i, [P, D], F32) for i in range(2)]
            B_xt = [Buf("xq%d" % i) for i in range(2)]
            gate = b.sb(st, "gate", [P, D], F32)
            B_gate = Buf("gate")
            b.dma("sp", gate[:], modv[l:l + 1, 2 * D:3 * D].partition_broadcast(P), "gate", writes=[B_gate])
            for cb in range(4):
                b.dma("pool", wo[:, :, cb * 512:(cb + 1) * 512], w_out[:, cb * 512:(cb + 1) * 512].rearrange("(c p) n -> p c n", p=P),
                      "wo%d" % cb, writes=[B_wo[cb]])
            n = 0
            for tt in range(4):
                oi = tt % 2
                for hl in range(NHL):
                    b.dma("sp", ot[oi][:].rearrange("p (r h) t -> p r h t", r=2)[:, :, hl, :],
                          og_t[hl].ap()[:, tt * 512:(tt + 1) * 512].rearrange("(r p) t -> p r t", p=P), "ot%d" % oi,
                          writes=[B_ot[oi]] if hl == 0 else [])
                B_ot[oi].w = b.ops["sp"][-1]
                for s4 in range(4):
                    t = tt * 4 + s4
                    xi = t % 2
                    b.dma("sp", xt[xi][:], src[t * P:(t + 1) * P, :], "xq%d" % xi, writes=[B_xt[xi]])
                    for cb in range(4):
                        pi = n % 4
                        n += 1
                        for c in range(NH):
                            b.op("pe", lambda e, oi=oi, c=c, s4=s4, cb=cb, pi=pi: e.matmul(
                                psb[pi][:, :], lhsT=ot[oi][:, c, s4 * P:(s4 + 1) * P], rhs=wo[:, c, cb * 512:(cb + 1) * 512],
                                start=(c == 0), stop=(c == NH - 1)),
                                reads=[B_ot[oi], B_wo[cb]], writes=[PS[pi]] if c == 0 else [])
                        PS[pi].w = b.ops["pe"][-1]
                        b.op("dve", lambda e, pi=pi, cb=cb: e.tensor_tensor(
                            out=psb[pi][:, :], in0=psb[pi][:, :], in1=gate[:, cb * 512:(cb + 1) * 512], op=ALU.mult),
                            reads=[B_gate], writes=[PS[pi]])
                        b.op("dve", lambda e, pi=pi, xi=xi, cb=cb: e.tensor_tensor(
                            out=xt[xi][:, cb * 512:(cb + 1) * 512], in0=psb[pi][:, :], in1=xt[xi][:, cb * 512:(cb + 1) * 512], op=ALU.add),
                            reads=[PS[pi]], writes=[B_xt[xi]])
                    b.dma("act", xs[t * P:(t + 1) * P, :], xt[xi][:], "xqs%d" % xi, reads=[B_xt[xi]])
            b.end_phase()

    def mixer_phase(l, kind, w_in):
        ngrp = 3 if kind == "A" else 1
        dil = [1, 4, 16] if kind == "A" else [1]
        with contextlib.ExitStack() as st:
            NW = 6
            wr = [b.sb(st, "wr%d" % i, [P, NCH, P], BF16) for i in range(NW)]
            B_wr = [Buf("wr%d" % i) for i in range(NW)]
            QT = [b.sb(st, "QT%d" % i, [P, S], BF16) for i in range(2)]
            KT = [b.sb(st, "KT%d" % i, [P, S], BF16) for i in range(2)]
            V = [b.sb(st, "V%d" % i, [P, NT, P], BF16) for i in range(2)]
            B_Q = [Buf("Q%d" % i) for i in range(2)]
            B_K = [Buf("K%d" % i) for i in range(2)]
            B_V = [Buf("V%d" % i) for i in range(2)]
            SZ = [b.sb(st, "SZ%d" % i, [P, S], BF16) for i in range(2)]
            B_SZ = [Buf("SZ%d" % i) for i in range(2)]
            acc_o = b.sb(st, "acc_o", [P, S], F32)
            acc_s = b.sb(st, "acc_s", [P, S], F32)
            B_acc = Buf("acc")
            osb = [b.sb(st, "osb%d" % i, [P, S], BF16) for i in range(2)]
            B_osb = [Buf("osb%d" % i) for i in range(2)]
            tmp = [b.sb(st, "tmp%d" % i, [P, 512], F32) for i in range(2)]
            B_tmp = [Buf("tmp%d" % i) for i in range(2)]
            PT = [b.sb(st, "PT%d" % i, [P, 512], BF16) for i in range(3)]
            B_PT = [Buf("PT%d" % i) for i in range(3)]
            tw = 12 * P if kind == "A" else 16 * P
            tab = [b.sb(st, "tab%d" % i, [P, tw], F32) for i in range(2)]
            B_tab = [Buf("tab%d" % i) for i in range(2)]
            tab_d = tabA_d if kind == "A" else tabB_d
            if kind == "B":
                kmf = b.sb(st, "kmf", [P, 8], F32)
                kmb = b.sb(st, "kmb", [P, 8], BF16)
                pastb = b.sb(st, "pastb", [P, NT, 8], F32)
                gsb = b.sb(st, "gsb", [P, NT, 8], F32)
                g2 = b.sb(st, "g2", [P, NT, 8], F32)
                eq = b.sb(st, "eq", [P, NT, 8], F32)
                mx = b.sb(st, "mx", [P, NT], F32)
                nsel = [b.sb(st, "nsel%d" % i, [P, NT, 8], BF16) for i in range(2)]
                B_nsel = [Buf("nsel%d" % i) for i in range(2)]
                B_km, B_g = Buf("km"), Buf("g")
                B_km2, B_kj = Buf("km2"), Buf("kj")
                kjunk = b.sb(st, "kjunk", [P, 256], BF16)
                B_pb = Buf("pastb")
                b.dma("sp", pastb[:].rearrange("p a b -> p (a b)"), pastb_d, "pastb", writes=[B_pb])
            wk = [0]
            un = [0]
            nxt = layers[layers.index(l) + 1] if layers.index(l) + 1 < len(layers) else None
            mbm = mb_glob
            mod_todo = list(range(NMU)) if nxt is not None else []

            def load_w(col0):
                i = wk[0] % NW
                wk[0] += 1
                b.dma("pool", wr[i][:], w_in[:, col0:col0 + P].rearrange("(c p) n -> p c n", p=P), "wr%d" % i, writes=[B_wr[i]])
                return i

            def proj_T(col0, dst, B_dst, d, func=None):
                wi = load_w(col0)
                for tt in range(4):
                    pi = ps_in[0] % 2
                    ps_in[0] += 1
                    for c in range(NCH):
                        b.op("pe", lambda e, wi=wi, c=c, tt=tt, pi=pi: e.matmul(
                            psb[pi][:, :], lhsT=wr[wi][:, c, :], rhs=hT[:, c, tt * 512:(tt + 1) * 512],
                            start=(c == 0), stop=(c == NCH - 1)),
                            reads=[B_wr[wi], B_hT], writes=[PS[pi]] if c == 0 else [])
                    PS[pi].w = b.ops["pe"][-1]
                    if d == 1:
                        o_ap = dst[:, tt * 512:(tt + 1) * 512]
                        i_ap = psb[pi][:, :]
                    else:
                        nl = 512 // d
                        o_ap = dst[:, :].rearrange("p (r l) -> p r l", r=d)[:, :, tt * nl:(tt + 1) * nl]
                        i_ap = psb[pi][:, :].rearrange("p (l r) -> p r l", r=d)
                    if func is None:
                        b.op("dve", lambda e, o_ap=o_ap, i_ap=i_ap: e.tensor_copy(out=o_ap, in_=i_ap), reads=[PS[pi]], writes=[B_dst])
                    else:
                        b.op("act", lambda e, o_ap=o_ap, i_ap=i_ap: e.activation(out=o_ap, in_=i_ap, func=func),
                             reads=[PS[pi]], writes=[B_dst])

            def proj_V(col0, dst, B_dst, d):
                wi = load_w(col0)
                L = S // d
                for j4 in range(4):
                    pi = ps_in[0] % 2
                    ps_in[0] += 1
                    for jj in range(4):
                        j = j4 * 4 + jj
                        r, l0 = (j * P) // L, (j * P) % L
                        for c in range(NCH):
                            if d == 1:
                                lh = hT[:, c, j * P:(j + 1) * P]
                            else:
                                lh = hT[:, c, :].rearrange("p (l r) -> p r l", r=d)[:, r, l0:l0 + P]
                            b.op("pe", lambda e, wi=wi, c=c, lh=lh, jj=jj, pi=pi: e.matmul(
                                psb[pi][:, jj * P:(jj + 1) * P], lhsT=lh, rhs=wr[wi][:, c, :],
                                start=(c == 0), stop=(c == NCH - 1)),
                                reads=[B_wr[wi], B_hT], writes=[PS[pi]] if (c == 0 and jj == 0) else [])
                    PS[pi].w = b.ops["pe"][-1]
                    b.op("act", lambda e, j4=j4, pi=pi: e.copy(out=dst[:, j4 * 4:(j4 + 1) * 4, :].rearrange("p a b -> p (a b)"),
                                                              in_=psb[pi][:, :]), reads=[PS[pi]], writes=[B_dst])

            def attention(h, gi, si, ti, units, first_group):
                qt, kt, v = QT[si], KT[si], V[si]
                flat = []
                for u in units:
                    firsts, lasts = {}, {}
                    n = 0
                    for stp in u["steps"]:
                        for (pos, ktile, sel) in stp["blocks"]:
                            firsts.setdefault(pos, n)
                            lasts[pos] = n
                            n += 1
                    uo = un[0]
                    un[0] += 1
                    n = 0
                    for sidx, stp in enumerate(u["steps"]):
                        flat.append((u, uo, sidx, stp, firsts, lasts, n))
                        n += len(stp["blocks"])

                def qk(item):
                    u, uo, sidx, stp, firsts, lasts, n0 = item
                    n = sn[0]
                    sn[0] += 1
                    sb_i, k2, k3 = 2 + n % 2, n % 2, n % 3
                    blocks = stp["blocks"]
                    nb = len(blocks)
                    for i, (pos, ktile, sel) in enumerate(blocks):
                        b.op("pe", lambda e, i=i, ktile=ktile, sb_i=sb_i, sel=sel, qc=u["qcols"][pos]: e.matmul(
                            psb[sb_i][:, i * P:(i + 1) * P], lhsT=kt[:, ktile * P:(ktile + 1) * P], rhs=qt[:, qc:qc + P],
                            start=True, stop=(sel is None)),
                            reads=[B_K[si], B_Q[si]], writes=[PS[sb_i]] if i == 0 else [])
                        if sel is not None:
                            b.op("pe", lambda e, i=i, sb_i=sb_i, sel=sel: e.matmul(
                                psb[sb_i][:, i * P:(i + 1) * P], lhsT=sel, rhs=ident_b[:], start=False, stop=True),
                                reads=[stp["selbuf"], B_const])
                    PS[sb_i].w = b.ops["pe"][-1]
                    tb = stp["tab"]
                    b.op("dve", lambda e, sb_i=sb_i, k2=k2, nb=nb, tb=tb: e.scalar_tensor_tensor(
                        out=tmp[k2][:, 0:nb * P].rearrange("p (a b) -> p a b", b=P),
                        in0=psb[sb_i][:, 0:nb * P].rearrange("p (a b) -> p a b", b=P), scalar=SCALE, in1=tb,
                        op0=ALU.mult, op1=ALU.add), reads=[PS[sb_i], B_tab[ti]], writes=[B_tmp[k2]])
                    b.op("act", lambda e, k2=k2, k3=k3, nb=nb: e.activation(
                        out=PT[k3][:, 0:nb * P], in_=tmp[k2][:, 0:nb * P], func=AF.Exp),
                        reads=[B_tmp[k2]], writes=[B_PT[k3]])
                    return k3

                def pv(item, k3):
                    u, uo, sidx, stp, firsts, lasts, n0 = item
                    po, pso = 4 + uo % 2, 6 + uo % 2
                    npos = len(u["qcols"])
                    for bank, lhs_fn, rd in ((po, lambda ktile: v[:, ktile, :], [B_V[si], B_PT[k3]]),
                                             (pso, lambda ktile: ones_b[:], [B_ones, B_PT[k3]])):
                        for i, (pos, ktile, sel) in enumerate(stp["blocks"]):
                            n = n0 + i
                            b.op("pe", lambda e, i=i, pos=pos, lh=lhs_fn(ktile), k3=k3, bank=bank, f=(firsts[pos] == n),
                                 la=(lasts[pos] == n): e.matmul(
                                psb[bank][:, pos * P:(pos + 1) * P], lhsT=lh, rhs=PT[k3][:, i * P:(i + 1) * P],
                                start=f, stop=la), reads=rd, writes=[PS[bank]] if (sidx == 0 and i == 0) else [])
                    if sidx != len(u["steps"]) - 1:
                        return
                    PS[po].w = b.ops["pe"][-1]
                    PS[pso].w = b.ops["pe"][-1]
                    ao, as_ = u["acc"](acc_o), u["acc"](acc_s)
                    shp = u["pshape"]
                    pin_o = psb[po][:, 0:npos * P]
                    pin_s = psb[pso][:, 0:npos * P]
                    if shp is not None:
                        pin_o = pin_o.rearrange("p (a b) -> p a b", b=shp)
                        pin_s = pin_s.rearrange("p (a b) -> p a b", b=shp)
                    if first_group:
                        b.op("dve", lambda e, ao=ao, pin_o=pin_o: e.tensor_copy(out=ao, in_=pin_o), reads=[PS[po]], writes=[B_acc])
                        b.op("dve", lambda e, as_=as_, pin_s=pin_s: e.tensor_copy(out=as_, in_=pin_s), reads=[PS[pso]], writes=[B_acc])
                    else:
                        b.op("dve", lambda e, ao=ao, pin_o=pin_o: e.tensor_tensor(out=ao, in0=pin_o, in1=ao, op=ALU.add),
                             reads=[PS[po]], writes=[B_acc])
                        b.op("dve", lambda e, as_=as_, pin_s=pin_s: e.tensor_tensor(out=as_, in0=pin_s, in1=as_, op=ALU.add),
                             reads=[PS[pso]], writes=[B_acc])

                prev = None
                for item in flat:
                    k3 = qk(item)
                    if prev is not None:
                        pv(*prev)
                    prev = (item, k3)
                pv(*prev)

            sn = [0]
            pv_cnt = [0]
            sidx_g = 0
            DH = D // 2
            pend_cc = None
            for h in range(NHL):
                ti = h % 2
                b.dma("sp", tab[ti][:], tab_d[h], "tab%d" % ti, writes=[B_tab[ti]])
                zi = h % 2
                zcol = (9 * DH if kind == "A" else 3 * DH) + h * P
                for gi in range(ngrp):
                    d = dil[gi]
                    L = S // d
                    si = sidx_g % 2
                    sidx_g += 1
                    base = gi * 3 * DH if kind == "A" else 0
                    proj_T(base + h * P, QT[si], B_Q[si], d)
                    proj_T(base + DH + h * P, KT[si], B_K[si], d)
                    proj_V(base + 2 * DH + h * P, V[si], B_V[si], d)
                    if pend_cc is not None:
                        pend_cc()
                        pend_cc = None
                    if gi == 0:
                        for _ in range((NMU + NHL - 1) // NHL):
                            if mod_todo:
                                mod_unit(nxt, mod_todo.pop(0), mbm)
                    if gi == 0:
                        proj_T(zcol, SZ[zi], B_SZ[zi], 1, func=AF.Silu)
                    units = []
                    if kind == "A":
                        tb0 = gi * 4 * P
                        for u4 in range(4):
                            js = [u4 * 4 + i for i in range(4)]
                            ns = [(j * P % L) // P for j in js]
                            steps = []
                            if gi == 2:
                                steps.append(dict(blocks=[(i, js[i], None) for i in range(4)],
                                                  tab=tab[ti][:, tb0:tb0 + 4 * P].rearrange("p (a b) -> p a b", b=P)))
                            else:
                                for half in range(2):
                                    blocks = []
                                    for i in (2 * half, 2 * half + 1):
                                        if ns[i] >= 1:
                                            blocks.append((i, js[i] - 1, None))
                                        blocks.append((i, js[i], None))
                                    off = 4 - len(blocks)
                                    steps.append(dict(blocks=blocks, tab=tab[ti][:, tb0 + off * P:tb0 + 4 * P].rearrange(
                                        "p (a b) -> p a b", b=P)))
                            if d == 1:
                                accf = lambda a, u4=u4: a[:, u4 * 512:(u4 + 1) * 512]
                                pshape = None
                            elif d == 4:
                                accf = lambda a, u4=u4: a[:, :].rearrange("p (l r) -> p r l", r=4)[:, u4, :]
                                pshape = None
                            else:
                                accf = lambda a, u4=u4: a[:, :].rearrange("p (l r) -> p r l", r=16)[:, u4 * 4:(u4 + 1) * 4, :]
                                pshape = P
                            units.append(dict(qcols=[j * P for j in js], steps=steps, acc=accf, pshape=pshape))
                    else:
                        ni = h % 2
                        b.op("dve", lambda e: e.memset(kmf[:], 0.0), writes=[B_km])
                        for n in range(8):
                            b.op("act", lambda e, si=si, n=n: e.activation(out=kjunk[:], in_=KT[si][:, n * 256:(n + 1) * 256], func=AF.Identity,
                                                                            scale=1.0 / 256, accum_out=kmf[:, n:n + 1]),
                                 reads=[B_K[si], B_km], writes=[B_kj] if n < 7 else [B_kj, B_km2])
                        b.op("dve", lambda e: e.tensor_copy(out=kmb[:], in_=kmf[:]), reads=[B_km2], writes=[B_km])
                        pi = ps_in[0] % 2
                        ps_in[0] += 1
                        for t in range(NT):
                            b.op("pe", lambda e, t=t, si=si, pi=pi: e.matmul(psb[pi][:, t * 8:(t + 1) * 8], lhsT=QT[si][:, t * P:(t + 1) * P],
                                                                            rhs=kmb[:], start=True, stop=True),
                                 reads=[B_Q[si], B_km], writes=[PS[pi]] if t == 0 else [])
                        PS[pi].w = b.ops["pe"][-1]
                        g3v = lambda a: a[:]
                        mxb = lambda: mx[:].unsqueeze(2).to_broadcast([P, NT, 8])
                        b.op("dve", lambda e, pi=pi: e.tensor_tensor(out=gsb[:], in0=psb[pi][:, 0:NT * 8].rearrange("p (a b) -> p a b", b=8),
                                                                     in1=pastb[:], op=ALU.add), reads=[PS[pi], B_pb], writes=[B_g])
                        b.op("dve", lambda e: e.tensor_copy(out=g2[:], in_=gsb[:]), reads=[B_g], writes=[B_g])
                        for rep in range(2):
                            b.op("dve", lambda e: e.tensor_reduce(out=mx[:], in_=g2[:], axis=AX.X, op=ALU.max), reads=[B_g], writes=[B_g])
                            b.op("dve", lambda e: e.tensor_tensor(out=eq[:], in0=g2[:], in1=mxb(), op=ALU.is_ge), reads=[B_g], writes=[B_g])
                            b.op("dve", lambda e: e.scalar_tensor_tensor(out=g2[:], in0=eq[:], scalar=-1e30, in1=g2[:],
                                                                        op0=ALU.mult, op1=ALU.add), reads=[B_g], writes=[B_g])
                        b.op("dve", lambda e: e.tensor_reduce(out=mx[:], in_=g2[:], axis=AX.X, op=ALU.max), reads=[B_g], writes=[B_g])
                        b.op("dve", lambda e: e.tensor_scalar(out=mx[:], in0=mx[:], scalar1=-1e29, scalar2=None, op0=ALU.max),
                             reads=[B_g], writes=[B_g])
                        b.op("dve", lambda e: e.tensor_tensor(out=eq[:], in0=gsb[:], in1=mxb(), op=ALU.is_ge), reads=[B_g], writes=[B_g])
                        if debug and h == 1 and l == layers[0]:
                            b.dma("sp", g_dbg, gsb[:].rearrange("p a b -> p (a b)"), "gdbg", reads=[B_g])
                            b.dma("sp", e_dbg, eq[:].rearrange("p a b -> p (a b)"), "edbg", reads=[B_g])
                            b.dma("sp", m_dbg, mx[:], "mdbg", reads=[B_g])
                        b.op("dve", lambda e, ni=ni: e.tensor_scalar(out=nsel[ni][:], in0=eq[:], scalar1=-1.0, scalar2=-NEG,
                                                                     op0=ALU.add, op1=ALU.mult), reads=[B_g], writes=[B_nsel[ni]])
                        for q4 in range(4):
                            qts = [q4 * 4 + i for i in range(4)]
                            steps = []
                            for pos in range(4):
                                qtile = qts[pos]
                                for k0_ in range(0, qtile + 1, 4):
                                    kts = list(range(k0_, min(k0_ + 4, qtile + 1)))
                                    blocks = []
                                    for ktile in kts:
                                        sel = None
                                        if qtile // 2 > ktile // 2:
                                            sel = nsel[ni][:, qtile, (ktile // 2):(ktile // 2) + 1].to_broadcast([P, P])
                                        blocks.append((pos, ktile, sel))
                                    r0 = 15 - qtile + kts[0]
                                    steps.append(dict(blocks=blocks, selbuf=B_nsel[ni],
                                                      tab=tab[ti][:, r0 * P:(r0 + len(kts)) * P].rearrange("p (a b) -> p a b", b=P)))
                            units.append(dict(qcols=[q * P for q in qts], steps=steps,
                                              acc=lambda a, q4=q4: a[:, q4 * 512:(q4 + 1) * 512], pshape=None))
                    attention(h, gi, si, ti, units, gi == 0)
                oi = h % 2
                b.op("dve", lambda e: e.reciprocal(out=acc_s[:], in_=acc_s[:]), reads=[B_acc], writes=[B_acc])
                b.op("dve", lambda e: e.tensor_tensor(out=acc_o[:], in0=acc_o[:], in1=acc_s[:], op=ALU.mult), reads=[B_acc], writes=[B_acc])
                b.op("dve", lambda e, oi=oi, zi=zi: e.tensor_tensor(out=osb[oi][:], in0=acc_o[:], in1=SZ[zi][:], op=ALU.mult),
                     reads=[B_acc, B_SZ[zi]], writes=[B_osb[oi]])
                st_op = b.dma("sp", oT_d[h], osb[oi][:], "ost%d" % oi, reads=[B_osb[oi]])

                def issue_cc(h=h, st_op=st_op):
                    o = b.coll(lambda e: e.collective_compute("AllGather", ALU.bypass, replica_groups=GROUPS,
                                                              ins=[oT_d[h].opt()], outs=[og_t[h].ap().opt()]), "cc")
                    o.waits.append(st_op)
                pend_cc = issue_cc
            pend_cc()
            b.end_phase()

    src = x_in
    for l in layers:
        norm_phase(l, src, False)
        mixer_phase(l, "A" if l % 2 == 0 else "B", w_in_d[l])
        out_phase(l, w_out_d[l], src)
        src = xs
    if do_final:
        norm_phase(0, src, True)
    else:
        with contextlib.ExitStack() as st:
            cp = [b.sb(st, "cp%d" % i, [P, D], F32) for i in range(2)]
            B_cp = [Buf("cp%d" % i) for i in range(2)]
            for t in range(NT):
                i = t % 2
                b.dma("sp", cp[i][:], src[t * P:(t + 1) * P, :], "cpl%d" % i, writes=[B_cp[i]])
                b.dma("sp", x_out[t * P:(t + 1) * P, :], cp[i][:], "cps%d" % i, reads=[B_cp[i]])
            b.end_phase()
    g.close()
    b.close()
    return nc


def _consts():
    slopes = np.exp2(-8.0 * np.arange(1, NH + 1, dtype=np.float64) / NH)
    k = np.arange(P)[:, None].astype(np.float64)
    q = np.arange(P)[None, :].astype(np.float64)
    tabA = np.zeros((NH, P, 12, P), np.float32)
    for h in range(NH):
        for gi, d in enumerate([1, 4, 16]):
            for blk in range(4):
                prev = (blk % 2 == 0) and gi < 2
                if not prev:
                    step = q - k
                    ok = step >= 0
                else:
                    step = q - k + P
                    ok = step <= P
                tabA[h, :, gi * 4 + blk, :] = np.where(ok, np.maximum(-slopes[h] * d * step, NEG), NEG)
    tabB = np.zeros((NH, P, 16, P), np.float32)
    for h in range(NH):
        for dl in range(16):
            dist = q - k + P * dl
            tabB[h, :, 15 - dl, :] = np.where(dist >= 0, np.maximum(-slopes[h] * dist, NEG), NEG)
    pastb = np.zeros((P, NT, 8), np.float32)
    for t in range(NT):
        for n in range(8):
            if n >= t // 2:
                pastb[:, t, n] = -1e30
    return (np.eye(P, dtype=np.float32), tabA.reshape(NH, P, 12 * P), tabB.reshape(NH, P, 16 * P), pastb.reshape(P, NT * 8))


_PROG_CACHE = {}


def _run(layers, do_final, xin, inputs, debug=False):
    key = (tuple(layers), do_final, debug)
    if key not in _PROG_CACHE:
        _PROG_CACHE[key] = build_program(list(layers), do_final, debug)
    nc = _PROG_CACHE[key]
    ident, tabA, tabB, pastb = _consts()
    c = np.asarray(inputs["c"], np.float32)
    nw = np.asarray(inputs["norm_w"], np.float32)
    nwT = np.ascontiguousarray(nw.reshape(DEPTH, NCH, P).transpose(2, 0, 1))
    fnw = np.asarray(inputs["final_norm_w"], np.float32).reshape(1, D)
    mod_b = np.ascontiguousarray(np.asarray(inputs["mod_b"], np.float32))
    per_half = [dict(), dict()]
    shared = {"mod_b": mod_b}
    for l in layers:
        shared["mod_w%d" % l] = np.ascontiguousarray(np.asarray(inputs["mod_w"][l], np.float32))
        pre = "a" if l % 2 == 0 else "b"
        w = np.asarray(inputs[pre + "_w_in"][l // 2], np.float32)
        nparts = w.shape[1] // D
        w4 = w.reshape(D, nparts, 2, D // 2)
        for hh in range(2):
            per_half[hh]["w_in%d" % l] = np.ascontiguousarray(w4[:, :, hh, :].reshape(D, nparts * (D // 2)))
        shared["w_out%d" % l] = np.ascontiguousarray(np.asarray(inputs[pre + "_w_out"][l // 2], np.float32))
    in_maps = []
    for core in range(N_CORES):
        bb, hh = core // 2, core % 2
        m = dict(shared)
        m.update(per_half[hh])
        m["x_in"] = np.ascontiguousarray(xin[bb])
        m["cT"] = np.ascontiguousarray(c[bb].reshape(NCH, P).T)
        m["nwT"] = nwT
        m["fnw"] = fnw
        m["ident"] = ident
        m["tabA"] = np.ascontiguousarray(tabA[hh * NHL:(hh + 1) * NHL])
        m["tabB"] = np.ascontiguousarray(tabB[hh * NHL:(hh + 1) * NHL])
        m["pastb"] = pastb
        in_maps.append(m)
    res = run_bass_kernel_spmd(nc, in_maps, core_ids=list(range(N_CORES)))
    if debug:
        _run.dbg = res.results
    return np.stack([np.asarray(res.results[2 * i]["x_out"], np.float32) for i in range(N_CORES // 2)], 0)


LAUNCH_PLAN = [([0, 1, 2, 3], True)]


def kernel(x, c, norm_w, mod_w, mod_b, a_w_in, a_w_out, b_w_in, b_w_out, final_norm_w):
    inputs = dict(c=c, norm_w=norm_w, mod_w=mod_w, mod_b=mod_b, a_w_in=a_w_in, a_w_out=a_w_out,
                  b_w_in=b_w_in, b_w_out=b_w_out, final_norm_w=final_norm_w)
    cur = np.asarray(x, np.float32)
    for layers, fin in LAUNCH_PLAN:
        cur = _run(layers, fin, cur, inputs)
    return cur
```

```python
import contextlib
import numpy as np
import ml_dtypes
import concourse.bass as bass
import concourse.mybir as mybir
from concourse.bass_utils import run_bass_kernel_spmd

F32 = mybir.dt.float32
BF16 = mybir.dt.bfloat16
AF = mybir.ActivationFunctionType
ALU = mybir.AluOpType
AX = mybir.AxisListType
NPBF = ml_dtypes.bfloat16

P = 128
D = 2048
S = 2048
NH = 16
DEPTH = 4
NCH = D // P
NT = S // P
EPS = 1e-6
SCALE = 128 ** -0.5
NEG = -30000.0
ENGS = ["pe", "act", "dve", "pool", "sp"]
N_CORES = 8
NHL = NH // 2
GROUPS = [[0, 1], [2, 3], [4, 5], [6, 7]]


class Op:
    __slots__ = ("eng", "fn", "waits", "signal", "count", "dsem", "dval", "inc", "phase", "raw")


class Buf:
    def __init__(self, name):
        self.name = name
        self.w = None
        self.r = {}


class Builder:
    def __init__(self, nc):
        self.nc = nc
        self.ops = {e: [] for e in ENGS}
        self.dcount = {}
        self.gstack = contextlib.ExitStack()
        self.esem = {e: self.gstack.enter_context(nc.semaphore("es_" + e)) for e in ENGS}
        self.dsem = {}
        self.ecount = {e: 0 for e in ENGS}
        self.seen = {e: {} for e in ENGS}
        self.phase = 0
        self.lastd = {}

    def sb(self, stack, name, shape, dt):
        self.uid = getattr(self, "uid", 0) + 1
        return stack.enter_context(self.nc.sbuf_tensor("s%d_%s" % (self.uid, name), list(shape), dt))

    def _op(self, eng, fn, waits):
        o = Op()
        o.eng = eng
        o.fn = fn
        o.waits = [w for w in waits if w is not None and w.phase == self.phase]
        for w in o.waits:
            if w.dsem is None:
                w.signal = True
        o.signal = False
        o.dsem = None
        o.count = 0
        o.dval = 0
        o.inc = 16
        o.phase = self.phase
        o.raw = ()
        self.ops[eng].append(o)
        return o

    def op(self, eng, fn, reads=(), writes=(), extra=(), dsem=None):
        waits = list(extra)
        raw = []
        for bf in reads:
            if bf.w is not None:
                waits.append(bf.w)
                raw.append(bf.w)
        for bf in writes:
            if bf.w is not None:
                waits.append(bf.w)
            waits.extend(bf.r.values())
        o = self._op(eng, fn, waits)
        o.raw = raw
        if dsem is not None:
            o.dsem = dsem
            self.dcount[dsem] = self.dcount.get(dsem, 0) + 16
            o.dval = self.dcount[dsem]
            self.lastd[dsem] = o
        key = eng if dsem is None else ("d", dsem)
        for bf in reads:
            bf.r[key] = o
        for bf in writes:
            bf.w = o
            bf.r = {}
        return o

    def dma(self, eng, out, in_, dsem, reads=(), writes=(), extra=(), **kw):
        return self.op(eng, lambda e: e.dma_start(out=out, in_=in_, **kw), reads, writes, extra, dsem=dsem)

    def coll(self, fn, dsem, reads=(), writes=()):
        o = self.op("pool", fn, reads, writes, dsem=dsem)
        self.dcount[dsem] += 1 - 16
        o.dval = self.dcount[dsem]
        o.inc = 1
        return o

    def end_phase(self, final_waits=()):
        nc = self.nc
        lasts = {}
        for e in ENGS:
            for o in reversed(self.ops[e]):
                if o.dsem is None and o.fn is not None:
                    lasts[e] = o
                    break
        dl = [o for o in self.lastd.values() if o.phase == self.phase]
        for e in ENGS:
            self._op(e, None, [lasts[x] for x in lasts if x != e] + dl)
        for e in ENGS:
            c = self.ecount[e]
            for o in self.ops[e]:
                if o.dsem is None and o.signal:
                    c += 1
                    o.count = c
            self.ecount[e] = c
        for k in self.dcount:
            if k not in self.dsem:
                self.dsem[k] = self.gstack.enter_context(nc.semaphore("ds_%s" % (k,)))
        ops, esem, dsem = self.ops, self.esem, self.dsem

        def run(engname):
            def f(eng):
                seen = self.seen[engname]
                for o in ops[engname]:
                    for w in o.waits:
                        if w.dsem is not None:
                            sem, val, key = dsem[w.dsem], w.dval, ("d", w.dsem)
                        else:
                            if w.eng == engname and (engname == "pe" or not any(w is r for r in o.raw)):
                                continue
                            sem, val, key = esem[w.eng], w.count, ("e", w.eng)
                        if seen.get(key, 0) >= val:
                            continue
                        eng.wait_ge(sem, val)
                        seen[key] = val
                    if o.fn is None:
                        continue
                    ins = o.fn(eng)
                    if o.dsem is not None:
                        ins.then_inc(dsem[o.dsem], o.inc)
                    elif o.signal:
                        ins.then_inc(esem[engname], 1)
            return f

        with nc.Block() as block:
            block.tensor(run("pe"))
            block.scalar(run("act"))
            block.vector(run("dve"))
            block.gpsimd(run("pool"))
            block.sync(run("sp"))
        self.ops = {e: [] for e in ENGS}
        self.phase += 1

    def close(self):
        self.gstack.close()


def build_program(layers, do_final, debug=False):
    nc = bass.Bass("TRN2", target_bir_lowering=False)
    dt_in = lambda n, s, d=F32: nc.dram_tensor(n, list(s), d, kind="ExternalInput").ap()
    x_in = dt_in("x_in", [S, D])
    cT_d = dt_in("cT", [P, NCH])
    nwT_d = dt_in("nwT", [P, DEPTH, NCH])
    fnw_d = dt_in("fnw", [1, D])
    mod_w = {l: dt_in("mod_w%d" % l, [D, 3 * D]) for l in layers}
    mod_b = dt_in("mod_b", [DEPTH, 3 * D])
    w_in_d = {l: dt_in("w_in%d" % l, [D, (10 if l % 2 == 0 else 4) * (D // 2)]) for l in layers}
    w_out_d = {l: dt_in("w_out%d" % l, [D, D]) for l in layers}
    ident_d = dt_in("ident", [P, P])
    tabA_d = dt_in("tabA", [NHL, P, 12 * P])
    tabB_d = dt_in("tabB", [NHL, P, 16 * P])
    pastb_d = dt_in("pastb", [P, NT * 8])
    x_out = nc.dram_tensor("x_out", [S, D], F32, kind="ExternalOutput").ap()
    xs = nc.dram_tensor("xs", [S, D], F32).ap()
    dk = dict(kind="ExternalOutput") if debug else {}
    oT_d = nc.dram_tensor("oT_s", [NHL, P, S], BF16, **dk).ap()
    og_t = [nc.dram_tensor("og%d" % i, [2 * P, S], BF16) for i in range(NHL)]
    modv = nc.dram_tensor("modv", [DEPTH, 3 * D], F32, **dk).ap()
    if debug:
        hT_dbg = nc.dram_tensor("hT_dbg", [P, NCH, S], BF16, kind="ExternalOutput").ap()
        g_dbg = nc.dram_tensor("g_dbg", [P, NT * 8], F32, kind="ExternalOutput").ap()
        e_dbg = nc.dram_tensor("e_dbg", [P, NT * 8], F32, kind="ExternalOutput").ap()
        m_dbg = nc.dram_tensor("m_dbg", [P, NT], F32, kind="ExternalOutput").ap()

    b = Builder(nc)
    g = contextlib.ExitStack()
    psb = [g.enter_context(nc.psum_tensor("ps%d" % i, [P, 512], F32)) for i in range(8)]
    PS = [Buf("ps%d" % i) for i in range(8)]
    ident_f = b.sb(g, "ident_f", [P, P], F32)
    ident_b = b.sb(g, "ident_b", [P, P], BF16)
    ones_b = b.sb(g, "ones_b", [P, P], BF16)
    hT = b.sb(g, "hT", [P, NCH, S], BF16)
    B_hT = Buf("hT")
    condT = b.sb(g, "condT", [P, NCH], BF16)
    nwT = b.sb(g, "nwT", [P, DEPTH, NCH], F32)
    B_const = Buf("const")
    B_id, B_nw, B_cond, B_ones = Buf("id"), Buf("nw"), Buf("cond"), Buf("ones")

    MW = 256
    NMU = 3 * D // MW
    ps_in = [0]

    def mod_bufs(st):
        return dict(w=[b.sb(st, "wmod%d" % i, [P, NCH, MW], BF16) for i in range(2)],
                    Bw=[Buf("wmod%d" % i) for i in range(2)],
                    mb=[b.sb(st, "mbp%d" % i, [1, MW], F32) for i in range(2)],
                    Bmb=[Buf("mbp%d" % i) for i in range(2)],
                    row=[b.sb(st, "mrow%d" % i, [1, MW], F32) for i in range(2)],
                    Brow=[Buf("mrow%d" % i) for i in range(2)], k=[0])

    mv_last = {}

    def mod_unit(m, cb, mbuf):
        k = mbuf["k"][0] % 2
        mbuf["k"][0] += 1
        wt, bw = mbuf["w"][k], mbuf["Bw"][k]
        cs = slice(cb * MW, (cb + 1) * MW)
        b.dma("pool", wt[:], mod_w[m][:, cs].rearrange("(c p) n -> p c n", p=P), "wmod%d" % k, writes=[bw])
        b.dma("sp", mbuf["mb"][k][:], mod_b[m:m + 1, cs], "mbp%d" % k, writes=[mbuf["Bmb"][k]])
        pi = ps_in[0] % 2
        ps_in[0] += 1
        for c in range(NCH):
            b.op("pe", lambda e, wt=wt, c=c, pi=pi: e.matmul(psb[pi][0:1, 0:MW], lhsT=condT[:, c:c + 1], rhs=wt[:, c, :],
                                                             start=(c == 0), stop=(c == NCH - 1)),
                 reads=[bw, B_cond], writes=[PS[pi]] if c == 0 else [])
        PS[pi].w = b.ops["pe"][-1]
        b.op("dve", lambda e, k=k, pi=pi: e.tensor_tensor(out=mbuf["row"][k][:], in0=psb[pi][0:1, 0:MW], in1=mbuf["mb"][k][:], op=ALU.add),
             reads=[PS[pi], mbuf["Bmb"][k]], writes=[mbuf["Brow"][k]])
        mv_last[k] = b.dma("sp", modv[m:m + 1, cs], mbuf["row"][k][:], "mvst%d" % k, reads=[mbuf["Brow"][k]])

    cT_f = b.sb(g, "cT_f", [P, NCH], F32)
    B_cT = Buf("cT")
    mb_glob = mod_bufs(g)
    b.dma("sp", ident_f[:], ident_d, "c0", writes=[B_id])
    b.dma("sp", nwT[:], nwT_d, "c1", writes=[B_nw])
    b.dma("sp", cT_f[:], cT_d, "c2", writes=[B_cT])
    b.op("dve", lambda e: e.tensor_copy(out=ident_b[:], in_=ident_f[:]), reads=[B_id], writes=[B_const])
    b.op("dve", lambda e: e.memset(ones_b[:], 1.0), writes=[B_ones])
    b.op("act", lambda e: e.activation(out=condT[:], in_=cT_f[:], func=AF.Silu), reads=[B_cT], writes=[B_cond])
    for cb in range(NMU):
        mod_unit(layers[0], cb, mb_glob)

    def norm_phase(l, src, final):
        with contextlib.ExitStack() as st:
            xt = [b.sb(st, "xt%d" % i, [P, D], F32) for i in range(2)]
            B_xt = [Buf("xt%d" % i) for i in range(2)]
            xn = [b.sb(st, "xn%d" % i, [P, D], F32) for i in range(2)]
            B_xn = [Buf("xn%d" % i) for i in range(2)]
            junk = b.sb(st, "junk", [P, D], BF16)
            ss = b.sb(st, "ss", [P, 2], F32)
            B_ss = Buf("ss")
            B_vec, B_sh, B_sc = Buf("vec"), Buf("sh"), Buf("sc")
            if final:
                fw = b.sb(st, "fw", [P, D], F32)
                b.dma("sp", fw[:], fnw_d.partition_broadcast(P), "fw", writes=[B_vec])
            else:
                sc = b.sb(st, "sc", [P, NCH], F32)
                sh = b.sb(st, "sh", [P, NCH], F32)
                weff = b.sb(st, "weff", [P, NCH], F32)
                b.dma("sp", sh[:], modv[l, 0:D].rearrange("(c p) -> p c", p=P), "sh", writes=[B_sh], extra=list(mv_last.values()), allow_slow_non_contiguous=True)
                b.dma("sp", sc[:], modv[l, D:2 * D].rearrange("(c p) -> p c", p=P), "sc", writes=[B_sc], extra=list(mv_last.values()), allow_slow_non_contiguous=True)
                b.op("dve", lambda e: e.scalar_tensor_tensor(out=weff[:], in0=sc[:], scalar=1.0, in1=nwT[:, l, :],
                                                            op0=ALU.add, op1=ALU.mult), reads=[B_sc, B_nw], writes=[B_vec])
            for t in range(NT):
                i = t % 2
                b.dma("sp", xt[i][:], src[t * P:(t + 1) * P, :], "xt%d" % i, writes=[B_xt[i]])
                b.op("dve", lambda e, i=i: e.memset(ss[:, i:i + 1], 0.0), writes=[B_ss])
                b.op("act", lambda e, i=i: e.activation(out=junk[:], in_=xt[i][:], func=AF.Square, accum_out=ss[:, i:i + 1]),
                     reads=[B_xt[i]], writes=[B_ss])
                b.op("dve", lambda e, i=i: e.tensor_scalar(out=ss[:, i:i + 1], in0=ss[:, i:i + 1], scalar1=1.0 / D, scalar2=EPS,
                                                           op0=ALU.mult, op1=ALU.add), reads=[B_ss], writes=[B_ss])
                b.op("act", lambda e, i=i: e.sqrt(out=ss[:, i:i + 1], in_=ss[:, i:i + 1]), reads=[B_ss], writes=[B_ss])
                b.op("dve", lambda e, i=i: e.reciprocal(out=ss[:, i:i + 1], in_=ss[:, i:i + 1]), reads=[B_ss], writes=[B_ss])
                if final:
                    b.op("dve", lambda e, i=i: e.scalar_tensor_tensor(out=xn[i][:], in0=xt[i][:], scalar=ss[:, i:i + 1], in1=fw[:],
                                                                      op0=ALU.mult, op1=ALU.mult),
                         reads=[B_xt[i], B_ss, B_vec], writes=[B_xn[i]])
                    b.dma("pool", x_out[t * P:(t + 1) * P, :], xn[i][:], "xo%d" % i, reads=[B_xn[i]])
                else:
                    b.op("dve", lambda e, i=i: e.tensor_scalar(out=xn[i][:], in0=xt[i][:], scalar1=ss[:, i:i + 1], scalar2=None,
                                                               op0=ALU.mult), reads=[B_xt[i], B_ss], writes=[B_xn[i]])
                    for q4 in range(4):
                        pi = (t * 4 + q4) % 4
                        for j in range(4):
                            c = q4 * 4 + j
                            b.op("pe", lambda e, i=i, c=c, j=j, pi=pi: e.transpose(out=psb[pi][:, j * P:(j + 1) * P],
                                                                                   in_=xn[i][:, c * P:(c + 1) * P], identity=ident_f[:]),
                                 reads=[B_xn[i], B_id], writes=[PS[pi]] if j == 0 else [])
                        PS[pi].w = b.ops["pe"][-1]
                        for j in range(4):
                            c = q4 * 4 + j
                            if j % 2 == 0:
                                b.op("act", lambda e, c=c, j=j, pi=pi, t=t: e.activation(
                                    out=hT[:, c, t * P:(t + 1) * P], in_=psb[pi][:, j * P:(j + 1) * P], func=AF.Identity,
                                    bias=sh[:, c:c + 1], scale=weff[:, c:c + 1]), reads=[PS[pi], B_vec, B_sh], writes=[B_hT])
                            else:
                                b.op("dve", lambda e, c=c, j=j, pi=pi, t=t: e.tensor_scalar(
                                    out=hT[:, c, t * P:(t + 1) * P], in0=psb[pi][:, j * P:(j + 1) * P],
                                    scalar1=weff[:, c:c + 1], scalar2=sh[:, c:c + 1], op0=ALU.mult, op1=ALU.add),
                                    reads=[PS[pi], B_vec, B_sh], writes=[B_hT])
            if debug and not final and l == layers[0]:
                b.dma("sp", hT_dbg, hT[:], "hdbg", reads=[B_hT])
            b.end_phase()

    def out_phase(l, w_out, src):
        with contextlib.ExitStack() as st:
            wo = b.sb(st, "wo", [P, NCH, D], BF16)
            B_wo = [Buf("wo%d" % i) for i in range(4)]
            ot = [b.sb(st, "ot%d" % i, [P, NH, 512], BF16) for i in range(2)]
            B_ot = [Buf("ot%d" % i) for i in range(2)]
            xt = [b.sb(st, "xq%d" % i, [P, D], F32) for i in range(2)]
            B_xt = [Buf("xq%d" % i) for i in range(2)]
            gate = b.sb(st, "gate", [P, D], F32)
            B_gate = Buf("gate")
            b.dma("sp", gate[:], modv[l:l + 1, 2 * D:3 * D].partition_broadcast(P), "gate", writes=[B_gate])
            for cb in range(4):
                b.dma("pool", wo[:, :, cb * 512:(cb + 1) * 512], w_out[:, cb * 512:(cb + 1) * 512].rearrange("(c p) n -> p c n", p=P),
                      "wo%d" % cb, writes=[B_wo[cb]])
            n = 0
            for tt in range(4):
                oi = tt % 2
                for hl in range(NHL):
                    b.dma("sp", ot[oi][:].rearrange("p (r h) t -> p r h t", r=2)[:, :, hl, :],
                          og_t[hl].ap()[:, tt * 512:(tt + 1) * 512].rearrange("(r p) t -> p r t", p=P), "ot%d" % oi,
                          writes=[B_ot[oi]] if hl == 0 else [])
                B_ot[oi].w = b.ops["sp"][-1]
                for s4 in range(4):
                    t = tt * 4 + s4
                    xi = t % 2
                    b.dma("sp", xt[xi][:], src[t * P:(t + 1) * P, :], "xq%d" % xi, writes=[B_xt[xi]])
                    for cb in range(4):
                        pi = n % 4
                        n += 1
                        for c in range(NH):
                            b.op("pe", lambda e, oi=oi, c=c, s4=s4, cb=cb, pi=pi: e.matmul(
                                psb[pi][:, :], lhsT=ot[oi][:, c, s4 * P:(s4 + 1) * P], rhs=wo[:, c, cb * 512:(cb + 1) * 512],
                                start=(c == 0), stop=(c == NH - 1)),
                                reads=[B_ot[oi], B_wo[cb]], writes=[PS[pi]] if c == 0 else [])
                        PS[pi].w = b.ops["pe"][-1]
                        b.op("dve", lambda e, pi=pi, cb=cb: e.tensor_tensor(
                            out=psb[pi][:, :], in0=psb[pi][:, :], in1=gate[:, cb * 512:(cb + 1) * 512], op=ALU.mult),
                            reads=[B_gate], writes=[PS[pi]])
                        b.op("dve", lambda e, pi=pi, xi=xi, cb=cb: e.tensor_tensor(
                            out=xt[xi][:, cb * 512:(cb + 1) * 512], in0=psb[pi][:, :], in1=xt[xi][:, cb * 512:(cb + 1) * 512], op=ALU.add),
                            reads=[PS[pi]], writes=[B_xt[xi]])
                    b.dma("act", xs[t * P:(t + 1) * P, :], xt[xi][:], "xqs%d" % xi, reads=[B_xt[xi]])
            b.end_phase()

    def mixer_phase(l, kind, w_in):
        ngrp = 3 if kind == "A" else 1
        dil = [1, 4, 16] if kind == "A" else [1]
        with contextlib.ExitStack() as st:
            NW = 6
            wr = [b.sb(st, "wr%d" % i, [P, NCH, P], BF16) for i in range(NW)]
            B_wr = [Buf("wr%d" % i) for i in range(NW)]
            QT = [b.sb(st, "QT%d" % i, [P, S], BF16) for i in range(2)]
            KT = [b.sb(st, "KT%d" % i, [P, S], BF16) for i in range(2)]
            V = [b.sb(st, "V%d" % i, [P, NT, P], BF16) for i in range(2)]
            B_Q = [Buf("Q%d" % i) for i in range(2)]
            B_K = [Buf("K%d" % i) for i in range(2)]
            B_V = [Buf("V%d" % i) for i in range(2)]
            SZ = [b.sb(st, "SZ%d" % i, [P, S], BF16) for i in range(2)]
            B_SZ = [Buf("SZ%d" % i) for i in range(2)]
            acc_o = b.sb(st, "acc_o", [P, S], F32)
            acc_s = b.sb(st, "acc_s", [P, S], F32)
            B_acc = Buf("acc")
            osb = [b.sb(st, "osb%d" % i, [P, S], BF16) for i in range(2)]
            B_osb = [Buf("osb%d" % i) for i in range(2)]
            tmp = [b.sb(st, "tmp%d" % i, [P, 512], F32) for i in range(2)]
            B_tmp = [Buf("tmp%d" % i) for i in range(2)]
            PT = [b.sb(st, "PT%d" % i, [P, 512], BF16) for i in range(3)]
            B_PT = [Buf("PT%d" % i) for i in range(3)]
            tw = 12 * P if kind == "A" else 16 * P
            tab = [b.sb(st, "tab%d" % i, [P, tw], F32) for i in range(2)]
            B_tab = [Buf("tab%d" % i) for i in range(2)]
            tab_d = tabA_d if kind == "A" else tabB_d
            if kind == "B":
                kmf = b.sb(st, "kmf", [P, 8], F32)
                kmb = b.sb(st, "kmb", [P, 8], BF16)
                pastb = b.sb(st, "pastb", [P, NT, 8], F32)
                gsb = b.sb(st, "gsb", [P, NT, 8], F32)
                g2 = b.sb(st, "g2", [P, NT, 8], F32)
                eq = b.sb(st, "eq", [P, NT, 8], F32)
                mx = b.sb(st, "mx", [P, NT], F32)
                nsel = [b.sb(st, "nsel%d" % i, [P, NT, 8], BF16) for i in range(2)]
                B_nsel = [Buf("nsel%d" % i) for i in range(2)]
                B_km, B_g = Buf("km"), Buf("g")
                B_km2, B_kj = Buf("km2"), Buf("kj")
                kjunk = b.sb(st, "kjunk", [P, 256], BF16)
                B_pb = Buf("pastb")
                b.dma("sp", pastb[:].rearrange("p a b -> p (a b)"), pastb_d, "pastb", writes=[B_pb])
            wk = [0]
            un = [0]
            nxt = layers[layers.index(l) + 1] if layers.index(l) + 1 < len(layers) else None
            mbm = mb_glob
            mod_todo = list(range(NMU)) if nxt is not None else []

            def load_w(col0):
                i = wk[0] % NW
                wk[0] += 1
                b.dma("pool", wr[i][:], w_in[:, col0:col0 + P].rearrange("(c p) n -> p c n", p=P), "wr%d" % i, writes=[B_wr[i]])
                return i

            def proj_T(col0, dst, B_dst, d, func=None):
                wi = load_w(col0)
                for tt in range(4):
                    pi = ps_in[0] % 2
                    ps_in[0] += 1
                    for c in range(NCH):
                        b.op("pe", lambda e, wi=wi, c=c, tt=tt, pi=pi: e.matmul(
                            psb[pi][:, :], lhsT=wr[wi][:, c, :], rhs=hT[:, c, tt * 512:(tt + 1) * 512],
                            start=(c == 0), stop=(c == NCH - 1)),
                            reads=[B_wr[wi], B_hT], writes=[PS[pi]] if c == 0 else [])
                    PS[pi].w = b.ops["pe"][-1]
                    if d == 1:
                        o_ap = dst[:, tt * 512:(tt + 1) * 512]
                        i_ap = psb[pi][:, :]
                    else:
                        nl = 512 // d
                        o_ap = dst[:, :].rearrange("p (r l) -> p r l", r=d)[:, :, tt * nl:(tt + 1) * nl]
                        i_ap = psb[pi][:, :].rearrange("p (l r) -> p r l", r=d)
                    if func is None:
                        b.op("dve", lambda e, o_ap=o_ap, i_ap=i_ap: e.tensor_copy(out=o_ap, in_=i_ap), reads=[PS[pi]], writes=[B_dst])
                    else:
                        b.op("act", lambda e, o_ap=o_ap, i_ap=i_ap: e.activation(out=o_ap, in_=i_ap, func=func),
                             reads=[PS[pi]], writes=[B_dst])

            def proj_V(col0, dst, B_dst, d):
                wi = load_w(col0)
                L = S // d
                for j4 in range(4):
                    pi = ps_in[0] % 2
                    ps_in[0] += 1
                    for jj in range(4):
                        j = j4 * 4 + jj
                        r, l0 = (j * P) // L, (j * P) % L
                        for c in range(NCH):
                            if d == 1:
                                lh = hT[:, c, j * P:(j + 1) * P]
                            else:
                                lh = hT[:, c, :].rearrange("p (l r) -> p r l", r=d)[:, r, l0:l0 + P]
                            b.op("pe", lambda e, wi=wi, c=c, lh=lh, jj=jj, pi=pi: e.matmul(
                                psb[pi][:, jj * P:(jj + 1) * P], lhsT=lh, rhs=wr[wi][:, c, :],
                                start=(c == 0), stop=(c == NCH - 1)),
                                reads=[B_wr[wi], B_hT], writes=[PS[pi]] if (c == 0 and jj == 0) else [])
                    PS[pi].w = b.ops["pe"][-1]
                    b.op("act", lambda e, j4=j4, pi=pi: e.copy(out=dst[:, j4 * 4:(j4 + 1) * 4, :].rearrange("p a b -> p (a b)"),
                                                              in_=psb[pi][:, :]), reads=[PS[pi]], writes=[B_dst])

            def attention(h, gi, si, ti, units, first_group):
                qt, kt, v = QT[si], KT[si], V[si]
                flat = []
                for u in units:
                    firsts, lasts = {}, {}
                    n = 0
                    for stp in u["steps"]:
                        for (pos, ktile, sel) in stp["blocks"]:
                            firsts.setdefault(pos, n)
                            lasts[pos] = n
                            n += 1
                    uo = un[0]
                    un[0] += 1
                    n = 0
                    for sidx, stp in enumerate(u["steps"]):
                        flat.append((u, uo, sidx, stp, firsts, lasts, n))
                        n += len(stp["blocks"])

                def qk(item):
                    u, uo, sidx, stp, firsts, lasts, n0 = item
                    n = sn[0]
                    sn[0] += 1
                    sb_i, k2, k3 = 2 + n % 2, n % 2, n % 3
                    blocks = stp["blocks"]
                    nb = len(blocks)
                    for i, (pos, ktile, sel) in enumerate(blocks):
                        b.op("pe", lambda e, i=i, ktile=ktile, sb_i=sb_i, sel=sel, qc=u["qcols"][pos]: e.matmul(
                            psb[sb_i][:, i * P:(i + 1) * P], lhsT=kt[:, ktile * P:(ktile + 1) * P], rhs=qt[:, qc:qc + P],
                            start=True, stop=(sel is None)),
                            reads=[B_K[si], B_Q[si]], writes=[PS[sb_i]] if i == 0 else [])
                        if sel is not None:
                            b.op("pe", lambda e, i=i, sb_i=sb_i, sel=sel: e.matmul(
                                psb[sb_i][:, i * P:(i + 1) * P], lhsT=sel, rhs=ident_b[:], start=False, stop=True),
                                reads=[stp["selbuf"], B_const])
                    PS[sb_i].w = b.ops["pe"][-1]
                    tb = stp["tab"]
                    b.op("dve", lambda e, sb_i=sb_i, k2=k2, nb=nb, tb=tb: e.scalar_tensor_tensor(
                        out=tmp[k2][:, 0:nb * P].rearrange("p (a b) -> p a b", b=P),
                        in0=psb[sb_i][:, 0:nb * P].rearrange("p (a b) -> p a b", b=P), scalar=SCALE, in1=tb,
                        op0=ALU.mult, op1=ALU.add), reads=[PS[sb_i], B_tab[ti]], writes=[B_tmp[k2]])
                    b.op("act", lambda e, k2=k2, k3=k3, nb=nb: e.activation(
                        out=PT[k3][:, 0:nb * P], in_=tmp[k2][:, 0:nb * P], func=AF.Exp),
                        reads=[B_tmp[k2]], writes=[B_PT[k3]])
                    return k3

                def pv(item, k3):
                    u, uo, sidx, stp, firsts, lasts, n0 = item
                    po, pso = 4 + uo % 2, 6 + uo % 2
                    npos = len(u["qcols"])
                    for bank, lhs_fn, rd in ((po, lambda ktile: v[:, ktile, :], [B_V[si], B_PT[k3]]),
                                             (pso, lambda ktile: ones_b[:], [B_ones, B_PT[k3]])):
                        for i, (pos, ktile, sel) in enumerate(stp["blocks"]):
                            n = n0 + i
                            b.op("pe", lambda e, i=i, pos=pos, lh=lhs_fn(ktile), k3=k3, bank=bank, f=(firsts[pos] == n),
                                 la=(lasts[pos] == n): e.matmul(
                                psb[bank][:, pos * P:(pos + 1) * P], lhsT=lh, rhs=PT[k3][:, i * P:(i + 1) * P],
                                start=f, stop=la), reads=rd, writes=[PS[bank]] if (sidx == 0 and i == 0) else [])
                    if sidx != len(u["steps"]) - 1:
                        return
                    PS[po].w = b.ops["pe"][-1]
                    PS[pso].w = b.ops["pe"][-1]
                    ao, as_ = u["acc"](acc_o), u["acc"](acc_s)
                    shp = u["pshape"]
                    pin_o = psb[po][:, 0:npos * P]
                    pin_s = psb[pso][:, 0:npos * P]
                    if shp is not None:
                        pin_o = pin_o.rearrange("p (a b) -> p a b", b=shp)
                        pin_s = pin_s.rearrange("p (a b) -> p a b", b=shp)
                    if first_group:
                        b.op("dve", lambda e, ao=ao, pin_o=pin_o: e.tensor_copy(out=ao, in_=pin_o), reads=[PS[po]], writes=[B_acc])
                        b.op("dve", lambda e, as_=as_, pin_s=pin_s: e.tensor_copy(out=as_, in_=pin_s), reads=[PS[pso]], writes=[B_acc])
                    else:
                        b.op("dve", lambda e, ao=ao, pin_o=pin_o: e.tensor_tensor(out=ao, in0=pin_o, in1=ao, op=ALU.add),
                             reads=[PS[po]], writes=[B_acc])
                        b.op("dve", lambda e, as_=as_, pin_s=pin_s: e.tensor_tensor(out=as_, in0=pin_s, in1=as_, op=ALU.add),
                             reads=[PS[pso]], writes=[B_acc])

                prev = None
                for item in flat:
                    k3 = qk(item)
                    if prev is not None:
                        pv(*prev)
                    prev = (item, k3)
                pv(*prev)

            sn = [0]
            pv_cnt = [0]
            sidx_g = 0
            DH = D // 2
            pend_cc = None
            for h in range(NHL):
                ti = h % 2
                b.dma("sp", tab[ti][:], tab_d[h], "tab%d" % ti, writes=[B_tab[ti]])
                zi = h % 2
                zcol = (9 * DH if kind == "A" else 3 * DH) + h * P
                for gi in range(ngrp):
                    d = dil[gi]
                    L = S // d
                    si = sidx_g % 2
                    sidx_g += 1
                    base = gi * 3 * DH if kind == "A" else 0
                    proj_T(base + h * P, QT[si], B_Q[si], d)
                    proj_T(base + DH + h * P, KT[si], B_K[si], d)
                    proj_V(base + 2 * DH + h * P, V[si], B_V[si], d)
                    if pend_cc is not None:
                        pend_cc()
                        pend_cc = None
                    if gi == 0:
                        for _ in range((NMU + NHL - 1) // NHL):
                            if mod_todo:
                                mod_unit(nxt, mod_todo.pop(0), mbm)
                    if gi == 0:
                        proj_T(zcol, SZ[zi], B_SZ[zi], 1, func=AF.Silu)
                    units = []
                    if kind == "A":
                        tb0 = gi * 4 * P
                        for u4 in range(4):
                            js = [u4 * 4 + i for i in range(4)]
                            ns = [(j * P % L) // P for j in js]
                            steps = []
                            if gi == 2:
                                steps.append(dict(blocks=[(i, js[i], None) for i in range(4)],
                                                  tab=tab[ti][:, tb0:tb0 + 4 * P].rearrange("p (a b) -> p a b", b=P)))
                            else:
                                for half in range(2):
                                    blocks = []
                                    for i in (2 * half, 2 * half + 1):
                                        if ns[i] >= 1:
                                            blocks.append((i, js[i] - 1, None))
                                        blocks.append((i, js[i], None))
                                    off = 4 - len(blocks)
                                    steps.append(dict(blocks=blocks, tab=tab[ti][:, tb0 + off * P:tb0 + 4 * P].rearrange(
                                        "p (a b) -> p a b", b=P)))
                            if d == 1:
                                accf = lambda a, u4=u4: a[:, u4 * 512:(u4 + 1) * 512]
                                pshape = None
                            elif d == 4:
                                accf = lambda a, u4=u4: a[:, :].rearrange("p (l r) -> p r l", r=4)[:, u4, :]
                                pshape = None
                            else:
                                accf = lambda a, u4=u4: a[:, :].rearrange("p (l r) -> p r l", r=16)[:, u4 * 4:(u4 + 1) * 4, :]
                                pshape = P
                            units.append(dict(qcols=[j * P for j in js], steps=steps, acc=accf, pshape=pshape))
                    else:
                        ni = h % 2
                        b.op("dve", lambda e: e.memset(kmf[:], 0.0), writes=[B_km])
                        for n in range(8):
                            b.op("act", lambda e, si=si, n=n: e.activation(out=kjunk[:], in_=KT[si][:, n * 256:(n + 1) * 256], func=AF.Identity,
                                                                            scale=1.0 / 256, accum_out=kmf[:, n:n + 1]),
                                 reads=[B_K[si], B_km], writes=[B_kj] if n < 7 else [B_kj, B_km2])
                        b.op("dve", lambda e: e.tensor_copy(out=kmb[:], in_=kmf[:]), reads=[B_km2], writes=[B_km])
                        pi = ps_in[0] % 2
                        ps_in[0] += 1
                        for t in range(NT):
                            b.op("pe", lambda e, t=t, si=si, pi=pi: e.matmul(psb[pi][:, t * 8:(t + 1) * 8], lhsT=QT[si][:, t * P:(t + 1) * P],
                                                                            rhs=kmb[:], start=True, stop=True),
                                 reads=[B_Q[si], B_km], writes=[PS[pi]] if t == 0 else [])
                        PS[pi].w = b.ops["pe"][-1]
                        g3v = lambda a: a[:]
                        mxb = lambda: mx[:].unsqueeze(2).to_broadcast([P, NT, 8])
                        b.op("dve", lambda e, pi=pi: e.tensor_tensor(out=gsb[:], in0=psb[pi][:, 0:NT * 8].rearrange("p (a b) -> p a b", b=8),
                                                                     in1=pastb[:], op=ALU.add), reads=[PS[pi], B_pb], writes=[B_g])
                        b.op("dve", lambda e: e.tensor_copy(out=g2[:], in_=gsb[:]), reads=[B_g], writes=[B_g])
                        for rep in range(2):
                            b.op("dve", lambda e: e.tensor_reduce(out=mx[:], in_=g2[:], axis=AX.X, op=ALU.max), reads=[B_g], writes=[B_g])
                            b.op("dve", lambda e: e.tensor_tensor(out=eq[:], in0=g2[:], in1=mxb(), op=ALU.is_ge), reads=[B_g], writes=[B_g])
                            b.op("dve", lambda e: e.scalar_tensor_tensor(out=g2[:], in0=eq[:], scalar=-1e30, in1=g2[:],
                                                                        op0=ALU.mult, op1=ALU.add), reads=[B_g], writes=[B_g])
                        b.op("dve", lambda e: e.tensor_reduce(out=mx[:], in_=g2[:], axis=AX.X, op=ALU.max), reads=[B_g], writes=[B_g])
                        b.op("dve", lambda e: e.tensor_scalar(out=mx[:], in0=mx[:], scalar1=-1e29, scalar2=None, op0=ALU.max),
                             reads=[B_g], writes=[B_g])
                        b.op("dve", lambda e: e.tensor_tensor(out=eq[:], in0=gsb[:], in1=mxb(), op=ALU.is_ge), reads=[B_g], writes=[B_g])
                        if debug and h == 1 and l == layers[0]:
                            b.dma("sp", g_dbg, gsb[:].rearrange("p a b -> p (a b)"), "gdbg", reads=[B_g])
                            b.dma("sp", e_dbg, eq[:].rearrange("p a b -> p (a b)"), "edbg", reads=[B_g])
                            b.dma("sp", m_dbg, mx[:], "mdbg", reads=[B_g])
                        b.op("dve", lambda e, ni=ni: e.tensor_scalar(out=nsel[ni][:], in0=eq[:], scalar1=-1.0, scalar2=-NEG,
                                                                     op0=ALU.add, op1=ALU.mult), reads=[B_g], writes=[B_nsel[ni]])
                        for q4 in range(4):
                            qts = [q4 * 4 + i for i in range(4)]
                            steps = []
                            for pos in range(4):
                                qtile = qts[pos]
                                for k0_ in range(0, qtile + 1, 4):
                                    kts = list(range(k0_, min(k0_ + 4, qtile + 1)))
                                    blocks = []
                                    for ktile in kts:
                                        sel = None
                                        if qtile // 2 > ktile // 2:
                                            sel = nsel[ni][:, qtile, (ktile // 2):(ktile // 2) + 1].to_broadcast([P, P])
                                        blocks.append((pos, ktile, sel))
                                    r0 = 15 - qtile + kts[0]
                                    steps.append(dict(blocks=blocks, selbuf=B_nsel[ni],
                                                      tab=tab[ti][:, r0 * P:(r0 + len(kts)) * P].rearrange("p (a b) -> p a b", b=P)))
                            units.append(dict(qcols=[q * P for q in qts], steps=steps,
                                              acc=lambda a, q4=q4: a[:, q4 * 512:(q4 + 1) * 512], pshape=None))
                    attention(h, gi, si, ti, units, gi == 0)
                oi = h % 2
                b.op("dve", lambda e: e.reciprocal(out=acc_s[:], in_=acc_s[:]), reads=[B_acc], writes=[B_acc])
                b.op("dve", lambda e: e.tensor_tensor(out=acc_o[:], in0=acc_o[:], in1=acc_s[:], op=ALU.mult), reads=[B_acc], writes=[B_acc])
                b.op("dve", lambda e, oi=oi, zi=zi: e.tensor_tensor(out=osb[oi][:], in0=acc_o[:], in1=SZ[zi][:], op=ALU.mult),
                     reads=[B_acc, B_SZ[zi]], writes=[B_osb[oi]])
                st_op = b.dma("sp", oT_d[h], osb[oi][:], "ost%d" % oi, reads=[B_osb[oi]])

                def issue_cc(h=h, st_op=st_op):
                    o = b.coll(lambda e: e.collective_compute("AllGather", ALU.bypass, replica_groups=GROUPS,
                                                              ins=[oT_d[h].opt()], outs=[og_t[h].ap().opt()]), "cc")
                    o.waits.append(st_op)
                pend_cc = issue_cc
            pend_cc()
            b.end_phase()

    src = x_in
    for l in layers:
        norm_phase(l, src, False)
        mixer_phase(l, "A" if l % 2 == 0 else "B", w_in_d[l])
        out_phase(l, w_out_d[l], src)
        src = xs
    if do_final:
        norm_phase(0, src, True)
    else:
        with contextlib.ExitStack() as st:
            cp = [b.sb(st, "cp%d" % i, [P, D], F32) for i in range(2)]
            B_cp = [Buf("cp%d" % i) for i in range(2)]
            for t in range(NT):
                i = t % 2
                b.dma("sp", cp[i][:], src[t * P:(t + 1) * P, :], "cpl%d" % i, writes=[B_cp[i]])
                b.dma("sp", x_out[t * P:(t + 1) * P, :], cp[i][:], "cps%d" % i, reads=[B_cp[i]])
            b.end_phase()
    g.close()
    b.close()
    return nc


def _consts():
    slopes = np.exp2(-8.0 * np.arange(1, NH + 1, dtype=np.float64) / NH)
    k = np.arange(P)[:, None].astype(np.float64)
    q = np.arange(P)[None, :].astype(np.float64)
    tabA = np.zeros((NH, P, 12, P), np.float32)
    for h in range(NH):
        for gi, d in enumerate([1, 4, 16]):
            for blk in range(4):
                prev = (blk % 2 == 0) and gi < 2
                if not prev:
                    step = q - k
                    ok = step >= 0
                else:
                    step = q - k + P
                    ok = step <= P
                tabA[h, :, gi * 4 + blk, :] = np.where(ok, np.maximum(-slopes[h] * d * step, NEG), NEG)
    tabB = np.zeros((NH, P, 16, P), np.float32)
    for h in range(NH):
        for dl in range(16):
            dist = q - k + P * dl
            tabB[h, :, 15 - dl, :] = np.where(dist >= 0, np.maximum(-slopes[h] * dist, NEG), NEG)
    pastb = np.zeros((P, NT, 8), np.float32)
    for t in range(NT):
        for n in range(8):
            if n >= t // 2:
                pastb[:, t, n] = -1e30
    return (np.eye(P, dtype=np.float32), tabA.reshape(NH, P, 12 * P), tabB.reshape(NH, P, 16 * P), pastb.reshape(P, NT * 8))


_PROG_CACHE = {}


def _run(layers, do_final, xin, inputs, debug=False):
    key = (tuple(layers), do_final, debug)
    if key not in _PROG_CACHE:
        _PROG_CACHE[key] = build_program(list(layers), do_final, debug)
    nc = _PROG_CACHE[key]
    ident, tabA, tabB, pastb = _consts()
    c = np.asarray(inputs["c"], np.float32)
    nw = np.asarray(inputs["norm_w"], np.float32)
    nwT = np.ascontiguousarray(nw.reshape(DEPTH, NCH, P).transpose(2, 0, 1))
    fnw = np.asarray(inputs["final_norm_w"], np.float32).reshape(1, D)
    mod_b = np.ascontiguousarray(np.asarray(inputs["mod_b"], np.float32))
    per_half = [dict(), dict()]
    shared = {"mod_b": mod_b}
    for l in layers:
        shared["mod_w%d" % l] = np.ascontiguousarray(np.asarray(inputs["mod_w"][l], np.float32))
        pre = "a" if l % 2 == 0 else "b"
        w = np.asarray(inputs[pre + "_w_in"][l // 2], np.float32)
        nparts = w.shape[1] // D
        w4 = w.reshape(D, nparts, 2, D // 2)
        for hh in range(2):
            per_half[hh]["w_in%d" % l] = np.ascontiguousarray(w4[:, :, hh, :].reshape(D, nparts * (D // 2)))
        shared["w_out%d" % l] = np.ascontiguousarray(np.asarray(inputs[pre + "_w_out"][l // 2], np.float32))
    in_maps = []
    for core in range(N_CORES):
        bb, hh = core // 2, core % 2
        m = dict(shared)
        m.update(per_half[hh])
        m["x_in"] = np.ascontiguousarray(xin[bb])
        m["cT"] = np.ascontiguousarray(c[bb].reshape(NCH, P).T)
        m["nwT"] = nwT
        m["fnw"] = fnw
        m["ident"] = ident
        m["tabA"] = np.ascontiguousarray(tabA[hh * NHL:(hh + 1) * NHL])
        m["tabB"] = np.ascontiguousarray(tabB[hh * NHL:(hh + 1) * NHL])
        m["pastb"] = pastb
        in_maps.append(m)
    res = run_bass_kernel_spmd(nc, in_maps, core_ids=list(range(N_CORES)))
    if debug:
        _run.dbg = res.results
    return np.stack([np.asarray(res.results[2 * i]["x_out"], np.float32) for i in range(N_CORES // 2)], 0)


LAUNCH_PLAN = [([0, 1, 2, 3], True)]


def kernel(x, c, norm_w, mod_w, mod_b, a_w_in, a_w_out, b_w_in, b_w_out, final_norm_w):
    inputs = dict(c=c, norm_w=norm_w, mod_w=mod_w, mod_b=mod_b, a_w_in=a_w_in, a_w_out=a_w_out,
                  b_w_in=b_w_in, b_w_out=b_w_out, final_norm_w=final_norm_w)
    cur = np.asarray(x, np.float32)
    for layers, fin in LAUNCH_PLAN:
        cur = _run(layers, fin, cur, inputs)
    return cur
```
